# Optimizing a Trainium2 kernel written in Bass

```python
import jax
import jax.numpy as jnp
from jax import lax
import numpy as np

D_MODEL = 1024
BATCH = 2
SEQ = 8192
DEPTH = 1
DEC_BATCH = 128
DEC_SEQ = 8
PAST_LEN = 2048
PAGE_SIZE = 128

H_GLA = 4
GLA_VAL_W = D_MODEL // 2
GLA_DV = GLA_VAL_W // H_GLA
GLA_DK = GLA_DV // 2
GLA_KEY_W = H_GLA * GLA_DK
GATE_RANK = 16
GATE_TAU = 16.0
GLA_CHUNK = 64
H_DIL = 4
DIL_W = D_MODEL - GLA_VAL_W
DIL_DH = DIL_W // H_DIL
DIL_PAIRS = ((128, 1), (512, 4), (2048, 16))
WINDOW_MAX = 2048
DIL_BLOCK = 128
MIX_W = GLA_VAL_W + DIL_W
PROJ_WIDTHS = (GLA_KEY_W, GLA_KEY_W, GLA_VAL_W, GLA_VAL_W, GATE_RANK, DIL_W, DIL_W, DIL_W)
PROJ_DIM = sum(PROJ_WIDTHS)
MEM_TOKENS = 256
MEM_HEADS = 4
MEM_DH = D_MODEL // MEM_HEADS
N_GROUPS = 4
EXPERTS_PER_GROUP = 8
N_EXPERTS = N_GROUPS * EXPERTS_PER_GROUP
TOP_K = 2
EXPERT_HIDDEN = D_MODEL // 2
ALPHA = (2.0 * DEPTH) ** 0.25
BETA = (8.0 * DEPTH) ** -0.25
LN_EPS = 1e-5
NEG_INF = -1e30

kernel_name = 'hymba_gla_dilated_hmoe_step'


def layer_norm(x, g, b):
    xf = x.astype(jnp.float32)
    mu = jnp.mean(xf, -1, keepdims=True)
    var = jnp.mean(jnp.square(xf - mu), -1, keepdims=True)
    return ((xf - mu) * lax.rsqrt(var + LN_EPS) * g + b).astype(x.dtype)


def project_mixer_inputs(x, w_in, w_gate_lr, b_gate):
    B, S, _ = x.shape
    pts, acc = [], 0
    for w in PROJ_WIDTHS[:-1]:
        acc += w
        pts.append(acc)
    h = x @ w_in
    q_g, k_g, v_g, r_g, a_lr, q_d, k_d, v_d = jnp.split(h, pts, axis=-1)
    q_g = q_g.reshape(B, S, H_GLA, GLA_DK) * (GLA_DK ** -0.5)
    k_g = k_g.reshape(B, S, H_GLA, GLA_DK)
    v_g = v_g.reshape(B, S, H_GLA, GLA_DV)
    log_a = jax.nn.log_sigmoid((a_lr @ w_gate_lr + b_gate).astype(jnp.float32)) / GATE_TAU
    log_a = log_a.reshape(B, S, H_GLA, GLA_DK)
    q_d = q_d.reshape(B, S, H_DIL, DIL_DH)
    k_d = k_d.reshape(B, S, H_DIL, DIL_DH)
    v_d = v_d.reshape(B, S, H_DIL, DIL_DH)
    return q_g, k_g, v_g, r_g, log_a, q_d, k_d, v_d


def gla_chunked(q, k, v, log_a, s0, chunk):
    B, S, H, dk = q.shape
    dv = v.shape[-1]
    n = S // chunk

    def to_chunks(t):
        return t.astype(jnp.float32).reshape(B, n, chunk, H, t.shape[-1]).transpose(1, 0, 3, 2, 4)

    qc, kc, vc, ac = to_chunks(q), to_chunks(k), to_chunks(v), to_chunks(log_a)
    causal = jnp.tril(jnp.ones((chunk, chunk), dtype=bool))

    def step(s, inp):
        qi, ki, vi, ai = inp
        b = jnp.cumsum(ai, axis=2)
        b_last = b[:, :, -1:, :]
        q_t = qi * jnp.exp(b)
        k_t = ki * jnp.exp(-b)
        a_in = jnp.where(causal, jnp.einsum('bhik,bhjk->bhij', q_t, k_t), 0.0)
        o = jnp.einsum('bhij,bhjv->bhiv', a_in, vi) + jnp.einsum('bhik,bhkv->bhiv', q_t, s)
        k_end = ki * jnp.exp(b_last - b)
        s_new = jnp.exp(b_last[:, :, 0, :])[..., None] * s + jnp.einsum('bhjk,bhjv->bhkv', k_end, vi)
        return s_new, o

    s_fin, o = lax.scan(step, s0.astype(jnp.float32), (qc, kc, vc, ac))
    o = o.transpose(1, 0, 3, 2, 4).reshape(B, S, H, dv)
    return o, s_fin


def combine_by_denominator(outs, lses):
    w = jax.nn.softmax(jnp.stack(lses, 0), axis=0)
    return jnp.sum(w[..., None] * jnp.stack(outs, 0), axis=0)


def dilated_attention_prompt(q, k, v):
    B, S, H, dh = q.shape
    scale = dh ** -0.5
    outs, lses = [], []
    for window, dil in DIL_PAIRS:
        span = window // dil
        L = S // dil
        nb = -(-L // DIL_BLOCK)
        pad = nb * DIL_BLOCK - L

        def to_sub(t):
            t = t.reshape(B, L, dil, H, dh).transpose(0, 2, 3, 1, 4)
            t = jnp.pad(t, ((0, 0), (0, 0), (0, 0), (0, pad), (0, 0)))
            return t.reshape(B, dil, H, nb, DIL_BLOCK, dh)

        def with_prev(t):
            prev = jnp.pad(t[:, :, :, :-1], ((0, 0), (0, 0), (0, 0), (1, 0), (0, 0), (0, 0)))
            return jnp.concatenate([prev, t], axis=4)

        qs = to_sub(q)
        kb = with_prev(to_sub(k))
        vb = with_prev(to_sub(v))
        s = jnp.einsum('bdhnqc,bdhnkc->bdhnqk', qs, kb).astype(jnp.float32) * scale
        qi = jnp.arange(DIL_BLOCK)[:, None]
        ki = jnp.arange(2 * DIL_BLOCK)[None, :]
        dist = qi + DIL_BLOCK - ki
        key_pos = jnp.arange(nb)[:, None, None] * DIL_BLOCK + ki[None] - DIL_BLOCK
        valid = (dist >= 0) & (dist <= span) & (key_pos >= 0)
        s = jnp.where(valid, s, NEG_INF)
        m = jnp.max(s, -1, keepdims=True)
        p = jnp.exp(s - m)
        den = jnp.sum(p, -1, keepdims=True)
        o = jnp.einsum('bdhnqk,bdhnkc->bdhnqc', p, vb.astype(jnp.float32)) / den
        lse = (m + jnp.log(den))[..., 0]
        o = o.reshape(B, dil, H, nb * DIL_BLOCK, dh)[:, :, :, :L].transpose(0, 3, 1, 2, 4).reshape(B, S, H, dh)
        lse = lse.reshape(B, dil, H, nb * DIL_BLOCK)[..., :L].transpose(0, 3, 1, 2).reshape(B, S, H)
        outs.append(o)
        lses.append(lse)
    return combine_by_denominator(outs, lses)


def dilated_attention_sample(q, k_new, v_new, k_buf, v_buf):
    DB, T, H, dh = q.shape
    scale = dh ** -0.5
    buf_len = k_buf.shape[1]
    k_all = jnp.concatenate([k_buf, k_new], axis=1)
    v_all = jnp.concatenate([v_buf, v_new], axis=1)
    outs, lses = [], []
    for window, dil in DIL_PAIRS:
        span = window // dil
        idx = buf_len + jnp.arange(T)[:, None] - dil * jnp.arange(span + 1)[None, :]
        valid = idx >= 0
        flat = jnp.maximum(idx, 0).reshape(-1)
        kg = jnp.take(k_all, flat, axis=1).reshape(DB, T, span + 1, H, dh)
        vg = jnp.take(v_all, flat, axis=1).reshape(DB, T, span + 1, H, dh)
        s = jnp.einsum('bthc,btkhc->bthk', q, kg).astype(jnp.float32) * scale
        s = jnp.where(valid[None, :, None, :], s, NEG_INF)
        m = jnp.max(s, -1, keepdims=True)
        p = jnp.exp(s - m)
        den = jnp.sum(p, -1, keepdims=True)
        o = jnp.einsum('bthk,btkhc->bthc', p, vg.astype(jnp.float32)) / den
        outs.append(o)
        lses.append((m + jnp.log(den))[..., 0])
    new_len = min(WINDOW_MAX, k_all.shape[1])
    return combine_by_denominator(outs, lses), k_all[:, -new_len:], v_all[:, -new_len:]


def mixer_output(o_gla, r_g, o_dil, g_gla, w_out):
    B, S = o_gla.shape[:2]
    on = o_gla * lax.rsqrt(jnp.mean(jnp.square(o_gla), -1, keepdims=True) + LN_EPS)
    on = on.reshape(B, S, GLA_VAL_W) * g_gla * jax.nn.silu(r_g.astype(jnp.float32))
    cat = jnp.concatenate([on.astype(r_g.dtype), o_dil.reshape(B, S, DIL_W).astype(r_g.dtype)], axis=-1)
    return cat @ w_out


def memory_attention(x, mem_k, mem_v, w_mem_q, w_mem_o):
    B, S, _ = x.shape
    q = (x @ w_mem_q).reshape(B, S, MEM_HEADS, MEM_DH)
    s = jnp.einsum('bshc,bmhc->bhsm', q, mem_k).astype(jnp.float32) * (MEM_DH ** -0.5)
    p = jax.nn.softmax(s, axis=-1).astype(mem_v.dtype)
    o = jnp.einsum('bhsm,bmhc->bshc', p, mem_v).reshape(B, S, D_MODEL)
    return o @ w_mem_o


def hierarchical_moe(x, w_route_group, b_route_group, w_route_expert, b_route_expert, w_exp_gate, w_exp_up, w_exp_down):
    B, S, D = x.shape
    xt = x.reshape(-1, D)
    g_logits = (xt @ w_route_group).astype(jnp.float32) + b_route_group
    g_prob = jax.nn.softmax(g_logits, axis=-1)
    g_sel = jnp.argmax(g_logits, axis=-1)
    g_w = jnp.take_along_axis(g_prob, g_sel[:, None], axis=-1)
    e_logits = ((xt @ w_route_expert).astype(jnp.float32) + b_route_expert).reshape(-1, N_GROUPS, EXPERTS_PER_GROUP)
    e_in = jnp.take_along_axis(e_logits, g_sel[:, None, None], axis=1)[:, 0]
    top_v, top_i = lax.top_k(e_in, TOP_K)
    top_w = jax.nn.softmax(top_v, axis=-1) * g_w
    expert_id = g_sel[:, None] * EXPERTS_PER_GROUP + top_i
    gate = jnp.sum(jax.nn.one_hot(expert_id, N_EXPERTS, dtype=jnp.float32) * top_w[..., None], axis=1)
    gate = gate.astype(x.dtype)
    y = jnp.zeros_like(xt)
    for e in range(N_EXPERTS):
        h = jax.nn.silu(xt @ w_exp_gate[e]) * (xt @ w_exp_up[e])
        y = y + gate[:, e:e + 1] * (h @ w_exp_down[e])
    return y.reshape(B, S, D)


def post_mixer(x, mix, mem_k, mem_v, ln_mix_g, ln_mix_b, w_mem_q, w_mem_o, ln_mem_g, ln_mem_b,
               w_route_group, b_route_group, w_route_expert, b_route_expert, w_exp_gate, w_exp_up, w_exp_down,
               ln_ffn_g, ln_ffn_b):
    x = layer_norm(ALPHA * x + mix, ln_mix_g, ln_mix_b)
    x = layer_norm(ALPHA * x + memory_attention(x, mem_k, mem_v, w_mem_q, w_mem_o), ln_mem_g, ln_mem_b)
    moe = hierarchical_moe(x, w_route_group, b_route_group, w_route_expert, b_route_expert, w_exp_gate, w_exp_up, w_exp_down)
    return layer_norm(ALPHA * x + moe, ln_ffn_g, ln_ffn_b)


def setup_inputs(seed: int = 0) -> dict:
    key = jax.random.key(seed)
    ks = jax.random.split(key, 30)

    def nrm(k, shape, scale=1.0):
        return jax.random.normal(k, shape, jnp.float32) * scale

    buf = min(WINDOW_MAX, PAST_LEN)
    return {
        'x_prompt': nrm(ks[0], (BATCH, SEQ, D_MODEL)),
        'x_sample': nrm(ks[1], (DEC_BATCH, DEC_SEQ, D_MODEL)),
        'mem_prompt': nrm(ks[2], (BATCH, MEM_TOKENS, D_MODEL)),
        'cache_dil_k': nrm(ks[3], (DEPTH, DEC_BATCH, buf, H_DIL, DIL_DH)),
        'cache_dil_v': nrm(ks[4], (DEPTH, DEC_BATCH, buf, H_DIL, DIL_DH)),
        'state_gla': nrm(ks[5], (DEPTH, DEC_BATCH, H_GLA, GLA_DK, GLA_DV), 0.3),
        'cache_mem_k': nrm(ks[6], (DEPTH, DEC_BATCH, MEM_TOKENS, MEM_HEADS, MEM_DH)),
        'cache_mem_v': nrm(ks[7], (DEPTH, DEC_BATCH, MEM_TOKENS, MEM_HEADS, MEM_DH)),
        'w_in': nrm(ks[8], (DEPTH, D_MODEL, PROJ_DIM), D_MODEL ** -0.5),
        'w_gate_lr': nrm(ks[9], (DEPTH, GATE_RANK, GLA_KEY_W), GATE_RANK ** -0.5),
        'b_gate': nrm(ks[10], (DEPTH, GLA_KEY_W), 0.1),
        'g_gla_norm': 1.0 + nrm(ks[11], (DEPTH, GLA_VAL_W), 0.02),
        'w_out': nrm(ks[12], (DEPTH, MIX_W, D_MODEL), BETA * MIX_W ** -0.5),
        'ln_mix_g': 1.0 + nrm(ks[13], (DEPTH, D_MODEL), 0.02),
        'ln_mix_b': nrm(ks[14], (DEPTH, D_MODEL), 0.02),
        'w_mem_q': nrm(ks[15], (DEPTH, D_MODEL, D_MODEL), D_MODEL ** -0.5),
        'w_mem_k': nrm(ks[16], (DEPTH, D_MODEL, D_MODEL), D_MODEL ** -0.5),
        'w_mem_v': nrm(ks[17], (DEPTH, D_MODEL, D_MODEL), D_MODEL ** -0.5),
        'w_mem_o': nrm(ks[18], (DEPTH, D_MODEL, D_MODEL), BETA * D_MODEL ** -0.5),
        'ln_mem_g': 1.0 + nrm(ks[19], (DEPTH, D_MODEL), 0.02),
        'ln_mem_b': nrm(ks[20], (DEPTH, D_MODEL), 0.02),
        'w_route_group': nrm(ks[21], (DEPTH, D_MODEL, N_GROUPS), D_MODEL ** -0.5),
        'b_route_group': nrm(ks[22], (DEPTH, N_GROUPS), 0.01),
        'w_route_expert': nrm(ks[23], (DEPTH, D_MODEL, N_EXPERTS), D_MODEL ** -0.5),
        'b_route_expert': nrm(ks[24], (DEPTH, N_EXPERTS), 0.01),
        'w_exp_gate': nrm(ks[25], (DEPTH, N_EXPERTS, D_MODEL, EXPERT_HIDDEN), D_MODEL ** -0.5),
        'w_exp_up': nrm(ks[26], (DEPTH, N_EXPERTS, D_MODEL, EXPERT_HIDDEN), D_MODEL ** -0.5),
        'w_exp_down': nrm(ks[27], (DEPTH, N_EXPERTS, EXPERT_HIDDEN, D_MODEL), BETA * EXPERT_HIDDEN ** -0.5),
        'ln_ffn_g': 1.0 + nrm(ks[28], (DEPTH, D_MODEL), 0.02),
        'ln_ffn_b': nrm(ks[29], (DEPTH, D_MODEL), 0.02),
    }


def reference(x_prompt, x_sample, mem_prompt, cache_dil_k, cache_dil_v, state_gla, cache_mem_k, cache_mem_v,
              w_in, w_gate_lr, b_gate, g_gla_norm, w_out, ln_mix_g, ln_mix_b,
              w_mem_q, w_mem_k, w_mem_v, w_mem_o, ln_mem_g, ln_mem_b,
              w_route_group, b_route_group, w_route_expert, b_route_expert,
              w_exp_gate, w_exp_up, w_exp_down, ln_ffn_g, ln_ffn_b):
    xp, xs = x_prompt, x_sample
    Bp = xp.shape[0]
    dk_p, dv_p, sg_p, mk_p, mv_p = [], [], [], [], []
    dk_s, dv_s, sg_s = [], [], []
    for l in range(DEPTH):
        post = (ln_mix_g[l], ln_mix_b[l], w_mem_q[l], w_mem_o[l], ln_mem_g[l], ln_mem_b[l],
                w_route_group[l], b_route_group[l], w_route_expert[l], b_route_expert[l],
                w_exp_gate[l], w_exp_up[l], w_exp_down[l], ln_ffn_g[l], ln_ffn_b[l])
        q_g, k_g, v_g, r_g, log_a, q_d, k_d, v_d = project_mixer_inputs(xp, w_in[l], w_gate_lr[l], b_gate[l])
        s0 = jnp.zeros((Bp, H_GLA, GLA_DK, GLA_DV), jnp.float32)
        o_g, s_fin = gla_chunked(q_g, k_g, v_g, log_a, s0, GLA_CHUNK)
        o_d = dilated_attention_prompt(q_d, k_d, v_d)
        mix = mixer_output(o_g, r_g, o_d, g_gla_norm[l], w_out[l])
        mem_k = (mem_prompt @ w_mem_k[l]).reshape(Bp, MEM_TOKENS, MEM_HEADS, MEM_DH)
        mem_v = (mem_prompt @ w_mem_v[l]).reshape(Bp, MEM_TOKENS, MEM_HEADS, MEM_DH)
        xp = post_mixer(xp, mix, mem_k, mem_v, *post)
        keep = min(WINDOW_MAX, k_d.shape[1])
        dk_p.append(k_d[:, -keep:])
        dv_p.append(v_d[:, -keep:])
        sg_p.append(s_fin.astype(x_prompt.dtype))
        mk_p.append(mem_k)
        mv_p.append(mem_v)
        q_g, k_g, v_g, r_g, log_a, q_d, k_d, v_d = project_mixer_inputs(xs, w_in[l], w_gate_lr[l], b_gate[l])
        o_g, s_new = gla_chunked(q_g, k_g, v_g, log_a, state_gla[l], xs.shape[1])
        o_d, nk, nv = dilated_attention_sample(q_d, k_d, v_d, cache_dil_k[l], cache_dil_v[l])
        mix = mixer_output(o_g, r_g, o_d, g_gla_norm[l], w_out[l])
        xs = post_mixer(xs, mix, cache_mem_k[l], cache_mem_v[l], *post)
        dk_s.append(nk)
        dv_s.append(nv)
        sg_s.append(s_new.astype(state_gla.dtype))
    new_dil_k_prompt = jnp.stack(dk_p, 0)
    new_dil_v_prompt = jnp.stack(dv_p, 0)
    new_state_gla_prompt = jnp.stack(sg_p, 0)
    new_mem_k_prompt = jnp.stack(mk_p, 0)
    new_mem_v_prompt = jnp.stack(mv_p, 0)
    new_dil_k_sample = jnp.stack(dk_s, 0)
    new_dil_v_sample = jnp.stack(dv_s, 0)
    new_state_gla_sample = jnp.stack(sg_s, 0)
    return (xp, xs, new_dil_k_prompt, new_dil_v_prompt, new_state_gla_prompt, new_mem_k_prompt, new_mem_v_prompt, new_dil_k_sample, new_dil_v_sample, new_state_gla_sample)
```

```python
import contextlib
import numpy as np
import concourse.bass as bass
import concourse.mybir as mybir
from concourse.bass_utils import run_bass_kernel_spmd

F32 = mybir.dt.float32
BF16 = mybir.dt.bfloat16
AF = mybir.ActivationFunctionType
ALU = mybir.AluOpType
AX = mybir.AxisListType

NEG = -30000.0
ALPHA = 2.0 ** 0.25
EPS = 1e-5
NTOK = 2176
NT = 17


class Tok:
    __slots__ = ("writer", "readers", "psum")

    def __init__(self, psum=False):
        self.writer = None
        self.readers = []
        self.psum = psum


class Op:
    __slots__ = ("eng", "fn", "deps", "is_dma", "signal", "sigval", "slot", "idx", "is_barrier")

    def __init__(self, eng, fn, is_dma):
        self.eng = eng
        self.fn = fn
        self.deps = []
        self.is_dma = is_dma
        self.signal = False
        self.sigval = 0
        self.slot = None
        self.idx = -1
        self.is_barrier = False


class Prog:
    NSLOT = 8
    CE = ("pe", "act", "dve", "pool")

    def __init__(self, nc):
        self.nc = nc
        self.ops = []
        self.engs = {"pe": nc.tensor, "act": nc.scalar, "dve": nc.vector, "pool": nc.gpsimd, "sp": nc.sync}
        self.last = {}
        self.dmas = []
        self.enabled = True

    def _add(self, eng, fn, reads, writes, is_dma):
        if not self.enabled:
            return None
        op = Op(eng, fn, is_dma)
        op.idx = len(self.ops)
        import sys as _sys
        fr = _sys._getframe(2)
        lines = []
        while fr is not None and len(lines) < 4:
            lines.append(fr.f_lineno)
            fr = fr.f_back
        self.lines = getattr(self, "lines", {})
        self.lines[op.idx] = lines
        deps = {}
        for t in reads:
            if t.writer is not None:
                deps[t.writer.idx] = t.writer
            if t.psum:
                for r in t.readers:
                    if r.eng != eng:
                        deps[r.idx] = r
        for t in writes:
            if t.writer is not None:
                deps[t.writer.idx] = t.writer
            for r in t.readers:
                deps[r.idx] = r
        for t in reads:
            t.readers.append(op)
        for t in writes:
            t.writer = op
            t.readers = []
        for d in deps.values():
            if (not d.is_dma) and (not is_dma) and d.eng == "pe" and eng == "pe":
                continue
            op.deps.append(d)
            d.signal = True
        self.ops.append(op)
        if is_dma:
            self.dmas.append(op)
        else:
            self.last[eng] = op
        return op

    def op(self, eng, fn, reads=(), writes=()):
        return self._add(eng, fn, list(reads), list(writes), False)

    def dma(self, queue, fn, reads=(), writes=()):
        o = self._add(queue, fn, list(reads), list(writes), True)
        if o is not None:
            o.signal = True
        return o

    def barrier(self):
        if not self.enabled:
            return
        prev = list(self.last.values()) + list(self.dmas)
        self.dmas = []
        for e in ("pe", "act", "dve", "pool", "sp"):
            op = Op(e, lambda eng: eng.nop(), False)
            op.is_barrier = True
            op.idx = len(self.ops)
            for d in prev:
                op.deps.append(d)
                d.signal = True
            self.ops.append(op)
            if e != "sp":
                self.last[e] = op

    def emit(self):
        nc = self.nc
        with contextlib.ExitStack() as es:
            esem = {e: es.enter_context(nc.semaphore("s_" + e)) for e in self.CE}
            dsem = {q: [es.enter_context(nc.semaphore("d_%s%d" % (q, i))) for i in range(self.NSLOT)]
                    for q in ("sp", "pool", "act")}
            ecount = {e: 0 for e in esem}
            dcount = {q: [0] * self.NSLOT for q in dsem}
            dnext = {q: 0 for q in dsem}
            waited = {}

            def wait(engname, sem, val):
                key = (engname, id(sem))
                if waited.get(key, 0) >= val:
                    return
                waited[key] = val
                self.engs[engname].wait_ge(sem, val)

            import os as _os
            _kstop = int(_os.environ.get("KSTOP", "0")) or len(self.ops)
            for op in self.ops[:_kstop]:
                e = op.eng
                for d in op.deps:
                    if d.is_dma:
                        wait(e, dsem[d.eng][d.slot], d.sigval)
                    else:
                        wait(e, esem[d.eng], d.sigval)
                if op.is_dma:
                    s = dnext[e]
                    dnext[e] = (s + 1) % self.NSLOT
                    if dcount[e][s] > 0:
                        wait(e, dsem[e][s], dcount[e][s])
                    ins = op.fn(self.engs[e])
                    dcount[e][s] += 16
                    op.slot = s
                    op.sigval = dcount[e][s]
                    ins.then_inc(dsem[e][s], 16)
                else:
                    if getattr(op, "is_barrier", False) and e == "pe":
                        self.marks = getattr(self, "marks", []) + [ecount["pe"]]
                    ins = op.fn(self.engs[e])
                    if op.signal and e in esem:
                        ecount[e] += 1
                        op.sigval = ecount[e]
                        ins.then_inc(esem[e], 1)
            self.stats = (dict(ecount), {q: list(v) for q, v in dcount.items()})
            for q in dsem:
                for s in range(self.NSLOT):
                    if dcount[q][s] > 0:
                        wait("sp", dsem[q][s], dcount[q][s])
            for e in esem:
                if ecount[e] > 0:
                    wait("sp", esem[e], ecount[e])


def _const_tables():
    c = {}
    j = np.arange(128)[:, None]
    i = np.arange(128)[None, :]
    c["uneg"] = np.where(j <= i, -1.0 / 16, 0.0).astype(np.float32)
    c["mgtneg"] = np.where(j > i, -1.0 / 16, 0.0).astype(np.float32)
    am = (j <= i).astype(np.float32)
    c["amask"] = np.ascontiguousarray(np.broadcast_to(am[:, None, :], (128, 4, 128)))
    sameb = (j // 8) == (i // 8)
    c["uneg_s"] = np.where(sameb & (j <= i), -1.0 / 16, 0.0).astype(np.float32)
    c["mgtneg_s"] = np.where(sameb & (j > i), -1.0 / 16, 0.0).astype(np.float32)
    ams = (sameb & (j <= i)).astype(np.float32)
    c["amask_s"] = np.ascontiguousarray(np.broadcast_to(ams[:, None, :], (128, 4, 128)))
    c["rowmask"] = ((np.arange(128)[:, None] // 8) == np.arange(16)[None, :]).astype(np.float32)
    qrel = np.arange(256)[None, :]
    kk = np.arange(128)[:, None]
    c["dilmask"] = np.where((qrel - kk >= 0) & (qrel - kk <= 128), 0.0, NEG).astype(np.float32)
    part = np.arange(128)[:, None, None]
    jj = np.arange(16)[None, :, None]
    t = np.arange(8)[None, None, :]
    delta = 2048 + t - (16 * part + jj)

    def mult(d):
        return ((d >= 0) & (d <= 128)).astype(np.float32) + ((d >= 0) & (d % 4 == 0) & (d <= 512)) + \
               ((d >= 0) & (d % 16 == 0) & (d <= 2048))
    sm = mult(delta).astype(np.float32)
    c["smult"] = np.ascontiguousarray(np.broadcast_to(sm[:, None, :, :], (128, 4, 16, 8))).reshape(128, 512)
    dn = (i % 8) - (j % 8)
    c["snew"] = np.where(sameb, mult(dn), 0.0).astype(np.float32)
    c["ident"] = np.eye(128, dtype=np.float32)
    return c


def build_nc(phases="STUPBCD"):
    nc = bass.Bass("TRN2", target_bir_lowering=False)

    ext_in, ext_out = [], []

    def din(name, shape, ph="*", dt=F32):
        on = ph == "*" or any(c in phases for c in ph)
        if on:
            ext_in.append(name)
        return nc.dram_tensor(name, list(shape), dt, kind="ExternalInput" if on else "Internal").ap()

    def dout(name, shape, ph="*"):
        on = ph == "*" or any(c in phases for c in ph)
        if on:
            ext_out.append(name)
        return nc.dram_tensor(name, list(shape), F32, kind="ExternalOutput" if on else "Internal").ap()

    def dscr(name, shape, dt):
        return nc.dram_tensor(name, list(shape), dt, kind="Internal").ap()

    xp = din("xp", [8192, 1024], "P")
    halo_bias = din("halo_bias", [128, 1])
    xs_in = din("xs", [128, 1024], "S")
    xres = din("xres", [NTOK, 1024], "C")
    memp = din("memp", [256, 1024], "C")
    cdk = din("cdk", [16, 2048, 512], "U")
    cdv = din("cdv", [16, 2048, 512], "U")
    sgla = din("sgla", [16, 4, 64, 128], "ST")
    cmk = din("cmk", [16, 256, 1024], "C")
    cmv = din("cmv", [16, 256, 1024], "C")
    w_in = din("w_in", [1024, 3088])
    wlr_in = din("wlr", [17, 256])
    g4_in = din("g4", [128, 4])
    w_out = din("w_out", [1024, 1024], "C")
    w_q = din("w_q", [1024, 1024], "C")
    w_k = din("w_k", [1024, 1024], "C")
    w_v = din("w_v", [1024, 1024], "C")
    w_o = din("w_o", [1024, 1024], "C")
    lnp = din("lnp", [6, 128, 1024], "CD")
    w_r = din("w_r", [1024, 36], "C")
    b_r = din("b_r", [128, 36], "C")
    w_eg = din("w_eg", [32, 1024, 512], "D")
    w_eu = din("w_eu", [32, 1024, 512], "D")
    w_ed = din("w_ed", [32, 512, 1024], "D")
    ctab = {k: din("c_" + k, list(v.shape)) for k, v in _const_tables().items()}
    y_out = dout("y", [NTOK, 1024], "D")
    ndk_p = dout("ndk_p", [2048, 512], "P")
    ndv_p = dout("ndv_p", [2048, 512], "P")
    sg_p = dout("sg_p", [4, 64, 128], "P")
    mk_p = dout("mk_p", [256, 1024], "C")
    mv_p = dout("mv_p", [256, 1024], "C")
    ndk_s = dout("ndk_s", [16, 2048, 512], "SU")
    ndv_s = dout("ndv_s", [16, 2048, 512], "SU")
    sg_s = dout("sg_s", [16, 4, 64, 128], "T")
    dbg = dout("dbg", [128, 2048], "Z")
    vscr = dscr("vscr", [4096, 512], BF16)
    qscr = dscr("qscr", [128, 4, 2048], BF16)
    kscr = dscr("kscr", [128, 4, 4096], BF16)
    x2scr = dscr("x2scr", [NTOK, 1024], F32)

    P = Prog(nc)

    def mm(out, lhsT, rhs, start, stop, reads, writes, skip=False):
        if skip:
            P.op("pe", lambda e: e.matmul(out, lhsT, rhs, start=start, stop=stop, skip_group_check=True), reads, writes)
        else:
            P.op("pe", lambda e: e.matmul(out, lhsT, rhs, start=start, stop=stop), reads, writes)

    def tr(out, in_, ident, reads, writes):
        P.op("pe", lambda e: e.transpose(out, in_, ident), reads, writes)

    def act(out, in_, func, reads, writes, bias=None, scale=1.0):
        if bias is None:
            P.op("act", lambda e: e.activation(out=out, in_=in_, func=func, scale=scale), reads, writes)
        else:
            P.op("act", lambda e: e.activation(out=out, in_=in_, func=func, bias=bias, scale=scale), reads, writes)

    def cp(eng, out, in_, reads, writes):
        if eng == "act":
            P.op("act", lambda e: e.copy(out, in_), reads, writes)
        else:
            P.op(eng, lambda e: e.tensor_copy(out, in_), reads, writes)

    def tt(out, in0, in1, op, reads, writes, eng="dve"):
        P.op(eng, lambda e: e.tensor_tensor(out=out, in0=in0, in1=in1, op=op), reads, writes)

    def stt(out, in0, scalar, in1, op0, op1, reads, writes, eng="dve"):
        P.op(eng, lambda e: e.scalar_tensor_tensor(out=out, in0=in0, scalar=scalar, in1=in1, op0=op0, op1=op1), reads, writes)

    def ts(out, in0, s1, s2, op0, op1, reads, writes, eng="dve"):
        if s2 is None:
            P.op(eng, lambda e: e.tensor_scalar(out=out, in0=in0, scalar1=s1, scalar2=None, op0=op0), reads, writes)
        else:
            P.op(eng, lambda e: e.tensor_scalar(out=out, in0=in0, scalar1=s1, scalar2=s2, op0=op0, op1=op1), reads, writes)

    def recip(out, in_, reads, writes):
        P.op("dve", lambda e: e.reciprocal(out, in_), reads, writes)

    def red(out, in_, op, reads, writes):
        P.op("dve", lambda e: e.tensor_reduce(out=out, in_=in_, axis=AX.X, op=op), reads, writes)

    def dma(q, out, in_, reads, writes):
        P.dma(q, lambda e: e.dma_start(out=out, in_=in_), reads, writes)

    with contextlib.ExitStack() as es0:
        def SB(es, name, shape, dt):
            return es.enter_context(nc.sbuf_tensor("sb_" + name, list(shape), dt))

        def PS(name, shape, dt):
            return es0.enter_context(nc.psum_tensor("ps_" + name, list(shape), dt))

        pX = PS("pX", [128, 1024], BF16)
        pF = [PS("pF0", [128, 512], F32), PS("pF1", [128, 512], F32)]
        pZ = PS("pZ", [128, 512], F32)
        pB = PS("pB", [128, 512], F32)
        pV = PS("pV", [128, 512], F32)
        pA = PS("pA", [128, 512], F32)
        pO = PS("pO", [128, 512], F32)
        tX, tF0, tF1, tZa, tBa, tV, tA, tO = [Tok(psum=True) for _ in range(8)]
        tZb, tBb = tZa, tBa
        tF = [tF0, tF1]
        fcnt = [0]

        def nextF():
            i = fcnt[0] % 2
            fcnt[0] += 1
            return pF[i], tF[i]

        ident_f = SB(es0, "ident_f", [128, 128], F32)
        ident_b = SB(es0, "ident_b", [128, 128], BF16)
        ones_b = SB(es0, "ones_b", [128, 128], BF16)
        ones_f = SB(es0, "ones_f", [128, 128], F32)
        cst = SB(es0, "cst", [128, 4], F32)
        hb = SB(es0, "hb", [128, 1], F32)
        catT = SB(es0, "catT", [128, 8, NTOK], BF16)
        gate = SB(es0, "gate", [128, NT, 32], F32)
        tC, tcat, tx2T, tgate = Tok(), Tok(), Tok(), Tok()
        tcatt = [Tok() for _ in range(NT)]
        x2T, tx2t = catT, tcatt
        dma("sp", ident_f[:], ctab["ident"], [], [tC])
        cp("dve", ident_b[:], ident_f[:], [tC], [tC])
        P.op("pool", lambda e: e.memset(ones_b[:], 1.0), [], [tC])
        P.op("pool", lambda e: e.memset(ones_f[:], 1.0), [], [tC])
        P.op("pool", lambda e: e.memset(cst[:, 0:1], 1.0), [], [tC])
        P.op("pool", lambda e: e.memset(cst[:, 1:2], 0.0), [], [tC])
        P.op("pool", lambda e: e.memset(cst[:, 2:3], EPS), [], [tC])
        dma("sp", hb[:], halo_bias, [], [tC])
        ONE = cst[:, 0:1]

        with contextlib.ExitStack() as esA:
            wi = SB(esA, "wi", [128, 8, 3088], BF16)
            twi = Tok()
            dma("pool", wi[:], w_in.rearrange("(kc p) n -> p kc n", p=128), [], [twi])
            wlr = SB(esA, "wlr", [17, 256], F32)
            g4 = SB(esA, "g4", [128, 4], F32)
            cm = {}
            for k in ("uneg", "mgtneg", "amask", "uneg_s", "mgtneg_s", "amask_s", "rowmask", "snew"):
                shp = list(ctab[k].shape)
                cm[k] = SB(esA, "c_" + k, shp, F32)
            tM = Tok()
            dma("sp", wlr[:], wlr_in, [], [tM])
            dma("sp", g4[:], g4_in, [], [tM])
            for k in cm:
                dma("sp", cm[k][:], ctab[k], [], [tM])

            alrT = SB(esA, "alrT", [32, 512], F32)
            talr = Tok()
            P.op("pool", lambda e: e.memset(alrT[:], 1.0), [], [talr])
            qg_f = SB(esA, "qg_f", [128, 2, 512], F32)
            kg_f = SB(esA, "kg_f", [128, 2, 512], F32)
            sr_f = SB(esA, "sr_f", [128, 4, 512], F32)
            tqg, tkg, tsr = Tok(), Tok(), Tok()
            e1 = SB(esA, "e1", [128, 256], F32)
            sp = SB(esA, "sp", [128, 256], F32)
            eb = SB(esA, "eb", [128, 2, 128], F32)
            enb = SB(esA, "enb", [128, 2, 128], F32)
            qt = SB(esA, "qt", [128, 2, 128], BF16)
            kt = SB(esA, "kt", [128, 4, 128], BF16)
            A_bf = SB(esA, "A_bf", [128, 4, 128], BF16)
            v_bf = SB(esA, "v_bf", [128, 512], BF16)
            ek = SB(esA, "ek", [128, 256], F32)
            kend = SB(esA, "kend", [128, 256], BF16)
            S32 = SB(esA, "S32", [128, 2, 128], F32)
            S_bf = SB(esA, "S_bf", [128, 4, 128], BF16)
            sq = SB(esA, "sq", [128, 512], BF16)
            rstd = SB(esA, "rstd", [128, 4, 128], F32)
            t1 = SB(esA, "t1", [128, 4, 128], F32)
            te1, tsp, teb, tenb, tqt, tkt, tAbf, tvbf, tek, tkend, tS32, tSbf, tsq, trstd, tt1 = [Tok() for _ in range(15)]
            P.op("pool", lambda e: e.memset(S32[:], 0.0), [], [tS32])
            P.op("pool", lambda e: e.memset(S_bf[:], 0.0), [], [tSbf])
            P.op("pool", lambda e: e.memset(kt[:], 0.0), [], [tkt])

            pZ3 = pZ
            pB3 = pB[:, 0:256].rearrange("p (a b) -> p a b", a=2)
            pA3 = pA[:, :].rearrange("p (a b) -> p a b", a=4)
            pO3 = pO[:, :].rearrange("p (a b) -> p a b", a=4)
            pV3 = pV[:, :].rearrange("p (a b) -> p a b", a=2)

            def fm_proj(col0, ncol, xT, txT, t0, ntok, evac):
                pf, tf = nextF()
                for kc in range(8):
                    mm(pf[0:ncol, 0:ntok], wi[:, kc, col0:col0 + ncol], xT[:, kc, t0:t0 + ntok], kc == 0, kc == 7,
                       [twi, txT], [tf])
                evac(pf[0:ncol, 0:ntok], tf)

            def tm_proj(dst, tdst, col0, ncol, xT, txT, t0):
                for kc in range(8):
                    mm(dst, xT[:, kc, t0:t0 + 128], wi[:, kc, col0:col0 + ncol], kc == 0, kc == 7, [twi, txT], [tdst])

            def block_fm_gla(xT, txT, ntok, with_q):
                fm_proj(1536, 16, xT, txT, 0, ntok,
                        lambda p, tf: cp("dve", alrT[0:16, 0:ntok], p, [tf], [talr]))
                if with_q:
                    for pr in range(2):
                        fm_proj(pr * 128, 128, xT, txT, 0, ntok,
                                lambda p, tf, pr=pr: cp("act", qg_f[:, pr, 0:ntok], p, [tf], [tqg]))
                        fm_proj(256 + pr * 128, 128, xT, txT, 0, ntok,
                                lambda p, tf, pr=pr: cp("dve", kg_f[:, pr, 0:ntok], p, [tf], [tkg]))
                    for h in range(4):
                        fm_proj(1024 + h * 128, 128, xT, txT, 0, ntok,
                                lambda p, tf, h=h: act(sr_f[:, h, 0:ntok], p, AF.Silu, [tf], [tsr]))

            def gla_chunk(xT, txT, c0, mode, cat_t0=None, tcat_tok=None):
                full = mode != "pre"
                sfx = "_s" if mode == "sample" else ""
                uneg, mgtneg, amask = cm["uneg" + sfx], cm["mgtneg" + sfx], cm["amask" + sfx]
                mm(pZ[:, 0:256], alrT[0:17, c0:c0 + 128], wlr[0:17, :], True, True, [talr, tM], [tZa])
                act(e1[:], pZ[:, 0:256], AF.Exp, [tZa], [te1], scale=-1.0)
                act(sp[:], e1[:], AF.Ln, [te1], [tsp], bias=ONE)
                tm_proj(pB[:, 256:512], tBb, 256, 256, xT, txT, c0)
                tm_proj(pV[:, :], tV, 512, 512, xT, txT, c0)
                cp("act", v_bf[:], pV[:, :], [tV], [tvbf])
                mm(pZ[:, 256:512], mgtneg[:], sp[:], True, True, [tM, tsp], [tZb])
                act(ek[:], pZ[:, 256:512], AF.Exp, [tZb], [tek])
                tt(kend[:], pB[:, 256:512], ek[:], ALU.mult, [tBb, tek], [tkend])
                if full:
                    for pr in range(2):
                        mm(pB3[:, pr, :], sp[:, pr * 128:(pr + 1) * 128], uneg[:], True, True, [tsp, tM], [tBa])
                    act(eb[:], pB3, AF.Exp, [tBa], [teb])
                    act(enb[:], pB3, AF.Exp, [tBa], [tenb], scale=-1.0)
                    stt(qt[:], qg_f[:, :, c0:c0 + 128], 0.125, eb[:], ALU.mult, ALU.mult, [tqg, teb], [tqt])
                    for hp in range(2):
                        rs = slice(64 * hp, 64 * hp + 64)
                        tt(kt[rs, hp::2, :], kg_f[rs, :, c0:c0 + 128], enb[rs, :, :], ALU.mult, [tkg, tenb], [tkt])
                    for h in range(4):
                        mm(pA3[:, h, :], kt[:, h, :], qt[:, h // 2, :], True, True, [tkt, tqt], [tA])
                    tt(A_bf[:], pA3, amask[:], ALU.mult, [tA, tM], [tAbf])
                else:
                    for pr in range(2):
                        mm(pB3[:, pr, 127:128], sp[:, pr * 128:(pr + 1) * 128], uneg[:, 127:128], True, True,
                           [tsp, tM], [tBa])
                    act(eb[:, :, 127:128], pB3[:, :, 127:128], AF.Exp, [tBa], [teb])
                return full

            def gla_out(c0, cat_t0, tcat_tok):
                act(sq[:], pO[:, :], AF.Square, [tO], [tsq])
                mm(pA[:, :], ones_b[:], sq[:], True, True, [tC, tsq], [tA])
                ts(rstd[:].rearrange("p a b -> p (a b)"), pA[:, :], 1.0 / 128, EPS, ALU.mult, ALU.add, [tA], [trstd])
                act(rstd[:].rearrange("p a b -> p (a b)"), rstd[:].rearrange("p a b -> p (a b)"), AF.Ln, [trstd], [trstd])
                act(rstd[:].rearrange("p a b -> p (a b)"), rstd[:].rearrange("p a b -> p (a b)"), AF.Exp, [trstd], [trstd], scale=-0.5)
                for h in range(4):
                    stt(t1[:, h, :], pO3[:, h, :], g4[:, h:h + 1], rstd[:, h, :], ALU.mult, ALU.mult, [tO, tM, trstd], [tt1])
                tt(catT[:, 0:4, cat_t0:cat_t0 + 128], t1[:], sr_f[:, :, c0:c0 + 128], ALU.mult, [tt1, tsr], [tcat_tok])

            def state_update():
                for pr in range(2):
                    mm(pV3[:, pr, :], kend[:, pr * 128:(pr + 1) * 128], v_bf[:, pr * 256:(pr + 1) * 256], True, True,
                       [tkend, tvbf], [tV])
                for h in range(4):
                    pr, r0 = h // 2, 64 * (h % 2)
                    stt(S32[r0:r0 + 64, pr, :], S32[r0:r0 + 64, pr, :], eb[r0:r0 + 64, pr, 127:128],
                        pV3[r0:r0 + 64, pr, (h % 2) * 128:(h % 2) * 128 + 128], ALU.mult, ALU.add, [tS32, teb, tV], [tS32])
                for hp in range(2):
                    rs = slice(64 * hp, 64 * hp + 64)
                    cp("act" if hp else "dve", S_bf[rs, hp::2, :], S32[rs, :, :], [tS32], [tSbf])

            P.enabled = "S" in phases
            with contextlib.ExitStack() as esS:
                xs_bf = SB(esS, "xs_bf", [128, 1024], BF16)
                xsT = SB(esS, "xsT", [128, 8, 128], BF16)
                qdT_s = SB(esS, "qdT_s", [128, 4, 128], BF16)
                kdT_s = SB(esS, "kdT_s", [128, 4, 128], BF16)
                kd_new = SB(esS, "kd_new", [128, 512], F32)
                vd_new = SB(esS, "vd_new", [128, 512], F32)
                vd_new_b = SB(esS, "vd_new_b", [128, 512], BF16)
                esG = contextlib.ExitStack()
                S0b = SB(esG, "S0b", [128, 16, 4, 128], BF16)
                S0f = SB(esG, "S0f", [128, 16, 2, 128], F32)
                Snew = S0f
                Vblk = SB(esG, "Vblk", [128, 16, 128], BF16)
                txs, txsT, tqd, tkd, tkdn, tvdn, tvdnb, tS0, tSn, tVb = [Tok() for _ in range(10)]
                dma("pool", xs_bf[:], xs_in, [], [txs])
                sg_v = sgla.rearrange("b (pr h2) k v -> (h2 k) b pr v", h2=2)
                P.op("pool", lambda e: e.memset(S0b[:], 0.0), [], [tS0])
                sg_h = sgla.rearrange("b (pr h2) k v -> h2 k b pr v", h2=2)
                for b4 in range(4):
                    for hp in range(2):
                        dma("pool", S0b[64 * hp:64 * hp + 64, b4 * 4:(b4 + 1) * 4, hp::2, :], sg_h[hp, :, b4 * 4:(b4 + 1) * 4], [], [tS0])
                    dma("sp", S0f[:, b4 * 4:(b4 + 1) * 4], sg_v[:, b4 * 4:(b4 + 1) * 4], [], [tS0])
                for kc in range(8):
                    tr(pX[:, kc * 128:(kc + 1) * 128], xs_bf[:, kc * 128:(kc + 1) * 128], ident_b[:], [txs, tC], [tX])
                cp("dve", xsT[:].rearrange("p a b -> p (a b)"), pX[:, :], [tX], [txsT])
                block_fm_gla(xsT, txsT, 128, True)
                for h in range(4):
                    fm_proj(1552 + h * 128, 128, xsT, txsT, 0, 128,
                            lambda p, tf, h=h: cp("dve", qdT_s[:, h, :], p, [tf], [tqd]))
                    fm_proj(2064 + h * 128, 128, xsT, txsT, 0, 128,
                            lambda p, tf, h=h: cp("act", kdT_s[:, h, :], p, [tf], [tkd]))
                pf, tf = nextF()
                tm_proj(pf[:, :], tf, 2064, 512, xsT, txsT, 0)
                cp("dve", kd_new[:], pf[:, :], [tf], [tkdn])
                pf, tf = nextF()
                tm_proj(pf[:, :], tf, 2576, 512, xsT, txsT, 0)
                cp("dve", vd_new[:], pf[:, :], [tf], [tvdn])
                cp("act", vd_new_b[:], pf[:, :], [tf], [tvdnb])
                for b in range(16):
                    dma("sp", ndk_s[b, 2040:2048, :], kd_new[8 * b:8 * b + 8, :], [tkdn], [])
                    dma("sp", ndv_s[b, 2040:2048, :], vd_new[8 * b:8 * b + 8, :], [tvdn], [])
                gla_chunk(xsT, txsT, 0, "sample")
                for h in range(4):
                    pr, r0 = h // 2, 64 * (h % 2)
                    mm(pO3[:, h, :], v_bf[:, h * 128:(h + 1) * 128], A_bf[:, h, :], True, False, [tvbf, tAbf], [tO], skip=True)
                    for b in range(16):
                        mm(pO3[:, h, 8 * b:8 * b + 8], S0b[:, b, h, :], qt[:, pr, 8 * b:8 * b + 8],
                           False, b == 15, [tS0, tqt], [tO], skip=True)
                gla_out(0, 2048, tcatt[16])
                P.enabled = "T" in phases
                for h in range(4):
                    pr, r0 = h // 2, 64 * (h % 2)
                    for b in range(16):
                        ts(Vblk[:, b, :], v_bf[:, h * 128:(h + 1) * 128], cm["rowmask"][:, b:b + 1], None, ALU.mult, None,
                           [tvbf, tM], [tVb])
                    for q4 in range(4):
                        pf, tf = nextF()
                        mm(pf[:, :], kend[:, pr * 128:(pr + 1) * 128],
                           Vblk[:, q4 * 4:(q4 + 1) * 4, :].rearrange("p a b -> p (a b)"), True, True, [tkend, tVb], [tf])
                        for bb in range(4):
                            b = q4 * 4 + bb
                            stt(Snew[r0:r0 + 64, b, pr, :], S0f[r0:r0 + 64, b, pr, :], eb[r0:r0 + 64, pr, 8 * b + 7:8 * b + 8],
                                pf[r0:r0 + 64, bb * 128:(bb + 1) * 128], ALU.mult, ALU.add, [tS0, teb, tf], [tSn])
                sgs_v = sg_s.rearrange("b (pr h2) k v -> (h2 k) b pr v", h2=2)
                for b4 in range(4):
                    dma("sp", sgs_v[:, b4 * 4:(b4 + 1) * 4], Snew[:, b4 * 4:(b4 + 1) * 4], [tSn], [])

                P.enabled = ("S" in phases) or ("T" in phases)
                P.barrier()
                esG.close()
                P.enabled = "U" in phases
                Kc = SB(esS, "Kc", [128, 16, 512], BF16)
                Vc = SB(esS, "Vc", [128, 16, 512], BF16)
                KcTb = [SB(esS, "KcT%d" % i, [128, 16, 128], BF16) for i in range(2)]
                tKcTb = [Tok(), Tok()]
                Pe = SB(esS, "Pe", [128, 512], F32)
                Pm = SB(esS, "Pm", [128, 4, 16, 8], BF16)
                smult = SB(esS, "smult", [128, 512], F32)
                Pn = SB(esS, "Pn", [128, 128], F32)
                Pnb = SB(esS, "Pnb", [128, 4, 128], BF16)
                rd = SB(esS, "rd", [128, 4, 128], F32)
                tKc, tVc, tPe, tPm, tsm, tPn, tPnb, trd = [Tok() for _ in range(8)]
                dma("sp", smult[:], ctab["smult"], [], [tsm])
                for h in range(4):
                    pf, tf = nextF()
                    mm(pf[:, 0:128], kdT_s[:, h, :], qdT_s[:, h, :], True, True, [tkd, tqd], [tf])
                    act(Pn[:], pf[:, 0:128], AF.Exp, [tf], [tPn], scale=128.0 ** -0.5)
                    tt(Pnb[:, h, :], Pn[:], cm["snew"][:], ALU.mult, [tPn, tM], [tPnb])
                Pms = SB(esS, "Pms", [128, 4, 8], F32)
                tPms = Tok()
                pVn = pV[:, :].rearrange("p (a b) -> p a b", a=4)
                for h in range(4):
                    mm(pO3[:, h, :], vd_new_b[:, h * 128:(h + 1) * 128], Pnb[:, h, :], True, False, [tvdnb, tPnb], [tO], skip=True)
                    mm(pVn[:, h, :], ones_b[:], Pnb[:, h, :], True, True, [tC, tPnb], [tV])
                for b in range(16):
                    dma("sp", ndk_s[b, 0:2040, :].rearrange("(a r) f -> a (r f)", a=120),
                        cdk[b, 8:2048, :].rearrange("(a r) f -> a (r f)", a=120), [], [])
                    dma("sp", ndv_s[b, 0:2040, :].rearrange("(a r) f -> a (r f)", a=120),
                        cdv[b, 8:2048, :].rearrange("(a r) f -> a (r f)", a=120), [], [])
                    dma("pool", Kc[:], cdk[b].rearrange("(p j) f -> p j f", j=16), [], [tKc])
                    dma("pool", Vc[:], cdv[b].rearrange("(p j) f -> p j f", j=16), [], [tVc])
                    pf, tf = nextF()
                    pf4 = pf[:, :].rearrange("p (h j t) -> p h j t", h=4, j=16)
                    for h in range(4):
                        KcT, tKcT = KcTb[h % 2], tKcTb[h % 2]
                        for j2 in range(2):
                            for jj in range(8):
                                j = j2 * 8 + jj
                                tr(pX[:, jj * 128:(jj + 1) * 128], Kc[:, j, h * 128:(h + 1) * 128], ident_b[:], [tKc, tC], [tX])
                            cp("dve" if j2 == 0 else "act", KcT[:, j2 * 8:(j2 + 1) * 8, :].rearrange("p a b -> p (a b)"),
                               pX[:, :], [tX], [tKcT])
                        for j in range(16):
                            mm(pf4[:, h, j, :], KcT[:, j, :], qdT_s[:, h, 8 * b:8 * b + 8], True, True, [tKcT, tqd], [tf])
                    act(Pe[:], pf[:, :], AF.Exp, [tf], [tPe], scale=128.0 ** -0.5)
                    tt(Pm[:].rearrange("p h j t -> p (h j t)"), Pe[:], smult[:], ALU.mult, [tPe, tsm], [tPm])
                    for h in range(4):
                        for j in range(16):
                            last = (b == 15 and j == 15)
                            mm(pO3[:, h, 8 * b:8 * b + 8], Vc[:, j, h * 128:(h + 1) * 128], Pm[:, h, j, :], False, last,
                               [tVc, tPm], [tO], skip=True)
                    P.op("dve", lambda e: e.tensor_reduce(out=Pms[:], in_=Pm[:].rearrange("p h j t -> p h t j"), axis=AX.X, op=ALU.add),
                         [tPm], [tPms])
                    mm(pA[:, b * 32:(b + 1) * 32], ones_f[:], Pms[:].rearrange("p h t -> p (h t)"), True, True, [tC, tPms], [tA])
                rd4 = rd[:].rearrange("p h (b t) -> p h b t", b=16)
                cp("act", rd[:], pVn, [tV], [trd])
                tt(rd4, pA[:, :].rearrange("p (b h t) -> p h b t", b=16, h=4), rd4, ALU.add, [tA, trd], [trd])
                recip(rd[:], rd[:], [trd], [trd])
                tt(catT[:, 4:8, 2048:2176], pO3, rd[:], ALU.mult, [tO, trd], [tcatt[16]])
            P.barrier()

            P.enabled = "P" in phases
            with contextlib.ExitStack() as esP:
                xld = [SB(esP, "xld%d" % i, [128, 4, 1024], BF16) for i in range(2)]
                xTb = [SB(esP, "xTb%d" % i, [128, 8, 512], BF16) for i in range(2)]
                txld = [Tok(), Tok()]
                txTb = [Tok(), Tok()]
                stg = [SB(esP, "stg%d" % i, [128, 512], BF16) for i in range(2)]
                tstg = [Tok(), Tok()]
                stf = [SB(esP, "stf%d" % i, [128, 512], F32) for i in range(2)]
                tstf = [Tok(), Tok()]
                scnt = [0, 0]
                tscrQ, tscrK, tscrV = Tok(), Tok(), Tok()
                for blk in range(16):
                    mode = "pre" if blk < 8 else ("halo" if blk < 12 else "main")
                    bi = blk % 2
                    xl, xT, txl, txT = xld[bi], xTb[bi], txld[bi], txTb[bi]
                    dma("pool", xl[:], xp[blk * 512:(blk + 1) * 512, :].rearrange("(j p) f -> p j f", p=128), [], [txl])
                    for j in range(4):
                        for kc in range(8):
                            tr(pX[:, kc * 128:(kc + 1) * 128], xl[:, j, kc * 128:(kc + 1) * 128], ident_b[:], [txl, tC], [tX])
                        cp("dve" if j % 2 == 0 else "act", xT[:, :, j * 128:(j + 1) * 128],
                           pX[:, :].rearrange("p (a b) -> p a b", a=8), [tX], [txT])
                    block_fm_gla(xT, txT, 512, mode == "main")
                    if mode != "pre":
                        hoff = (blk - 8) * 512
                        for h in range(4):
                            def ev_k(p, tf, h=h):
                                i = scnt[0] % 2
                                scnt[0] += 1
                                cp("dve", stg[i][:, :], p, [tf], [tstg[i]])
                                dma("sp", kscr[:, h, hoff:hoff + 512], stg[i][:, :], [tstg[i]], [tscrK])
                            fm_proj(2064 + h * 128, 128, xT, txT, 0, 512, ev_k)
                        if mode == "main":
                            moff = (blk - 12) * 512
                            for h in range(4):
                                def ev_q(p, tf, h=h):
                                    i = scnt[0] % 2
                                    scnt[0] += 1
                                    cp("act", stg[i][:, :], p, [tf], [tstg[i]])
                                    dma("sp", qscr[:, h, moff:moff + 512], stg[i][:, :], [tstg[i]], [tscrQ])
                                fm_proj(1552 + h * 128, 128, xT, txT, 0, 512, ev_q)
                    for j in range(4):
                        c0 = j * 128
                        full = gla_chunk(xT, txT, c0, "main" if mode == "main" else "pre")
                        if mode != "pre":
                            pf, tf = nextF()
                            tm_proj(pf[:, :], tf, 2576, 512, xT, txT, c0)
                            i = scnt[0] % 2
                            scnt[0] += 1
                            cp("act", stg[i][:, :], pf[:, :], [tf], [tstg[i]])
                            row0 = (blk - 8) * 512 + c0
                            dma("sp", vscr[row0:row0 + 128, :], stg[i][:, :], [tstg[i]], [tscrV])
                            if mode == "main":
                                orow = (blk - 12) * 512 + c0
                                i2 = scnt[1] % 2
                                scnt[1] += 1
                                cp("dve", stf[i2][:, :], pf[:, :], [tf], [tstf[i2]])
                                dma("sp", ndv_p[orow:orow + 128, :], stf[i2][:, :], [tstf[i2]], [])
                                pf, tf = nextF()
                                tm_proj(pf[:, :], tf, 2064, 512, xT, txT, c0)
                                i2 = scnt[1] % 2
                                scnt[1] += 1
                                cp("dve", stf[i2][:, :], pf[:, :], [tf], [tstf[i2]])
                                dma("sp", ndk_p[orow:orow + 128, :], stf[i2][:, :], [tstf[i2]], [])
                        if full:
                            tok0 = (blk - 12) * 512 + c0
                            for h in range(4):
                                pr, r0 = h // 2, 64 * (h % 2)
                                mm(pO3[:, h, :], v_bf[:, h * 128:(h + 1) * 128], A_bf[:, h, :], True, False, [tvbf, tAbf], [tO])
                                mm(pO3[:, h, :], S_bf[:, h, :], qt[:, pr, :], False, True, [tSbf, tqt], [tO])
                            gla_out(c0, tok0, tcatt[tok0 // 128])
                        state_update()
                dma("sp", sg_p.rearrange("(pr h2) k v -> (h2 k) pr v", h2=2), S32[:], [tS32], [])
            P.barrier()
        P.barrier()

        P.enabled = "B" in phases
        with contextlib.ExitStack() as esB:
            QT = SB(esB, "QT", [128, 4, 2048], BF16)
            KT = SB(esB, "KT", [128, 4, 4096], BF16)
            dmask = SB(esB, "dmask", [128, 256], F32)
            dmask_b = SB(esB, "dmask_b", [128, 256], BF16)
            nacc = SB(esB, "nacc", [128, 2048], F32)
            dacc = SB(esB, "dacc", [128, 2048], F32)
            tQ, tK, tdm, tnacc, tdacc = [Tok() for _ in range(5)]
            NV = 8
            Vt = [SB(esB, "Vt%d" % i, [128, 128], BF16) for i in range(NV)]
            tVt = [Tok() for _ in range(NV)]
            NP_ = 8
            PT = [SB(esB, "PT%d" % i, [128, 256], BF16) for i in range(NP_)]
            tPT = [Tok() for _ in range(NP_)]
            for h in range(4):
                dma("sp", QT[:, h, :], qscr[:, h, :], [tscrQ], [tQ])
                dma("sp", KT[:, h, :], kscr[:, h, :], [tscrK], [tK])
            dma("sp", dmask[:], ctab["dilmask"], [], [tdm])
            cp("dve", dmask_b[:], dmask[:], [tdm], [tdm])
            scale = 128.0 ** -0.5
            numb = [(pO, tO), (pA, tA)]
            denb = [(pZ, tZa), (pB, tBa)]
            cnt = [0, 0, 0]
            for h in range(4):
                first_branch = True
                for (d, nres, nb) in ((1, 1, 16), (4, 4, 4), (16, 16, 1)):
                    qlist = [(r, qb) for r in range(nres) for qb in range(nb)]
                    for g0 in range(0, len(qlist), 4):
                        grp = qlist[g0:g0 + 4]
                        pn, tn = numb[cnt[2] % 2]
                        pd, td = denb[cnt[2] % 2]
                        cnt[2] += 1
                        ktiles = []
                        for (r, qb) in grp:
                            for kt_ in (qb - 1, qb):
                                if (r, kt_) not in ktiles:
                                    ktiles.append((r, kt_))
                        slot_of = {rq: i for i, rq in enumerate(grp)}
                        opened = set()
                        pend = []
                        for (r, kt_) in ktiles:
                            qbs = [qb for qb in (kt_, kt_ + 1) if (r, qb) in slot_of]
                            q_lo = min(qbs)
                            ncol = 128 * len(qbs)
                            mcol0 = 128 * (q_lo - kt_)
                            kidx0 = 2048 + r + d * 128 * kt_
                            qidx0 = r + d * 128 * q_lo
                            pf, tf = nextF()
                            mm(pf[:, 0:ncol], KT[:, h, kidx0:kidx0 + d * 127 + 1:d], QT[:, h, qidx0:qidx0 + d * (ncol - 1) + 1:d],
                               True, False, [tK, tQ], [tf])
                            mm(pf[:, 0:ncol], ident_b[:], dmask_b[:, mcol0:mcol0 + ncol], False, True, [tC, tdm], [tf])
                            ip = cnt[0] % NP_
                            cnt[0] += 1
                            act(PT[ip][:, 0:ncol], pf[:, 0:ncol], AF.Exp, [tf, tC], [tPT[ip]],
                                bias=(hb[:, 0:1] if kt_ < 0 else cst[:, 1:2]), scale=scale)
                            iv = cnt[1] % NV
                            cnt[1] += 1
                            row0 = 2048 + r + d * 128 * kt_
                            dma("sp", Vt[iv][:, :], vscr[row0:row0 + d * 127 + 1:d, h * 128:(h + 1) * 128], [tscrV], [tVt[iv]])
                            pend.append((ip, iv, qbs, q_lo, r, kt_))
                        contrib = {}
                        for (ip, iv, qbs, q_lo, r, kt_) in pend:
                            for qb in qbs:
                                contrib.setdefault((r, qb), []).append((ip, iv, 128 * (qb - q_lo)))
                        for (r, qb), lst in contrib.items():
                            sl = slot_of[(r, qb)]
                            for n_, (ip, iv, c0) in enumerate(lst):
                                st, sp_ = n_ == 0, n_ == len(lst) - 1
                                mm(pn[:, sl * 128:(sl + 1) * 128], Vt[iv][:, :], PT[ip][:, c0:c0 + 128], st, sp_,
                                   [tVt[iv], tPT[ip]], [tn])
                                mm(pd[:, sl * 128:(sl + 1) * 128], ones_b[:], PT[ip][:, c0:c0 + 128], st, sp_,
                                   [tC, tPT[ip]], [td])
                        for sl, (r, qb) in enumerate(grp):
                            a0 = r + d * 128 * qb
                            dst_n = nacc[:, a0:a0 + d * 127 + 1:d]
                            dst_d = dacc[:, a0:a0 + d * 127 + 1:d]
                            if first_branch:
                                cp("dve", dst_n, pn[:, sl * 128:(sl + 1) * 128], [tn], [tnacc])
                                cp("act", dst_d, pd[:, sl * 128:(sl + 1) * 128], [td], [tdacc])
                            else:
                                tt(dst_n, dst_n, pn[:, sl * 128:(sl + 1) * 128], ALU.add, [tn, tnacc], [tnacc])
                                tt(dst_d, dst_d, pd[:, sl * 128:(sl + 1) * 128], ALU.add, [td, tdacc], [tdacc])
                    first_branch = False
                recip(dacc[:], dacc[:], [tdacc], [tdacc])
                for q4 in range(4):
                    tt(catT[:, 4 + h, q4 * 512:(q4 + 1) * 512], nacc[:, q4 * 512:(q4 + 1) * 512], dacc[:, q4 * 512:(q4 + 1) * 512],
                       ALU.mult, [tnacc, tdacc], [tcatt[q4 * 4 + i] for i in range(4)])
        P.barrier()

        P.enabled = "C" in phases
        with contextlib.ExitStack() as esC:
            wo_ = SB(esC, "wout", [128, 8, 1024], BF16)
            wq_ = SB(esC, "wq", [128, 8, 1024], BF16)
            wmo_ = SB(esC, "wmo", [128, 8, 1024], BF16)
            mKT = SB(esC, "mKT", [128, 8, 256], BF16)
            mV = SB(esC, "mV", [128, 2, 1024], BF16)
            lnt = SB(esC, "lnt", [128, 4, 1024], F32)
            wr_ = SB(esC, "wr", [128, 8, 36], F32)
            br_ = SB(esC, "br", [128, 36], F32)
            esM = contextlib.ExitStack()
            wtmp = SB(esM, "wtmp", [128, 8, 1024], BF16)
            tW, tWt, tln = Tok(), Tok(), Tok()
            tWo, tWq, tWmo = Tok(), Tok(), Tok()
            for i in range(4):
                dma("sp", lnt[:, i, :], lnp[i], [], [tln])
            dma("sp", wr_[:], w_r.rearrange("(kc p) n -> p kc n", p=128), [], [tln])
            dma("sp", br_[:], b_r, [], [tln])
            mem_bf = SB(esM, "mem_bf", [128, 2, 1024], BF16)
            memT = SB(esM, "memT", [128, 8, 256], BF16)
            mo32 = [SB(esM, "mo32_%d" % i, [128, 512], F32) for i in range(2)]
            tmem, tmemT, tmKT, tmV = Tok(), Tok(), Tok(), Tok()
            tmo32 = [Tok(), Tok()]
            dma("pool", mem_bf[:], memp.rearrange("(mt p) f -> p mt f", p=128), [], [tmem])
            for mt in range(2):
                for kc in range(8):
                    tr(pX[:, kc * 128:(kc + 1) * 128], mem_bf[:, mt, kc * 128:(kc + 1) * 128], ident_b[:], [tmem, tC], [tX])
                cp("dve", memT[:, :, mt * 128:(mt + 1) * 128], pX[:, :].rearrange("p (a b) -> p a b", a=8), [tX], [tmemT])
            mcnt = [0]
            for which, wsrc, dst in ((0, w_k, mk_p), (1, w_v, mv_p)):
                dma("pool", wtmp[:], wsrc.rearrange("(kc p) n -> p kc n", p=128), [], [tWt])
                if which == 0:
                    dma("pool", wo_[:], w_out.rearrange("(kc p) n -> p kc n", p=128), [], [tWo])
                    dma("pool", wq_[:], w_q.rearrange("(kc p) n -> p kc n", p=128), [], [tWq])
                    dma("pool", wmo_[:], w_o.rearrange("(kc p) n -> p kc n", p=128), [], [tWmo])
                for mt in range(2):
                    for half in range(2):
                        pf, tf = nextF()
                        for kc in range(8):
                            mm(pf[:, :], memT[:, kc, mt * 128:(mt + 1) * 128], wtmp[:, kc, half * 512:(half + 1) * 512],
                               kc == 0, kc == 7, [tmemT, tWt], [tf])
                        i = mcnt[0] % 2
                        mcnt[0] += 1
                        cp("dve", mo32[i][:, :], pf[:, :], [tf], [tmo32[i]])
                        dma("sp", dst[mt * 128:(mt + 1) * 128, half * 512:(half + 1) * 512], mo32[i][:, :], [tmo32[i]], [])
                        if which == 1:
                            cp("act", mV[:, mt, half * 512:(half + 1) * 512], pf[:, :], [tf], [tmV])
                if which == 0:
                    for ch in range(8):
                        pf, tf = nextF()
                        for kc in range(8):
                            mm(pf[:, 0:256], wtmp[:, kc, ch * 128:(ch + 1) * 128], memT[:, kc, :], kc == 0, kc == 7,
                               [tWt, tmemT], [tf])
                        cp("act", mKT[:, ch, :], pf[:, 0:256], [tf], [tmKT])

            P.barrier()
            esM.close()
            xin = [SB(esC, "xin%d" % i, [128, 1024], F32) for i in range(2)]
            txin = [Tok(), Tok()]
            SETS = []
            for si in range(2):
                d_ = {}
                for nm, shp, dt in (("u", [128, 1024], F32), ("x1", [128, 1024], F32), ("x1b", [128, 1024], BF16),
                                    ("x1T", [128, 8, 128], BF16), ("qT", [128, 8, 128], BF16), ("pTm", [128, 2, 128], BF16),
                                    ("omT", [128, 8, 128], BF16), ("rdn", [128, 128], F32), ("x2", [128, 1024], F32),
                                    ("x2b", [128, 1024], BF16), ("x2Tf", [128, 8, 128], F32), ("st", [128, 16], F32),
                                    ("lg", [128, 36], F32), ("rt", [128, 8, 32], F32)):
                    d_[nm] = SB(esC, "%s_%d" % (nm, si), shp, dt)
                    d_["t" + nm] = Tok()
                SETS.append(d_)
            cK = SB(esC, "cK", [128, 2, 1024], BF16)
            cV = SB(esC, "cV", [128, 2, 1024], BF16)
            cKT = SB(esC, "cKT", [128, 8, 256], BF16)
            pTs = SB(esC, "pTs", [128, 2, 4, 8], BF16)
            tcK, tcV, tcKT, tpTs = Tok(), Tok(), Tok(), Tok()

            def layer_norm(src, tsrc, gi, dst, tdst):
                red(st[:, 0:1], src[:, :], ALU.add, [tsrc], [tst])
                act(dst[:, :], src[:, :], AF.Square, [tsrc], [tdst])
                red(st[:, 1:2], dst[:, :], ALU.add, [tdst], [tst])
                ts(st[:, 2:3], st[:, 0:1], 1.0 / 1024, None, ALU.mult, None, [tst], [tst])
                tt(st[:, 3:4], st[:, 2:3], st[:, 2:3], ALU.mult, [tst], [tst])
                stt(st[:, 4:5], st[:, 1:2], 1.0 / 1024, st[:, 3:4], ALU.mult, ALU.subtract, [tst], [tst])
                ts(st[:, 5:6], st[:, 4:5], EPS, None, ALU.add, None, [tst], [tst])
                act(st[:, 5:6], st[:, 5:6], AF.Ln, [tst], [tst])
                act(st[:, 5:6], st[:, 5:6], AF.Exp, [tst], [tst], scale=-0.5)
                ts(dst[:, :], src[:, :], st[:, 2:3], st[:, 5:6], ALU.subtract, ALU.mult, [tsrc, tst], [tdst])
                tt(dst[:, :], dst[:, :], lnt[:, gi, :], ALU.mult, [tdst, tln], [tdst], eng="pool")
                tt(dst[:, :], dst[:, :], lnt[:, gi + 1, :], ALU.add, [tdst, tln], [tdst], eng="pool")

            pM = [pO, pA]
            tMM = [tO, tA]
            pO8 = [pZ, pB]
            tO8 = [tZa, tBa]
            for ti in range(NT):
                d_ = SETS[ti % 2]
                u, x1, x1b, x1T, qT, pTm, omT, rdn, x2, x2b, x2Tf, st, lg, rt = [d_[k] for k in (
                    "u", "x1", "x1b", "x1T", "qT", "pTm", "omT", "rdn", "x2", "x2b", "x2Tf", "st", "lg", "rt")]
                tu, tx1, tx1b, tx1T, tqT, tpTm, tomT, trdn, tx2, tx2b, tx2Tf, tst, tlg, trt = [d_["t" + k] for k in (
                    "u", "x1", "x1b", "x1T", "qT", "pTm", "omT", "rdn", "x2", "x2b", "x2Tf", "st", "lg", "rt")]
                xi, txi = xin[ti % 2], txin[ti % 2]
                dma("sp", xi[:], xres[ti * 128:(ti + 1) * 128, :], [], [txi])
                for half in range(2):
                    for kc in range(8):
                        mm(pM[half][:, :], catT[:, kc, ti * 128:(ti + 1) * 128], wo_[:, kc, half * 512:(half + 1) * 512],
                           kc == 0, kc == 7, [tcatt[ti], tWo], [tMM[half]])
                    stt(u[:, half * 512:(half + 1) * 512], xi[:, half * 512:(half + 1) * 512], ALPHA, pM[half][:, :],
                        ALU.mult, ALU.add, [txi, tMM[half]], [tu])
                layer_norm(u, tu, 0, x1, tx1)
                cp("act", x1b[:], x1[:], [tx1], [tx1b])
                for kc in range(8):
                    tr(pX[:, kc * 128:(kc + 1) * 128], x1b[:, kc * 128:(kc + 1) * 128], ident_b[:], [tx1b, tC], [tX])
                cp("dve", x1T[:].rearrange("p a b -> p (a b)"), pX[:, :], [tX], [tx1T])
                for ch in range(8):
                    pf, tf = nextF()
                    for kc in range(8):
                        mm(pf[:, 0:128], wq_[:, kc, ch * 128:(ch + 1) * 128], x1T[:, kc, :], kc == 0, kc == 7, [tWq, tx1T], [tf])
                    cp("act" if ch % 2 else "dve", qT[:, ch, :], pf[:, 0:128], [tf], [tqT])
                if ti < 16:
                    for h in range(4):
                        pf, tf = nextF()
                        pf3 = pf[:, 0:256].rearrange("p (a b) -> p a b", a=2)
                        for mt in range(2):
                            for cc in range(2):
                                mm(pf3[:, mt, :], mKT[:, h * 2 + cc, mt * 128:(mt + 1) * 128], qT[:, h * 2 + cc, :],
                                   cc == 0, cc == 1, [tmKT, tqT], [tf])
                        act(pTm[:], pf3, AF.Exp, [tf], [tpTm], scale=1.0 / 16)
                        for cc in range(2):
                            ch = h * 2 + cc
                            bank, tb = pO8[ch // 4], tO8[ch // 4]
                            for mt in range(2):
                                mm(bank[:, (ch % 4) * 128:(ch % 4 + 1) * 128], mV[:, mt, ch * 128:(ch + 1) * 128], pTm[:, mt, :],
                                   mt == 0, mt == 1, [tmV, tpTm], [tb])
                        pf2, tf2 = nextF()
                        for mt in range(2):
                            mm(pf2[:, 0:128], ones_b[:], pTm[:, mt, :], mt == 0, mt == 1, [tC, tpTm], [tf2])
                        recip(rdn[:], pf2[:, 0:128], [tf2], [trdn])
                        for cc in range(2):
                            ch = h * 2 + cc
                            bank, tb = pO8[ch // 4], tO8[ch // 4]
                            tt(omT[:, ch, :], bank[:, (ch % 4) * 128:(ch % 4 + 1) * 128], rdn[:], ALU.mult, [tb, trdn], [tomT])
                else:
                    pf_d, tf_d = pV, tV
                    pfd4 = pf_d[:, :].rearrange("p (b h t) -> p h b t", b=16, h=4)
                    for b in range(16):
                        dma("pool", cK[:], cmk[b].rearrange("(mt p) f -> p mt f", p=128), [], [tcK])
                        dma("pool", cV[:], cmv[b].rearrange("(mt p) f -> p mt f", p=128), [], [tcV])
                        for mt in range(2):
                            for kc in range(8):
                                tr(pX[:, kc * 128:(kc + 1) * 128], cK[:, mt, kc * 128:(kc + 1) * 128], ident_b[:], [tcK, tC], [tX])
                            cp("dve" if mt == 0 else "act", cKT[:, :, mt * 128:(mt + 1) * 128],
                               pX[:, :].rearrange("p (a b) -> p a b", a=8), [tX], [tcKT])
                        pf, tf = nextF()
                        pf4 = pf[:, 0:64].rearrange("p (m h t) -> p m h t", m=2, h=4)
                        for h in range(4):
                            for mt in range(2):
                                for cc in range(2):
                                    mm(pf4[:, mt, h, :], cKT[:, h * 2 + cc, mt * 128:(mt + 1) * 128], qT[:, h * 2 + cc, 8 * b:8 * b + 8],
                                       cc == 0, cc == 1, [tcKT, tqT], [tf])
                        act(pTs[:].rearrange("p m h t -> p (m h t)"), pf[:, 0:64], AF.Exp, [tf], [tpTs], scale=1.0 / 16)
                        for ch in range(8):
                            h = ch // 2
                            bank, tb = pO8[ch // 4], tO8[ch // 4]
                            for mt in range(2):
                                mm(bank[:, (ch % 4) * 128 + 8 * b:(ch % 4) * 128 + 8 * b + 8], cV[:, mt, ch * 128:(ch + 1) * 128],
                                   pTs[:, mt, h, :], mt == 0, mt == 1, [tcV, tpTs], [tb])
                        for mt in range(2):
                            mm(pf_d[:, b * 32:(b + 1) * 32], ones_b[:], pTs[:, mt].rearrange("p h t -> p (h t)"), mt == 0, mt == 1,
                               [tC, tpTs], [tf_d])
                    for h in range(4):
                        recip(rdn[:].rearrange("p (b t) -> p b t", b=16), pfd4[:, h], [tf_d], [trdn])
                        for cc in range(2):
                            ch = h * 2 + cc
                            bank, tb = pO8[ch // 4], tO8[ch // 4]
                            tt(omT[:, ch, :], bank[:, (ch % 4) * 128:(ch % 4 + 1) * 128], rdn[:], ALU.mult, [tb, trdn], [tomT])
                for half in range(2):
                    for kc in range(8):
                        mm(pM[half][:, :], omT[:, kc, :], wmo_[:, kc, half * 512:(half + 1) * 512], kc == 0, kc == 7,
                           [tomT, tWmo], [tMM[half]])
                    stt(u[:, half * 512:(half + 1) * 512], x1[:, half * 512:(half + 1) * 512], ALPHA, pM[half][:, :],
                        ALU.mult, ALU.add, [tx1, tMM[half]], [tu])
                layer_norm(u, tu, 2, x2, tx2)
                cp("act", x2b[:], x2[:], [tx2], [tx2b])
                for kc in range(8):
                    tr(pX[:, kc * 128:(kc + 1) * 128], x2b[:, kc * 128:(kc + 1) * 128], ident_b[:], [tx2b, tC], [tX])
                cp("dve", x2T[:, :, ti * 128:(ti + 1) * 128], pX[:, :].rearrange("p (a b) -> p a b", a=8), [tX], [tx2t[ti]])
                for half in range(2):
                    for k4 in range(4):
                        kc = half * 4 + k4
                        tr(pM[half][:, k4 * 128:(k4 + 1) * 128], x2[:, kc * 128:(kc + 1) * 128], ident_f[:], [tx2, tC], [tMM[half]])
                    cp("act", x2Tf[:, half * 4:(half + 1) * 4, :].rearrange("p a b -> p (a b)"), pM[half][:, :], [tMM[half]], [tx2Tf])
                pf, tf = nextF()
                for kc in range(8):
                    mm(pf[:, 0:36], x2Tf[:, kc, :], wr_[:, kc, :], kc == 0, kc == 7, [tx2Tf, tln], [tf])
                tt(lg[:], pf[:, 0:36], br_[:], ALU.add, [tf, tln], [tlg])
                R = [tlg, trt]
                red(rt[:, 0, 0:1], lg[:, 0:4], ALU.max, R, [trt])
                ts(rt[:, 0, 1:2], rt[:, 0, 0:1], -1.0, None, ALU.mult, None, R, [trt])
                act(rt[:, 1, 0:4], lg[:, 0:4], AF.Exp, R, [trt], bias=rt[:, 0, 1:2])
                red(rt[:, 0, 2:3], rt[:, 1, 0:4], ALU.add, R, [trt])
                recip(rt[:, 0, 3:4], rt[:, 0, 2:3], R, [trt])
                ts(rt[:, 1, 4:8], lg[:, 0:4], rt[:, 0, 0:1], None, ALU.is_equal, None, R, [trt])
                ts(rt[:, 1, 8:12], rt[:, 1, 4:8], -1.0, 30000.0, ALU.add, ALU.mult, R, [trt])
                for g in range(4):
                    ts(rt[:, 2, g * 8:(g + 1) * 8], lg[:, 4 + g * 8:4 + (g + 1) * 8], rt[:, 1, 8 + g:9 + g], None,
                       ALU.add, None, R, [trt])
                red(rt[:, 0, 4:5], rt[:, 2, :], ALU.max, R, [trt])
                ts(rt[:, 3, :], rt[:, 2, :], rt[:, 0, 4:5], None, ALU.is_equal, None, R, [trt])
                stt(rt[:, 4, :], rt[:, 3, :], -60000.0, rt[:, 2, :], ALU.mult, ALU.add, R, [trt])
                red(rt[:, 0, 5:6], rt[:, 4, :], ALU.max, R, [trt])
                ts(rt[:, 5, :], rt[:, 4, :], rt[:, 0, 5:6], None, ALU.is_equal, None, R, [trt])
                tt(rt[:, 0, 6:7], rt[:, 0, 5:6], rt[:, 0, 4:5], ALU.subtract, R, [trt])
                act(rt[:, 0, 7:8], rt[:, 0, 6:7], AF.Exp, R, [trt])
                ts(rt[:, 0, 8:9], rt[:, 0, 7:8], 1.0, None, ALU.add, None, R, [trt])
                recip(rt[:, 0, 9:10], rt[:, 0, 8:9], R, [trt])
                tt(rt[:, 0, 10:11], rt[:, 0, 9:10], rt[:, 0, 7:8], ALU.mult, R, [trt])
                tt(rt[:, 0, 11:12], rt[:, 0, 9:10], rt[:, 0, 3:4], ALU.mult, R, [trt])
                tt(rt[:, 0, 12:13], rt[:, 0, 10:11], rt[:, 0, 3:4], ALU.mult, R, [trt])
                ts(rt[:, 6, :], rt[:, 3, :], rt[:, 0, 11:12], None, ALU.mult, None, R, [trt])
                stt(gate[:, ti, :], rt[:, 5, :], rt[:, 0, 12:13], rt[:, 6, :], ALU.mult, ALU.add, R, [tgate])
                ts(x2[:, :], x2[:, :], ALPHA, None, ALU.mult, None, [tx2], [tx2], eng="pool")
                dma("sp", x2scr[ti * 128:(ti + 1) * 128, :], x2[:, :], [tx2], [tgate])
        P.barrier()

        P.enabled = "D" in phases
        with contextlib.ExitStack() as esD:
            yacc = SB(esD, "yacc", [128, NT, 1024], F32)
            lnf = SB(esD, "lnf", [128, 2, 1024], F32)
            tyt = [Tok() for _ in range(NT)]
            tlnf = Tok()
            for ti in range(NT):
                dma("sp", yacc[:, ti, :], x2scr[ti * 128:(ti + 1) * 128, :], [tgate], [tyt[ti]])
            dma("sp", lnf[:, 0, :], lnp[4], [], [tlnf])
            dma("sp", lnf[:, 1, :], lnp[5], [], [tlnf])
            esE = contextlib.ExitStack()
            Wg = [SB(esE, "Wg%d" % i, [128, 8, 512], BF16) for i in range(2)]
            Wu = [SB(esE, "Wu%d" % i, [128, 8, 512], BF16) for i in range(2)]
            Wd = [SB(esE, "Wd%d" % i, [128, 4, 1024], BF16) for i in range(2)]
            tWg, tWu, tWd = [Tok(), Tok()], [Tok(), Tok()], [Tok(), Tok()]
            sg_ = [SB(esE, "sg%d" % i, [128, 512], F32) for i in range(2)]
            tsg = [Tok(), Tok()]
            hT = [SB(esE, "hT%d" % i, [128, 4, 512], BF16) for i in range(2)]
            thT = [Tok(), Tok()]
            ytmp = [SB(esE, "ytmp%d" % i, [128, 512], F32) for i in range(2)]
            tytmp = [Tok(), Tok()]
            pG = [(pF[0], tF0), (pF[1], tF1)]
            pU = [(pZ, tZa), (pB, tBa)]
            pY = [(pO, tO), (pA, tA), (pV, tV)]
            blocks = [(0, 512), (512, 512), (1024, 512), (1536, 512), (2048, 128)]
            cg, cy, ch_ = 0, 0, 0
            for e in range(32):
                wb = e % 2
                dma("pool", Wg[wb][:], w_eg[e].rearrange("(kc p) n -> p kc n", p=128), [], [tWg[wb]])
                dma("pool", Wu[wb][:], w_eu[e].rearrange("(kc p) n -> p kc n", p=128), [], [tWu[wb]])
                dma("pool", Wd[wb][:], w_ed[e].rearrange("(kc p) n -> p kc n", p=128), [], [tWd[wb]])
                for (t0, nt) in blocks:
                    hb_i = ch_ % 2
                    ch_ += 1
                    tiles = list(range(t0 // 128, (t0 + nt) // 128))
                    for hc in range(4):
                        (pg, tg), (pu, tu_) = pG[cg % 2], pU[cg % 2]
                        sgi = cg % 2
                        cg += 1
                        for kc in range(8):
                            mm(pg[:, 0:nt], Wg[wb][:, kc, hc * 128:(hc + 1) * 128], x2T[:, kc, t0:t0 + nt], kc == 0, kc == 7,
                               [tWg[wb]] + [tx2t[i] for i in tiles], [tg])
                        for kc in range(8):
                            mm(pu[:, 0:nt], Wu[wb][:, kc, hc * 128:(hc + 1) * 128], x2T[:, kc, t0:t0 + nt], kc == 0, kc == 7,
                               [tWu[wb]] + [tx2t[i] for i in tiles], [tu_])
                        act(sg_[sgi][:, 0:nt], pg[:, 0:nt], AF.Silu, [tg], [tsg[sgi]])
                        tt(hT[hb_i][:, hc, 0:nt], sg_[sgi][:, 0:nt], pu[:, 0:nt], ALU.mult, [tsg[sgi], tu_], [thT[hb_i]])
                    for ti in tiles:
                        o0 = ti * 128 - t0
                        for half in range(2):
                            py, ty = pY[cy % 3]
                            cy += 1
                            for hc in range(4):
                                mm(py[:, :], hT[hb_i][:, hc, o0:o0 + 128], Wd[wb][:, hc, half * 512:(half + 1) * 512], hc == 0, hc == 3,
                                   [thT[hb_i], tWd[wb]], [ty])
                            ya = yacc[:, ti, half * 512:(half + 1) * 512]
                            if half == 0:
                                stt(ya, py[:, :], gate[:, ti, e:e + 1], ya, ALU.mult, ALU.add, [ty, tgate, tyt[ti]], [tyt[ti]])
                            else:
                                iy = ti % 2
                                act(ytmp[iy][:, :], py[:, :], AF.Identity, [ty, tgate], [tytmp[iy]], scale=gate[:, ti, e:e + 1])
                                tt(ya, ya, ytmp[iy][:, :], ALU.add, [tytmp[iy], tyt[ti]], [tyt[ti]], eng="pool")
            P.barrier()
            esE.close()
            st2 = SB(esD, "st2", [128, 16], F32)
            junk2 = SB(esD, "junk2", [128, 1024], F32)
            yo = [SB(esD, "yo%d" % i, [128, 1024], F32) for i in range(2)]
            tst2, tj2 = Tok(), Tok()
            tyo = [Tok(), Tok()]
            for ti in range(NT):
                src = yacc[:, ti, :]
                dst, tdst = yo[ti % 2], tyo[ti % 2]
                red(st2[:, 0:1], src, ALU.add, [tyt[ti]], [tst2])
                act(junk2[:], src, AF.Square, [tyt[ti]], [tj2])
                red(st2[:, 1:2], junk2[:, :], ALU.add, [tj2], [tst2])
                ts(st2[:, 2:3], st2[:, 0:1], 1.0 / 1024, None, ALU.mult, None, [tst2], [tst2])
                tt(st2[:, 3:4], st2[:, 2:3], st2[:, 2:3], ALU.mult, [tst2], [tst2])
                stt(st2[:, 4:5], st2[:, 1:2], 1.0 / 1024, st2[:, 3:4], ALU.mult, ALU.subtract, [tst2], [tst2])
                ts(st2[:, 5:6], st2[:, 4:5], EPS, None, ALU.add, None, [tst2], [tst2])
                act(st2[:, 5:6], st2[:, 5:6], AF.Ln, [tst2], [tst2])
                act(st2[:, 5:6], st2[:, 5:6], AF.Exp, [tst2], [tst2], scale=-0.5)
                ts(dst[:, :], src, st2[:, 2:3], st2[:, 5:6], ALU.subtract, ALU.mult, [tyt[ti], tst2], [tdst])
                tt(dst[:, :], dst[:, :], lnf[:, 0, :], ALU.mult, [tdst, tlnf], [tdst], eng="pool")
                tt(dst[:, :], dst[:, :], lnf[:, 1, :], ALU.add, [tdst, tlnf], [tdst], eng="pool")
                dma("sp", y_out[ti * 128:(ti + 1) * 128, :], dst[:, :], [tdst], [])
        P.enabled = True
        P.emit()
        nc._kstats = P.stats
        nc._nops = len(P.ops)
        nc._marks = getattr(P, 'marks', [])
        nc._oplist = [(o.eng, o.is_dma, P.lines.get(o.idx)) for o in P.ops]
        nc._ext_in, nc._ext_out = ext_in, ext_out
    return nc


_NC_CACHE = {}


def _make_in_maps(x_prompt, x_sample, mem_prompt, cache_dil_k, cache_dil_v, state_gla, cache_mem_k, cache_mem_v,
           w_in, w_gate_lr, b_gate, g_gla_norm, w_out, ln_mix_g, ln_mix_b,
           w_mem_q, w_mem_k, w_mem_v, w_mem_o, ln_mem_g, ln_mem_b,
           w_route_group, b_route_group, w_route_expert, b_route_expert,
           w_exp_gate, w_exp_up, w_exp_down, ln_ffn_g, ln_ffn_b):
    f = lambda a: np.ascontiguousarray(np.asarray(a, dtype=np.float32))
    x_prompt, x_sample = f(x_prompt), f(x_sample)
    consts = _const_tables()
    rep = lambda v: np.ascontiguousarray(np.broadcast_to(np.asarray(v, np.float32).reshape(1, -1), (128, np.asarray(v).size)))
    shared = {
        "w_in": f(w_in[0]),
        "wlr": np.ascontiguousarray(np.concatenate([f(w_gate_lr[0]), f(b_gate[0]).reshape(1, 256)], axis=0)),
        "g4": np.ascontiguousarray(f(g_gla_norm[0]).reshape(4, 128).T),
        "w_out": f(w_out[0]), "w_q": f(w_mem_q[0]), "w_k": f(w_mem_k[0]), "w_v": f(w_mem_v[0]), "w_o": f(w_mem_o[0]),
        "lnp": np.ascontiguousarray(np.stack([rep(ln_mix_g[0]), rep(ln_mix_b[0]), rep(ln_mem_g[0]), rep(ln_mem_b[0]),
                                              rep(ln_ffn_g[0]), rep(ln_ffn_b[0])], axis=0)),
        "w_r": np.ascontiguousarray(np.concatenate([f(w_route_group[0]), f(w_route_expert[0])], axis=1)),
        "b_r": rep(np.concatenate([f(b_route_group[0]), f(b_route_expert[0])], axis=0)),
        "w_eg": f(w_exp_gate[0]), "w_eu": f(w_exp_up[0]), "w_ed": f(w_exp_down[0]),
    }
    for k, v in consts.items():
        shared["c_" + k] = v
    in_maps = []
    for c in range(8):
        b, s = c // 4, c % 4
        xp = np.zeros((8192, 1024), np.float32)
        lo = (s - 3) * 2048
        src_lo = max(lo, 0)
        xp[src_lo - lo:] = x_prompt[b, src_lo:(s + 1) * 2048]
        xs = x_sample[16 * c:16 * c + 16].reshape(128, 1024)
        m = dict(shared)
        m.update({
            "xp": xp,
            "halo_bias": np.full((128, 1), 0.0 if s > 0 else NEG, np.float32),
            "xs": np.ascontiguousarray(xs),
            "xres": np.ascontiguousarray(np.concatenate([x_prompt[b, s * 2048:(s + 1) * 2048], xs], axis=0)),
            "memp": f(mem_prompt[b]),
            "cdk": f(cache_dil_k[0, 16 * c:16 * c + 16]).reshape(16, 2048, 512),
            "cdv": f(cache_dil_v[0, 16 * c:16 * c + 16]).reshape(16, 2048, 512),
            "sgla": f(state_gla[0, 16 * c:16 * c + 16]),
            "cmk": f(cache_mem_k[0, 16 * c:16 * c + 16]).reshape(16, 256, 1024),
            "cmv": f(cache_mem_v[0, 16 * c:16 * c + 16]).reshape(16, 256, 1024),
        })
        in_maps.append(m)
    return in_maps


def kernel(**inputs):
    if "nc" not in _NC_CACHE:
        _NC_CACHE["nc"] = build_nc()
    nc = _NC_CACHE["nc"]
    in_maps = _make_in_maps(**inputs)
    res = run_bass_kernel_spmd(nc, in_maps, core_ids=list(range(8)))
    return _assemble(res.results)


def _assemble(R):
    y_prompt = np.stack([np.concatenate([R[4 * b + s]["y"][:2048] for s in range(4)], axis=0) for b in range(2)], axis=0)
    y_sample = np.concatenate([R[c]["y"][2048:].reshape(16, 8, 1024) for c in range(8)], axis=0)
    ndk_p = np.stack([R[4 * b + 3]["ndk_p"].reshape(2048, 4, 128) for b in range(2)], axis=0)[None]
    ndv_p = np.stack([R[4 * b + 3]["ndv_p"].reshape(2048, 4, 128) for b in range(2)], axis=0)[None]
    sg_p = np.stack([R[4 * b + 3]["sg_p"] for b in range(2)], axis=0)[None]
    mk_p = np.stack([R[4 * b]["mk_p"].reshape(256, 4, 256) for b in range(2)], axis=0)[None]
    mv_p = np.stack([R[4 * b]["mv_p"].reshape(256, 4, 256) for b in range(2)], axis=0)[None]
    ndk_s = np.concatenate([R[c]["ndk_s"].reshape(16, 2048, 4, 128) for c in range(8)], axis=0)[None]
    ndv_s = np.concatenate([R[c]["ndv_s"].reshape(16, 2048, 4, 128) for c in range(8)], axis=0)[None]
    sg_s = np.concatenate([R[c]["sg_s"] for c in range(8)], axis=0)[None]
    outs = (y_prompt, y_sample, ndk_p, ndv_p, sg_p, mk_p, mv_p, ndk_s, ndv_s, sg_s)
    return tuple(np.ascontiguousarray(o.astype(np.float32)) for o in outs)
```

```python
import contextlib
import numpy as np
import concourse.bass as bass
import concourse.mybir as mybir
from concourse.bass_utils import run_bass_kernel_spmd

F32 = mybir.dt.float32
BF16 = mybir.dt.bfloat16
AF = mybir.ActivationFunctionType
ALU = mybir.AluOpType
AX = mybir.AxisListType

NEG = -30000.0
ALPHA = 2.0 ** 0.25
EPS = 1e-5
NTOK = 2176
NT = 17


class Tok:
    __slots__ = ("writer", "readers", "psum")

    def __init__(self, psum=False):
        self.writer = None
        self.readers = []
        self.psum = psum


class Op:
    __slots__ = ("eng", "fn", "deps", "is_dma", "signal", "sigval", "slot", "idx", "is_barrier")

    def __init__(self, eng, fn, is_dma):
        self.eng = eng
        self.fn = fn
        self.deps = []
        self.is_dma = is_dma
        self.signal = False
        self.sigval = 0
        self.slot = None
        self.idx = -1
        self.is_barrier = False


class Prog:
    NSLOT = 8
    CE = ("pe", "act", "dve", "pool")

    def __init__(self, nc):
        self.nc = nc
        self.ops = []
        self.engs = {"pe": nc.tensor, "act": nc.scalar, "dve": nc.vector, "pool": nc.gpsimd, "sp": nc.sync}
        self.last = {}
        self.dmas = []
        self.enabled = True

    def _add(self, eng, fn, reads, writes, is_dma):
        if not self.enabled:
            return None
        op = Op(eng, fn, is_dma)
        op.idx = len(self.ops)
        import sys as _sys
        fr = _sys._getframe(2)
        lines = []
        while fr is not None and len(lines) < 4:
            lines.append(fr.f_lineno)
            fr = fr.f_back
        self.lines = getattr(self, "lines", {})
        self.lines[op.idx] = lines
        deps = {}
        for t in reads:
            if t.writer is not None:
                deps[t.writer.idx] = t.writer
            if t.psum:
                for r in t.readers:
                    if r.eng != eng:
                        deps[r.idx] = r
        for t in writes:
            if t.writer is not None:
                deps[t.writer.idx] = t.writer
            for r in t.readers:
                deps[r.idx] = r
        for t in reads:
            t.readers.append(op)
        for t in writes:
            t.writer = op
            t.readers = []
        for d in deps.values():
            if (not d.is_dma) and (not is_dma) and d.eng == "pe" and eng == "pe":
                continue
            op.deps.append(d)
            d.signal = True
        self.ops.append(op)
        if is_dma:
            self.dmas.append(op)
        else:
            self.last[eng] = op
        return op

    def op(self, eng, fn, reads=(), writes=()):
        return self._add(eng, fn, list(reads), list(writes), False)

    def dma(self, queue, fn, reads=(), writes=()):
        o = self._add(queue, fn, list(reads), list(writes), True)
        if o is not None:
            o.signal = True
        return o

    def barrier(self):
        if not self.enabled:
            return
        prev = list(self.last.values()) + list(self.dmas)
        self.dmas = []
        for e in ("pe", "act", "dve", "pool", "sp"):
            op = Op(e, lambda eng: eng.nop(), False)
            op.is_barrier = True
            op.idx = len(self.ops)
            for d in prev:
                op.deps.append(d)
                d.signal = True
            self.ops.append(op)
            if e != "sp":
                self.last[e] = op

    def emit(self):
        nc = self.nc
        with contextlib.ExitStack() as es:
            esem = {e: es.enter_context(nc.semaphore("s_" + e)) for e in self.CE}
            dsem = {q: [es.enter_context(nc.semaphore("d_%s%d" % (q, i))) for i in range(self.NSLOT)]
                    for q in ("sp", "pool", "act")}
            ecount = {e: 0 for e in esem}
            dcount = {q: [0] * self.NSLOT for q in dsem}
            dnext = {q: 0 for q in dsem}
            waited = {}

            def wait(engname, sem, val):
                key = (engname, id(sem))
                if waited.get(key, 0) >= val:
                    return
                waited[key] = val
                self.engs[engname].wait_ge(sem, val)

            import os as _os
            _kstop = int(_os.environ.get("KSTOP", "0")) or len(self.ops)
            for op in self.ops[:_kstop]:
                e = op.eng
                for d in op.deps:
                    if d.is_dma:
                        wait(e, dsem[d.eng][d.slot], d.sigval)
                    else:
                        wait(e, esem[d.eng], d.sigval)
                if op.is_dma:
                    s = dnext[e]
                    dnext[e] = (s + 1) % self.NSLOT
                    if dcount[e][s] > 0:
                        wait(e, dsem[e][s], dcount[e][s])
                    ins = op.fn(self.engs[e])
                    dcount[e][s] += 16
                    op.slot = s
                    op.sigval = dcount[e][s]
                    ins.then_inc(dsem[e][s], 16)
                else:
                    if getattr(op, "is_barrier", False) and e == "pe":
                        self.marks = getattr(self, "marks", []) + [ecount["pe"]]
                    ins = op.fn(self.engs[e])
                    if op.signal and e in esem:
                        ecount[e] += 1
                        op.sigval = ecount[e]
                        ins.then_inc(esem[e], 1)
            self.stats = (dict(ecount), {q: list(v) for q, v in dcount.items()})
            for q in dsem:
                for s in range(self.NSLOT):
                    if dcount[q][s] > 0:
                        wait("sp", dsem[q][s], dcount[q][s])
            for e in esem:
                if ecount[e] > 0:
                    wait("sp", esem[e], ecount[e])


def _const_tables():
    c = {}
    j = np.arange(128)[:, None]
    i = np.arange(128)[None, :]
    c["uneg"] = np.where(j <= i, -1.0 / 16, 0.0).astype(np.float32)
    c["mgtneg"] = np.where(j > i, -1.0 / 16, 0.0).astype(np.float32)
    am = (j <= i).astype(np.float32)
    c["amask"] = np.ascontiguousarray(np.broadcast_to(am[:, None, :], (128, 4, 128)))
    sameb = (j // 8) == (i // 8)
    c["uneg_s"] = np.where(sameb & (j <= i), -1.0 / 16, 0.0).astype(np.float32)
    c["mgtneg_s"] = np.where(sameb & (j > i), -1.0 / 16, 0.0).astype(np.float32)
    ams = (sameb & (j <= i)).astype(np.float32)
    c["amask_s"] = np.ascontiguousarray(np.broadcast_to(ams[:, None, :], (128, 4, 128)))
    c["rowmask"] = ((np.arange(128)[:, None] // 8) == np.arange(16)[None, :]).astype(np.float32)
    qrel = np.arange(256)[None, :]
    kk = np.arange(128)[:, None]
    c["dilmask"] = np.where((qrel - kk >= 0) & (qrel - kk <= 128), 0.0, NEG).astype(np.float32)
    part = np.arange(128)[:, None, None]
    jj = np.arange(16)[None, :, None]
    t = np.arange(8)[None, None, :]
    delta = 2048 + t - (16 * part + jj)

    def mult(d):
        return ((d >= 0) & (d <= 128)).astype(np.float32) + ((d >= 0) & (d % 4 == 0) & (d <= 512)) + \
               ((d >= 0) & (d % 16 == 0) & (d <= 2048))
    sm = mult(delta).astype(np.float32)
    c["smult"] = np.ascontiguousarray(np.broadcast_to(sm[:, None, :, :], (128, 4, 16, 8))).reshape(128, 512)
    dn = (i % 8) - (j % 8)
    c["snew"] = np.where(sameb, mult(dn), 0.0).astype(np.float32)
    c["ident"] = np.eye(128, dtype=np.float32)
    return c


def build_nc(phases="STUPBCD"):
    nc = bass.Bass("TRN2", target_bir_lowering=False)

    ext_in, ext_out = [], []

    def din(name, shape, ph="*", dt=F32):
        on = ph == "*" or any(c in phases for c in ph)
        if on:
            ext_in.append(name)
        return nc.dram_tensor(name, list(shape), dt, kind="ExternalInput" if on else "Internal").ap()

    def dout(name, shape, ph="*"):
        on = ph == "*" or any(c in phases for c in ph)
        if on:
            ext_out.append(name)
        return nc.dram_tensor(name, list(shape), F32, kind="ExternalOutput" if on else "Internal").ap()

    def dscr(name, shape, dt):
        return nc.dram_tensor(name, list(shape), dt, kind="Internal").ap()

    xp = din("xp", [8192, 1024], "P")
    halo_bias = din("halo_bias", [128, 1])
    xs_in = din("xs", [128, 1024], "S")
    xres = din("xres", [NTOK, 1024], "C")
    memp = din("memp", [256, 1024], "C")
    cdk = din("cdk", [16, 2048, 512], "U")
    cdv = din("cdv", [16, 2048, 512], "U")
    sgla = din("sgla", [16, 4, 64, 128], "ST")
    cmk = din("cmk", [16, 256, 1024], "C")
    cmv = din("cmv", [16, 256, 1024], "C")
    w_in = din("w_in", [1024, 3088])
    wlr_in = din("wlr", [17, 256])
    g4_in = din("g4", [128, 4])
    w_out = din("w_out", [1024, 1024], "C")
    w_q = din("w_q", [1024, 1024], "C")
    w_k = din("w_k", [1024, 1024], "C")
    w_v = din("w_v", [1024, 1024], "C")
    w_o = din("w_o", [1024, 1024], "C")
    lnp = din("lnp", [6, 128, 1024], "CD")
    w_r = din("w_r", [1024, 36], "C")
    b_r = din("b_r", [128, 36], "C")
    w_eg = din("w_eg", [32, 1024, 512], "D")
    w_eu = din("w_eu", [32, 1024, 512], "D")
    w_ed = din("w_ed", [32, 512, 1024], "D")
    ctab = {k: din("c_" + k, list(v.shape)) for k, v in _const_tables().items()}
    y_out = dout("y", [NTOK, 1024], "D")
    ndk_p = dout("ndk_p", [2048, 512], "P")
    ndv_p = dout("ndv_p", [2048, 512], "P")
    sg_p = dout("sg_p", [4, 64, 128], "P")
    mk_p = dout("mk_p", [256, 1024], "C")
    mv_p = dout("mv_p", [256, 1024], "C")
    ndk_s = dout("ndk_s", [16, 2048, 512], "SU")
    ndv_s = dout("ndv_s", [16, 2048, 512], "SU")
    sg_s = dout("sg_s", [16, 4, 64, 128], "T")
    dbg = dout("dbg", [128, 2048], "Z")
    vscr = dscr("vscr", [4096, 512], BF16)
    qscr = dscr("qscr", [128, 4, 2048], BF16)
    kscr = dscr("kscr", [128, 4, 4096], BF16)
    x2scr = dscr("x2scr", [NTOK, 1024], F32)

    P = Prog(nc)

    def mm(out, lhsT, rhs, start, stop, reads, writes, skip=False):
        if skip:
            P.op("pe", lambda e: e.matmul(out, lhsT, rhs, start=start, stop=stop, skip_group_check=True), reads, writes)
        else:
            P.op("pe", lambda e: e.matmul(out, lhsT, rhs, start=start, stop=stop), reads, writes)

    def tr(out, in_, ident, reads, writes):
        P.op("pe", lambda e: e.transpose(out, in_, ident), reads, writes)

    def act(out, in_, func, reads, writes, bias=None, scale=1.0):
        if bias is None:
            P.op("act", lambda e: e.activation(out=out, in_=in_, func=func, scale=scale), reads, writes)
        else:
            P.op("act", lambda e: e.activation(out=out, in_=in_, func=func, bias=bias, scale=scale), reads, writes)

    def cp(eng, out, in_, reads, writes):
        if eng == "act":
            P.op("act", lambda e: e.copy(out, in_), reads, writes)
        else:
            P.op(eng, lambda e: e.tensor_copy(out, in_), reads, writes)

    def tt(out, in0, in1, op, reads, writes, eng="dve"):
        P.op(eng, lambda e: e.tensor_tensor(out=out, in0=in0, in1=in1, op=op), reads, writes)

    def stt(out, in0, scalar, in1, op0, op1, reads, writes, eng="dve"):
        P.op(eng, lambda e: e.scalar_tensor_tensor(out=out, in0=in0, scalar=scalar, in1=in1, op0=op0, op1=op1), reads, writes)

    def ts(out, in0, s1, s2, op0, op1, reads, writes, eng="dve"):
        if s2 is None:
            P.op(eng, lambda e: e.tensor_scalar(out=out, in0=in0, scalar1=s1, scalar2=None, op0=op0), reads, writes)
        else:
            P.op(eng, lambda e: e.tensor_scalar(out=out, in0=in0, scalar1=s1, scalar2=s2, op0=op0, op1=op1), reads, writes)

    def recip(out, in_, reads, writes):
        P.op("dve", lambda e: e.reciprocal(out, in_), reads, writes)

    def red(out, in_, op, reads, writes):
        P.op("dve", lambda e: e.tensor_reduce(out=out, in_=in_, axis=AX.X, op=op), reads, writes)

    def dma(q, out, in_, reads, writes):
        P.dma(q, lambda e: e.dma_start(out=out, in_=in_), reads, writes)

    with contextlib.ExitStack() as es0:
        def SB(es, name, shape, dt):
            return es.enter_context(nc.sbuf_tensor("sb_" + name, list(shape), dt))

        def PS(name, shape, dt):
            return es0.enter_context(nc.psum_tensor("ps_" + name, list(shape), dt))

        pX = PS("pX", [128, 1024], BF16)
        pF = [PS("pF0", [128, 512], F32), PS("pF1", [128, 512], F32)]
        pZ = PS("pZ", [128, 512], F32)
        pB = PS("pB", [128, 512], F32)
        pV = PS("pV", [128, 512], F32)
        pA = PS("pA", [128, 512], F32)
        pO = PS("pO", [128, 512], F32)
        tX, tF0, tF1, tZa, tBa, tV, tA, tO = [Tok(psum=True) for _ in range(8)]
        tZb, tBb = tZa, tBa
        tF = [tF0, tF1]
        fcnt = [0]

        def nextF():
            i = fcnt[0] % 2
            fcnt[0] += 1
            return pF[i], tF[i]

        ident_f = SB(es0, "ident_f", [128, 128], F32)
        ident_b = SB(es0, "ident_b", [128, 128], BF16)
        ones_b = SB(es0, "ones_b", [128, 128], BF16)
        ones_f = SB(es0, "ones_f", [128, 128], F32)
        cst = SB(es0, "cst", [128, 4], F32)
        hb = SB(es0, "hb", [128, 1], F32)
        catT = SB(es0, "catT", [128, 8, NTOK], BF16)
        gate = SB(es0, "gate", [128, NT, 32], F32)
        tC, tcat, tx2T, tgate = Tok(), Tok(), Tok(), Tok()
        tcatt = [Tok() for _ in range(NT)]
        x2T, tx2t = catT, tcatt
        dma("sp", ident_f[:], ctab["ident"], [], [tC])
        cp("dve", ident_b[:], ident_f[:], [tC], [tC])
        P.op("pool", lambda e: e.memset(ones_b[:], 1.0), [], [tC])
        P.op("pool", lambda e: e.memset(ones_f[:], 1.0), [], [tC])
        P.op("pool", lambda e: e.memset(cst[:, 0:1], 1.0), [], [tC])
        P.op("pool", lambda e: e.memset(cst[:, 1:2], 0.0), [], [tC])
        P.op("pool", lambda e: e.memset(cst[:, 2:3], EPS), [], [tC])
        dma("sp", hb[:], halo_bias, [], [tC])
        ONE = cst[:, 0:1]

        with contextlib.ExitStack() as esA:
            wi = SB(esA, "wi", [128, 8, 3088], BF16)
            twi = Tok()
            dma("pool", wi[:], w_in.rearrange("(kc p) n -> p kc n", p=128), [], [twi])
            wlr = SB(esA, "wlr", [17, 256], F32)
            g4 = SB(esA, "g4", [128, 4], F32)
            cm = {}
            for k in ("uneg", "mgtneg", "amask", "uneg_s", "mgtneg_s", "amask_s", "rowmask", "snew"):
                shp = list(ctab[k].shape)
                cm[k] = SB(esA, "c_" + k, shp, F32)
            tM = Tok()
            dma("sp", wlr[:], wlr_in, [], [tM])
            dma("sp", g4[:], g4_in, [], [tM])
            for k in cm:
                dma("sp", cm[k][:], ctab[k], [], [tM])

            alrT = SB(esA, "alrT", [32, 512], F32)
            talr = Tok()
            P.op("pool", lambda e: e.memset(alrT[:], 1.0), [], [talr])
            qg_f = SB(esA, "qg_f", [128, 2, 512], F32)
            kg_f = SB(esA, "kg_f", [128, 2, 512], F32)
            sr_f = SB(esA, "sr_f", [128, 4, 512], F32)
            tqg, tkg, tsr = Tok(), Tok(), Tok()
            e1 = SB(esA, "e1", [128, 256], F32)
            sp = SB(esA, "sp", [128, 256], F32)
            eb = SB(esA, "eb", [128, 2, 128], F32)
            enb = SB(esA, "enb", [128, 2, 128], F32)
            qt = SB(esA, "qt", [128, 2, 128], BF16)
            kt = SB(esA, "kt", [128, 4, 128], BF16)
            A_bf = SB(esA, "A_bf", [128, 4, 128], BF16)
            v_bf = SB(esA, "v_bf", [128, 512], BF16)
            ek = SB(esA, "ek", [128, 256], F32)
            kend = SB(esA, "kend", [128, 256], BF16)
            S32 = SB(esA, "S32", [128, 2, 128], F32)
            S_bf = SB(esA, "S_bf", [128, 4, 128], BF16)
            sq = SB(esA, "sq", [128, 512], BF16)
            rstd = SB(esA, "rstd", [128, 4, 128], F32)
            t1 = SB(esA, "t1", [128, 4, 128], F32)
            te1, tsp, teb, tenb, tqt, tkt, tAbf, tvbf, tek, tkend, tS32, tSbf, tsq, trstd, tt1 = [Tok() for _ in range(15)]
            P.op("pool", lambda e: e.memset(S32[:], 0.0), [], [tS32])
            P.op("pool", lambda e: e.memset(S_bf[:], 0.0), [], [tSbf])
            P.op("pool", lambda e: e.memset(kt[:], 0.0), [], [tkt])

            pZ3 = pZ
            pB3 = pB[:, 0:256].rearrange("p (a b) -> p a b", a=2)
            pA3 = pA[:, :].rearrange("p (a b) -> p a b", a=4)
            pO3 = pO[:, :].rearrange("p (a b) -> p a b", a=4)
            pV3 = pV[:, :].rearrange("p (a b) -> p a b", a=2)

            def fm_proj(col0, ncol, xT, txT, t0, ntok, evac):
                pf, tf = nextF()
                for kc in range(8):
                    mm(pf[0:ncol, 0:ntok], wi[:, kc, col0:col0 + ncol], xT[:, kc, t0:t0 + ntok], kc == 0, kc == 7,
                       [twi, txT], [tf])
                evac(pf[0:ncol, 0:ntok], tf)

            def tm_proj(dst, tdst, col0, ncol, xT, txT, t0):
                for kc in range(8):
                    mm(dst, xT[:, kc, t0:t0 + 128], wi[:, kc, col0:col0 + ncol], kc == 0, kc == 7, [twi, txT], [tdst])

            def block_fm_gla(xT, txT, ntok, with_q):
                fm_proj(1536, 16, xT, txT, 0, ntok,
                        lambda p, tf: cp("dve", alrT[0:16, 0:ntok], p, [tf], [talr]))
                if with_q:
                    for pr in range(2):
                        fm_proj(pr * 128, 128, xT, txT, 0, ntok,
                                lambda p, tf, pr=pr: cp("act", qg_f[:, pr, 0:ntok], p, [tf], [tqg]))
                        fm_proj(256 + pr * 128, 128, xT, txT, 0, ntok,
                                lambda p, tf, pr=pr: cp("dve", kg_f[:, pr, 0:ntok], p, [tf], [tkg]))
                    for h in range(4):
                        fm_proj(1024 + h * 128, 128, xT, txT, 0, ntok,
                                lambda p, tf, h=h: act(sr_f[:, h, 0:ntok], p, AF.Silu, [tf], [tsr]))

            def gla_chunk(xT, txT, c0, mode, cat_t0=None, tcat_tok=None):
                full = mode != "pre"
                sfx = "_s" if mode == "sample" else ""
                uneg, mgtneg, amask = cm["uneg" + sfx], cm["mgtneg" + sfx], cm["amask" + sfx]
                mm(pZ[:, 0:256], alrT[0:17, c0:c0 + 128], wlr[0:17, :], True, True, [talr, tM], [tZa])
                act(e1[:], pZ[:, 0:256], AF.Exp, [tZa], [te1], scale=-1.0)
                act(sp[:], e1[:], AF.Ln, [te1], [tsp], bias=ONE)
                tm_proj(pB[:, 256:512], tBb, 256, 256, xT, txT, c0)
                tm_proj(pV[:, :], tV, 512, 512, xT, txT, c0)
                cp("act", v_bf[:], pV[:, :], [tV], [tvbf])
                mm(pZ[:, 256:512], mgtneg[:], sp[:], True, True, [tM, tsp], [tZb])
                act(ek[:], pZ[:, 256:512], AF.Exp, [tZb], [tek])
                tt(kend[:], pB[:, 256:512], ek[:], ALU.mult, [tBb, tek], [tkend])
                if full:
                    for pr in range(2):
                        mm(pB3[:, pr, :], sp[:, pr * 128:(pr + 1) * 128], uneg[:], True, True, [tsp, tM], [tBa])
                    act(eb[:], pB3, AF.Exp, [tBa], [teb])
                    act(enb[:], pB3, AF.Exp, [tBa], [tenb], scale=-1.0)
                    stt(qt[:], qg_f[:, :, c0:c0 + 128], 0.125, eb[:], ALU.mult, ALU.mult, [tqg, teb], [tqt])
                    for hp in range(2):
                        rs = slice(64 * hp, 64 * hp + 64)
                        tt(kt[rs, hp::2, :], kg_f[rs, :, c0:c0 + 128], enb[rs, :, :], ALU.mult, [tkg, tenb], [tkt])
                    for h in range(4):
                        mm(pA3[:, h, :], kt[:, h, :], qt[:, h // 2, :], True, True, [tkt, tqt], [tA])
                    tt(A_bf[:], pA3, amask[:], ALU.mult, [tA, tM], [tAbf])
                else:
                    for pr in range(2):
                        mm(pB3[:, pr, 127:128], sp[:, pr * 128:(pr + 1) * 128], uneg[:, 127:128], True, True,
                           [tsp, tM], [tBa])
                    act(eb[:, :, 127:128], pB3[:, :, 127:128], AF.Exp, [tBa], [teb])
                return full

            def gla_out(c0, cat_t0, tcat_tok):
                act(sq[:], pO[:, :], AF.Square, [tO], [tsq])
                mm(pA[:, :], ones_b[:], sq[:], True, True, [tC, tsq], [tA])
                ts(rstd[:].rearrange("p a b -> p (a b)"), pA[:, :], 1.0 / 128, EPS, ALU.mult, ALU.add, [tA], [trstd])
                act(rstd[:].rearrange("p a b -> p (a b)"), rstd[:].rearrange("p a b -> p (a b)"), AF.Ln, [trstd], [trstd])
                act(rstd[:].rearrange("p a b -> p (a b)"), rstd[:].rearrange("p a b -> p (a b)"), AF.Exp, [trstd], [trstd], scale=-0.5)
                for h in range(4):
                    stt(t1[:, h, :], pO3[:, h, :], g4[:, h:h + 1], rstd[:, h, :], ALU.mult, ALU.mult, [tO, tM, trstd], [tt1])
                tt(catT[:, 0:4, cat_t0:cat_t0 + 128], t1[:], sr_f[:, :, c0:c0 + 128], ALU.mult, [tt1, tsr], [tcat_tok])

            def state_update():
                for pr in range(2):
                    mm(pV3[:, pr, :], kend[:, pr * 128:(pr + 1) * 128], v_bf[:, pr * 256:(pr + 1) * 256], True, True,
                       [tkend, tvbf], [tV])
                for h in range(4):
                    pr, r0 = h // 2, 64 * (h % 2)
                    stt(S32[r0:r0 + 64, pr, :], S32[r0:r0 + 64, pr, :], eb[r0:r0 + 64, pr, 127:128],
                        pV3[r0:r0 + 64, pr, (h % 2) * 128:(h % 2) * 128 + 128], ALU.mult, ALU.add, [tS32, teb, tV], [tS32])
                for hp in range(2):
                    rs = slice(64 * hp, 64 * hp + 64)
                    cp("act" if hp else "dve", S_bf[rs, hp::2, :], S32[rs, :, :], [tS32], [tSbf])

            P.enabled = "S" in phases
            with contextlib.ExitStack() as esS:
                xs_bf = SB(esS, "xs_bf", [128, 1024], BF16)
                xsT = SB(esS, "xsT", [128, 8, 128], BF16)
                qdT_s = SB(esS, "qdT_s", [128, 4, 128], BF16)
                kdT_s = SB(esS, "kdT_s", [128, 4, 128], BF16)
                kd_new = SB(esS, "kd_new", [128, 512], F32)
                vd_new = SB(esS, "vd_new", [128, 512], F32)
                vd_new_b = SB(esS, "vd_new_b", [128, 512], BF16)
                esG = contextlib.ExitStack()
                S0b = SB(esG, "S0b", [128, 16, 4, 128], BF16)
                S0f = SB(esG, "S0f", [128, 16, 2, 128], F32)
                Snew = S0f
                Vblk = SB(esG, "Vblk", [128, 16, 128], BF16)
                txs, txsT, tqd, tkd, tkdn, tvdn, tvdnb, tS0, tSn, tVb = [Tok() for _ in range(10)]
                dma("pool", xs_bf[:], xs_in, [], [txs])
                sg_v = sgla.rearrange("b (pr h2) k v -> (h2 k) b pr v", h2=2)
                P.op("pool", lambda e: e.memset(S0b[:], 0.0), [], [tS0])
                sg_h = sgla.rearrange("b (pr h2) k v -> h2 k b pr v", h2=2)
                for b4 in range(4):
                    for hp in range(2):
                        dma("pool", S0b[64 * hp:64 * hp + 64, b4 * 4:(b4 + 1) * 4, hp::2, :], sg_h[hp, :, b4 * 4:(b4 + 1) * 4], [], [tS0])
                    dma("sp", S0f[:, b4 * 4:(b4 + 1) * 4], sg_v[:, b4 * 4:(b4 + 1) * 4], [], [tS0])
                for kc in range(8):
                    tr(pX[:, kc * 128:(kc + 1) * 128], xs_bf[:, kc * 128:(kc + 1) * 128], ident_b[:], [txs, tC], [tX])
                cp("dve", xsT[:].rearrange("p a b -> p (a b)"), pX[:, :], [tX], [txsT])
                block_fm_gla(xsT, txsT, 128, True)
                for h in range(4):
                    fm_proj(1552 + h * 128, 128, xsT, txsT, 0, 128,
                            lambda p, tf, h=h: cp("dve", qdT_s[:, h, :], p, [tf], [tqd]))
                    fm_proj(2064 + h * 128, 128, xsT, txsT, 0, 128,
                            lambda p, tf, h=h: cp("act", kdT_s[:, h, :], p, [tf], [tkd]))
                pf, tf = nextF()
                tm_proj(pf[:, :], tf, 2064, 512, xsT, txsT, 0)
                cp("dve", kd_new[:], pf[:, :], [tf], [tkdn])
                pf, tf = nextF()
                tm_proj(pf[:, :], tf, 2576, 512, xsT, txsT, 0)
                cp("dve", vd_new[:], pf[:, :], [tf], [tvdn])
                cp("act", vd_new_b[:], pf[:, :], [tf], [tvdnb])
                for b in range(16):
                    dma("sp", ndk_s[b, 2040:2048, :], kd_new[8 * b:8 * b + 8, :], [tkdn], [])
                    dma("sp", ndv_s[b, 2040:2048, :], vd_new[8 * b:8 * b + 8, :], [tvdn], [])
                gla_chunk(xsT, txsT, 0, "sample")
                for h in range(4):
                    pr, r0 = h // 2, 64 * (h % 2)
                    mm(pO3[:, h, :], v_bf[:, h * 128:(h + 1) * 128], A_bf[:, h, :], True, False, [tvbf, tAbf], [tO], skip=True)
                    for b in range(16):
                        mm(pO3[:, h, 8 * b:8 * b + 8], S0b[:, b, h, :], qt[:, pr, 8 * b:8 * b + 8],
                           False, b == 15, [tS0, tqt], [tO], skip=True)
                gla_out(0, 2048, tcatt[16])
                P.enabled = "T" in phases
                for h in range(4):
                    pr, r0 = h // 2, 64 * (h % 2)
                    for b in range(16):
                        ts(Vblk[:, b, :], v_bf[:, h * 128:(h + 1) * 128], cm["rowmask"][:, b:b + 1], None, ALU.mult, None,
                           [tvbf, tM], [tVb])
                    for q4 in range(4):
                        pf, tf = nextF()
                        mm(pf[:, :], kend[:, pr * 128:(pr + 1) * 128],
                           Vblk[:, q4 * 4:(q4 + 1) * 4, :].rearrange("p a b -> p (a b)"), True, True, [tkend, tVb], [tf])
                        for bb in range(4):
                            b = q4 * 4 + bb
                            stt(Snew[r0:r0 + 64, b, pr, :], S0f[r0:r0 + 64, b, pr, :], eb[r0:r0 + 64, pr, 8 * b + 7:8 * b + 8],
                                pf[r0:r0 + 64, bb * 128:(bb + 1) * 128], ALU.mult, ALU.add, [tS0, teb, tf], [tSn])
                sgs_v = sg_s.rearrange("b (pr h2) k v -> (h2 k) b pr v", h2=2)
                for b4 in range(4):
                    dma("sp", sgs_v[:, b4 * 4:(b4 + 1) * 4], Snew[:, b4 * 4:(b4 + 1) * 4], [tSn], [])

                P.enabled = ("S" in phases) or ("T" in phases)
                P.barrier()
                esG.close()
                P.enabled = "U" in phases
                Kc = SB(esS, "Kc", [128, 16, 512], BF16)
                Vc = SB(esS, "Vc", [128, 16, 512], BF16)
                KcTb = [SB(esS, "KcT%d" % i, [128, 16, 128], BF16) for i in range(2)]
                tKcTb = [Tok(), Tok()]
                Pe = SB(esS, "Pe", [128, 512], F32)
                Pm = SB(esS, "Pm", [128, 4, 16, 8], BF16)
                smult = SB(esS, "smult", [128, 512], F32)
                Pn = SB(esS, "Pn", [128, 128], F32)
                Pnb = SB(esS, "Pnb", [128, 4, 128], BF16)
                rd = SB(esS, "rd", [128, 4, 128], F32)
                tKc, tVc, tPe, tPm, tsm, tPn, tPnb, trd = [Tok() for _ in range(8)]
                dma("sp", smult[:], ctab["smult"], [], [tsm])
                for h in range(4):
                    pf, tf = nextF()
                    mm(pf[:, 0:128], kdT_s[:, h, :], qdT_s[:, h, :], True, True, [tkd, tqd], [tf])
                    act(Pn[:], pf[:, 0:128], AF.Exp, [tf], [tPn], scale=128.0 ** -0.5)
                    tt(Pnb[:, h, :], Pn[:], cm["snew"][:], ALU.mult, [tPn, tM], [tPnb])
                Pms = SB(esS, "Pms", [128, 4, 8], F32)
                tPms = Tok()
                pVn = pV[:, :].rearrange("p (a b) -> p a b", a=4)
                for h in range(4):
                    mm(pO3[:, h, :], vd_new_b[:, h * 128:(h + 1) * 128], Pnb[:, h, :], True, False, [tvdnb, tPnb], [tO], skip=True)
                    mm(pVn[:, h, :], ones_b[:], Pnb[:, h, :], True, True, [tC, tPnb], [tV])
                for b in range(16):
                    dma("sp", ndk_s[b, 0:2040, :].rearrange("(a r) f -> a (r f)", a=120),
                        cdk[b, 8:2048, :].rearrange("(a r) f -> a (r f)", a=120), [], [])
                    dma("sp", ndv_s[b, 0:2040, :].rearrange("(a r) f -> a (r f)", a=120),
                        cdv[b, 8:2048, :].rearrange("(a r) f -> a (r f)", a=120), [], [])
                    dma("pool", Kc[:], cdk[b].rearrange("(p j) f -> p j f", j=16), [], [tKc])
                    dma("pool", Vc[:], cdv[b].rearrange("(p j) f -> p j f", j=16), [], [tVc])
                    pf, tf = nextF()
                    pf4 = pf[:, :].rearrange("p (h j t) -> p h j t", h=4, j=16)
                    for h in range(4):
                        KcT, tKcT = KcTb[h % 2], tKcTb[h % 2]
                        for j2 in range(2):
                            for jj in range(8):
                                j = j2 * 8 + jj
                                tr(pX[:, jj * 128:(jj + 1) * 128], Kc[:, j, h * 128:(h + 1) * 128], ident_b[:], [tKc, tC], [tX])
                            cp("dve" if j2 == 0 else "act", KcT[:, j2 * 8:(j2 + 1) * 8, :].rearrange("p a b -> p (a b)"),
                               pX[:, :], [tX], [tKcT])
                        for j in range(16):
                            mm(pf4[:, h, j, :], KcT[:, j, :], qdT_s[:, h, 8 * b:8 * b + 8], True, True, [tKcT, tqd], [tf])
                    act(Pe[:], pf[:, :], AF.Exp, [tf], [tPe], scale=128.0 ** -0.5)
                    tt(Pm[:].rearrange("p h j t -> p (h j t)"), Pe[:], smult[:], ALU.mult, [tPe, tsm], [tPm])
                    for h in range(4):
                        for j in range(16):
                            last = (b == 15 and j == 15)
                            mm(pO3[:, h, 8 * b:8 * b + 8], Vc[:, j, h * 128:(h + 1) * 128], Pm[:, h, j, :], False, last,
                               [tVc, tPm], [tO], skip=True)
                    P.op("dve", lambda e: e.tensor_reduce(out=Pms[:], in_=Pm[:].rearrange("p h j t -> p h t j"), axis=AX.X, op=ALU.add),
                         [tPm], [tPms])
                    mm(pA[:, b * 32:(b + 1) * 32], ones_f[:], Pms[:].rearrange("p h t -> p (h t)"), True, True, [tC, tPms], [tA])
                rd4 = rd[:].rearrange("p h (b t) -> p h b t", b=16)
                cp("act", rd[:], pVn, [tV], [trd])
                tt(rd4, pA[:, :].rearrange("p (b h t) -> p h b t", b=16, h=4), rd4, ALU.add, [tA, trd], [trd])
                recip(rd[:], rd[:], [trd], [trd])
                tt(catT[:, 4:8, 2048:2176], pO3, rd[:], ALU.mult, [tO, trd], [tcatt[16]])
            P.barrier()

            P.enabled = "P" in phases
            with contextlib.ExitStack() as esP:
                xld = [SB(esP, "xld%d" % i, [128, 4, 1024], BF16) for i in range(2)]
                xTb = [SB(esP, "xTb%d" % i, [128, 8, 512], BF16) for i in range(2)]
                txld = [Tok(), Tok()]
                txTb = [Tok(), Tok()]
                stg = [SB(esP, "stg%d" % i, [128, 512], BF16) for i in range(2)]
                tstg = [Tok(), Tok()]
                stf = [SB(esP, "stf%d" % i, [128, 512], F32) for i in range(2)]
                tstf = [Tok(), Tok()]
                scnt = [0, 0]
                tscrQ, tscrK, tscrV = Tok(), Tok(), Tok()
                for blk in range(16):
                    mode = "pre" if blk < 8 else ("halo" if blk < 12 else "main")
                    bi = blk % 2
                    xl, xT, txl, txT = xld[bi], xTb[bi], txld[bi], txTb[bi]
                    dma("pool", xl[:], xp[blk * 512:(blk + 1) * 512, :].rearrange("(j p) f -> p j f", p=128), [], [txl])
                    for j in range(4):
                        for kc in range(8):
                            tr(pX[:, kc * 128:(kc + 1) * 128], xl[:, j, kc * 128:(kc + 1) * 128], ident_b[:], [txl, tC], [tX])
                        cp("dve" if j % 2 == 0 else "act", xT[:, :, j * 128:(j + 1) * 128],
                           pX[:, :].rearrange("p (a b) -> p a b", a=8), [tX], [txT])
                    block_fm_gla(xT, txT, 512, mode == "main")
                    if mode != "pre":
                        hoff = (blk - 8) * 512
                        for h in range(4):
                            def ev_k(p, tf, h=h):
                                i = scnt[0] % 2
                                scnt[0] += 1
                                cp("dve", stg[i][:, :], p, [tf], [tstg[i]])
                                dma("sp", kscr[:, h, hoff:hoff + 512], stg[i][:, :], [tstg[i]], [tscrK])
                            fm_proj(2064 + h * 128, 128, xT, txT, 0, 512, ev_k)
                        if mode == "main":
                            moff = (blk - 12) * 512
                            for h in range(4):
                                def ev_q(p, tf, h=h):
                                    i = scnt[0] % 2
                                    scnt[0] += 1
                                    cp("act", stg[i][:, :], p, [tf], [tstg[i]])
                                    dma("sp", qscr[:, h, moff:moff + 512], stg[i][:, :], [tstg[i]], [tscrQ])
                                fm_proj(1552 + h * 128, 128, xT, txT, 0, 512, ev_q)
                    for j in range(4):
                        c0 = j * 128
                        full = gla_chunk(xT, txT, c0, "main" if mode == "main" else "pre")
                        if mode != "pre":
                            pf, tf = nextF()
                            tm_proj(pf[:, :], tf, 2576, 512, xT, txT, c0)
                            i = scnt[0] % 2
                            scnt[0] += 1
                            cp("act", stg[i][:, :], pf[:, :], [tf], [tstg[i]])
                            row0 = (blk - 8) * 512 + c0
                            dma("sp", vscr[row0:row0 + 128, :], stg[i][:, :], [tstg[i]], [tscrV])
                            if mode == "main":
                                orow = (blk - 12) * 512 + c0
                                i2 = scnt[1] % 2
                                scnt[1] += 1
                                cp("dve", stf[i2][:, :], pf[:, :], [tf], [tstf[i2]])
                                dma("sp", ndv_p[orow:orow + 128, :], stf[i2][:, :], [tstf[i2]], [])
                                pf, tf = nextF()
                                tm_proj(pf[:, :], tf, 2064, 512, xT, txT, c0)
                                i2 = scnt[1] % 2
                                scnt[1] += 1
                                cp("dve", stf[i2][:, :], pf[:, :], [tf], [tstf[i2]])
                                dma("sp", ndk_p[orow:orow + 128, :], stf[i2][:, :], [tstf[i2]], [])
                        if full:
                            tok0 = (blk - 12) * 512 + c0
                            for h in range(4):
                                pr, r0 = h // 2, 64 * (h % 2)
                                mm(pO3[:, h, :], v_bf[:, h * 128:(h + 1) * 128], A_bf[:, h, :], True, False, [tvbf, tAbf], [tO])
                                mm(pO3[:, h, :], S_bf[:, h, :], qt[:, pr, :], False, True, [tSbf, tqt], [tO])
                            gla_out(c0, tok0, tcatt[tok0 // 128])
                        state_update()
                dma("sp", sg_p.rearrange("(pr h2) k v -> (h2 k) pr v", h2=2), S32[:], [tS32], [])
            P.barrier()
        P.barrier()

        P.enabled = "B" in phases
        with contextlib.ExitStack() as esB:
            QT = SB(esB, "QT", [128, 4, 2048], BF16)
            KT = SB(esB, "KT", [128, 4, 4096], BF16)
            dmask = SB(esB, "dmask", [128, 256], F32)
            dmask_b = SB(esB, "dmask_b", [128, 256], BF16)
            nacc = SB(esB, "nacc", [128, 2048], F32)
            dacc = SB(esB, "dacc", [128, 2048], F32)
            tQ, tK, tdm, tnacc, tdacc = [Tok() for _ in range(5)]
            NV = 8
            Vt = [SB(esB, "Vt%d" % i, [128, 128], BF16) for i in range(NV)]
            tVt = [Tok() for _ in range(NV)]
            NP_ = 8
            PT = [SB(esB, "PT%d" % i, [128, 256], BF16) for i in range(NP_)]
            tPT = [Tok() for _ in range(NP_)]
            for h in range(4):
                dma("sp", QT[:, h, :], qscr[:, h, :], [tscrQ], [tQ])
                dma("sp", KT[:, h, :], kscr[:, h, :], [tscrK], [tK])
            dma("sp", dmask[:], ctab["dilmask"], [], [tdm])
            cp("dve", dmask_b[:], dmask[:], [tdm], [tdm])
            scale = 128.0 ** -0.5
            numb = [(pO, tO), (pA, tA)]
            denb = [(pZ, tZa), (pB, tBa)]
            cnt = [0, 0, 0]
            for h in range(4):
                first_branch = True
                for (d, nres, nb) in ((1, 1, 16), (4, 4, 4), (16, 16, 1)):
                    qlist = [(r, qb) for r in range(nres) for qb in range(nb)]
                    for g0 in range(0, len(qlist), 4):
                        grp = qlist[g0:g0 + 4]
                        pn, tn = numb[cnt[2] % 2]
                        pd, td = denb[cnt[2] % 2]
                        cnt[2] += 1
                        ktiles = []
                        for (r, qb) in grp:
                            for kt_ in (qb - 1, qb):
                                if (r, kt_) not in ktiles:
                                    ktiles.append((r, kt_))
                        slot_of = {rq: i for i, rq in enumerate(grp)}
                        opened = set()
                        pend = []
                        for (r, kt_) in ktiles:
                            qbs = [qb for qb in (kt_, kt_ + 1) if (r, qb) in slot_of]
                            q_lo = min(qbs)
                            ncol = 128 * len(qbs)
                            mcol0 = 128 * (q_lo - kt_)
                            kidx0 = 2048 + r + d * 128 * kt_
                            qidx0 = r + d * 128 * q_lo
                            pf, tf = nextF()
                            mm(pf[:, 0:ncol], KT[:, h, kidx0:kidx0 + d * 127 + 1:d], QT[:, h, qidx0:qidx0 + d * (ncol - 1) + 1:d],
                               True, False, [tK, tQ], [tf])
                            mm(pf[:, 0:ncol], ident_b[:], dmask_b[:, mcol0:mcol0 + ncol], False, True, [tC, tdm], [tf])
                            ip = cnt[0] % NP_
                            cnt[0] += 1
                            act(PT[ip][:, 0:ncol], pf[:, 0:ncol], AF.Exp, [tf, tC], [tPT[ip]],
                                bias=(hb[:, 0:1] if kt_ < 0 else cst[:, 1:2]), scale=scale)
                            iv = cnt[1] % NV
                            cnt[1] += 1
                            row0 = 2048 + r + d * 128 * kt_
                            dma("sp", Vt[iv][:, :], vscr[row0:row0 + d * 127 + 1:d, h * 128:(h + 1) * 128], [tscrV], [tVt[iv]])
                            pend.append((ip, iv, qbs, q_lo, r, kt_))
                        contrib = {}
                        for (ip, iv, qbs, q_lo, r, kt_) in pend:
                            for qb in qbs:
                                contrib.setdefault((r, qb), []).append((ip, iv, 128 * (qb - q_lo)))
                        for (r, qb), lst in contrib.items():
                            sl = slot_of[(r, qb)]
                            for n_, (ip, iv, c0) in enumerate(lst):
                                st, sp_ = n_ == 0, n_ == len(lst) - 1
                                mm(pn[:, sl * 128:(sl + 1) * 128], Vt[iv][:, :], PT[ip][:, c0:c0 + 128], st, sp_,
                                   [tVt[iv], tPT[ip]], [tn])
                                mm(pd[:, sl * 128:(sl + 1) * 128], ones_b[:], PT[ip][:, c0:c0 + 128], st, sp_,
                                   [tC, tPT[ip]], [td])
                        for sl, (r, qb) in enumerate(grp):
                            a0 = r + d * 128 * qb
                            dst_n = nacc[:, a0:a0 + d * 127 + 1:d]
                            dst_d = dacc[:, a0:a0 + d * 127 + 1:d]
                            if first_branch:
                                cp("dve", dst_n, pn[:, sl * 128:(sl + 1) * 128], [tn], [tnacc])
                                cp("act", dst_d, pd[:, sl * 128:(sl + 1) * 128], [td], [tdacc])
                            else:
                                tt(dst_n, dst_n, pn[:, sl * 128:(sl + 1) * 128], ALU.add, [tn, tnacc], [tnacc])
                                tt(dst_d, dst_d, pd[:, sl * 128:(sl + 1) * 128], ALU.add, [td, tdacc], [tdacc])
                    first_branch = False
                recip(dacc[:], dacc[:], [tdacc], [tdacc])
                for q4 in range(4):
                    tt(catT[:, 4 + h, q4 * 512:(q4 + 1) * 512], nacc[:, q4 * 512:(q4 + 1) * 512], dacc[:, q4 * 512:(q4 + 1) * 512],
                       ALU.mult, [tnacc, tdacc], [tcatt[q4 * 4 + i] for i in range(4)])
        P.barrier()

        P.enabled = "C" in phases
        with contextlib.ExitStack() as esC:
            wo_ = SB(esC, "wout", [128, 8, 1024], BF16)
            wq_ = SB(esC, "wq", [128, 8, 1024], BF16)
            wmo_ = SB(esC, "wmo", [128, 8, 1024], BF16)
            mKT = SB(esC, "mKT", [128, 8, 256], BF16)
            mV = SB(esC, "mV", [128, 2, 1024], BF16)
            lnt = SB(esC, "lnt", [128, 4, 1024], F32)
            wr_ = SB(esC, "wr", [128, 8, 36], F32)
            br_ = SB(esC, "br", [128, 36], F32)
            esM = contextlib.ExitStack()
            wtmp = SB(esM, "wtmp", [128, 8, 1024], BF16)
            tW, tWt, tln = Tok(), Tok(), Tok()
            tWo, tWq, tWmo = Tok(), Tok(), Tok()
            for i in range(4):
                dma("sp", lnt[:, i, :], lnp[i], [], [tln])
            dma("sp", wr_[:], w_r.rearrange("(kc p) n -> p kc n", p=128), [], [tln])
            dma("sp", br_[:], b_r, [], [tln])
            mem_bf = SB(esM, "mem_bf", [128, 2, 1024], BF16)
            memT = SB(esM, "memT", [128, 8, 256], BF16)
            mo32 = [SB(esM, "mo32_%d" % i, [128, 512], F32) for i in range(2)]
            tmem, tmemT, tmKT, tmV = Tok(), Tok(), Tok(), Tok()
            tmo32 = [Tok(), Tok()]
            dma("pool", mem_bf[:], memp.rearrange("(mt p) f -> p mt f", p=128), [], [tmem])
            for mt in range(2):
                for kc in range(8):
                    tr(pX[:, kc * 128:(kc + 1) * 128], mem_bf[:, mt, kc * 128:(kc + 1) * 128], ident_b[:], [tmem, tC], [tX])
                cp("dve", memT[:, :, mt * 128:(mt + 1) * 128], pX[:, :].rearrange("p (a b) -> p a b", a=8), [tX], [tmemT])
            mcnt = [0]
            for which, wsrc, dst in ((0, w_k, mk_p), (1, w_v, mv_p)):
                dma("pool", wtmp[:], wsrc.rearrange("(kc p) n -> p kc n", p=128), [], [tWt])
                if which == 0:
                    dma("pool", wo_[:], w_out.rearrange("(kc p) n -> p kc n", p=128), [], [tWo])
                    dma("pool", wq_[:], w_q.rearrange("(kc p) n -> p kc n", p=128), [], [tWq])
                    dma("pool", wmo_[:], w_o.rearrange("(kc p) n -> p kc n", p=128), [], [tWmo])
                for mt in range(2):
                    for half in range(2):
                        pf, tf = nextF()
                        for kc in range(8):
                            mm(pf[:, :], memT[:, kc, mt * 128:(mt + 1) * 128], wtmp[:, kc, half * 512:(half + 1) * 512],
                               kc == 0, kc == 7, [tmemT, tWt], [tf])
                        i = mcnt[0] % 2
                        mcnt[0] += 1
                        cp("dve", mo32[i][:, :], pf[:, :], [tf], [tmo32[i]])
                        dma("sp", dst[mt * 128:(mt + 1) * 128, half * 512:(half + 1) * 512], mo32[i][:, :], [tmo32[i]], [])
                        if which == 1:
                            cp("act", mV[:, mt, half * 512:(half + 1) * 512], pf[:, :], [tf], [tmV])
                if which == 0:
                    for ch in range(8):
                        pf, tf = nextF()
                        for kc in range(8):
                            mm(pf[:, 0:256], wtmp[:, kc, ch * 128:(ch + 1) * 128], memT[:, kc, :], kc == 0, kc == 7,
                               [tWt, tmemT], [tf])
                        cp("act", mKT[:, ch, :], pf[:, 0:256], [tf], [tmKT])

            P.barrier()
            esM.close()
            xin = [SB(esC, "xin%d" % i, [128, 1024], F32) for i in range(2)]
            txin = [Tok(), Tok()]
            SETS = []
            for si in range(2):
                d_ = {}
                for nm, shp, dt in (("u", [128, 1024], F32), ("x1", [128, 1024], F32), ("x1b", [128, 1024], BF16),
                                    ("x1T", [128, 8, 128], BF16), ("qT", [128, 8, 128], BF16), ("pTm", [128, 2, 128], BF16),
                                    ("omT", [128, 8, 128], BF16), ("rdn", [128, 128], F32), ("x2", [128, 1024], F32),
                                    ("x2b", [128, 1024], BF16), ("x2Tf", [128, 8, 128], F32), ("st", [128, 16], F32),
                                    ("lg", [128, 36], F32), ("rt", [128, 8, 32], F32)):
                    d_[nm] = SB(esC, "%s_%d" % (nm, si), shp, dt)
                    d_["t" + nm] = Tok()
                SETS.append(d_)
            cK = SB(esC, "cK", [128, 2, 1024], BF16)
            cV = SB(esC, "cV", [128, 2, 1024], BF16)
            cKT = SB(esC, "cKT", [128, 8, 256], BF16)
            pTs = SB(esC, "pTs", [128, 2, 4, 8], BF16)
            tcK, tcV, tcKT, tpTs = Tok(), Tok(), Tok(), Tok()

            def layer_norm(src, tsrc, gi, dst, tdst, st, tst):
                red(st[:, 0:1], src[:, :], ALU.add, [tsrc], [tst])
                act(dst[:, :], src[:, :], AF.Square, [tsrc], [tdst])
                red(st[:, 1:2], dst[:, :], ALU.add, [tdst], [tst])
                ts(st[:, 2:3], st[:, 0:1], 1.0 / 1024, None, ALU.mult, None, [tst], [tst])
                tt(st[:, 3:4], st[:, 2:3], st[:, 2:3], ALU.mult, [tst], [tst])
                stt(st[:, 4:5], st[:, 1:2], 1.0 / 1024, st[:, 3:4], ALU.mult, ALU.subtract, [tst], [tst])
                ts(st[:, 5:6], st[:, 4:5], EPS, None, ALU.add, None, [tst], [tst])
                act(st[:, 5:6], st[:, 5:6], AF.Ln, [tst], [tst])
                act(st[:, 5:6], st[:, 5:6], AF.Exp, [tst], [tst], scale=-0.5)
                ts(dst[:, :], src[:, :], st[:, 2:3], st[:, 5:6], ALU.subtract, ALU.mult, [tsrc, tst], [tdst])
                tt(dst[:, :], dst[:, :], lnt[:, gi, :], ALU.mult, [tdst, tln], [tdst], eng="pool")
                tt(dst[:, :], dst[:, :], lnt[:, gi + 1, :], ALU.add, [tdst, tln], [tdst], eng="pool")

            pM = [pO, pA]
            tMM = [tO, tA]
            pO8 = [pZ, pB]
            tO8 = [tZa, tBa]
            def tile_body(ti):
                d_ = SETS[ti % 2]
                u, x1, x1b, x1T, qT, pTm, omT, rdn, x2, x2b, x2Tf, st, lg, rt = [d_[k] for k in (
                    "u", "x1", "x1b", "x1T", "qT", "pTm", "omT", "rdn", "x2", "x2b", "x2Tf", "st", "lg", "rt")]
                tu, tx1, tx1b, tx1T, tqT, tpTm, tomT, trdn, tx2, tx2b, tx2Tf, tst, tlg, trt = [d_["t" + k] for k in (
                    "u", "x1", "x1b", "x1T", "qT", "pTm", "omT", "rdn", "x2", "x2b", "x2Tf", "st", "lg", "rt")]
                xi, txi = xin[ti % 2], txin[ti % 2]
                dma("sp", xi[:], xres[ti * 128:(ti + 1) * 128, :], [], [txi])
                for half in range(2):
                    for kc in range(8):
                        mm(pM[half][:, :], catT[:, kc, ti * 128:(ti + 1) * 128], wo_[:, kc, half * 512:(half + 1) * 512],
                           kc == 0, kc == 7, [tcatt[ti], tWo], [tMM[half]])
                    stt(u[:, half * 512:(half + 1) * 512], xi[:, half * 512:(half + 1) * 512], ALPHA, pM[half][:, :],
                        ALU.mult, ALU.add, [txi, tMM[half]], [tu])
                yield
                layer_norm(u, tu, 0, x1, tx1, st, tst)
                cp("act", x1b[:], x1[:], [tx1], [tx1b])
                for kc in range(8):
                    tr(pX[:, kc * 128:(kc + 1) * 128], x1b[:, kc * 128:(kc + 1) * 128], ident_b[:], [tx1b, tC], [tX])
                cp("dve", x1T[:].rearrange("p a b -> p (a b)"), pX[:, :], [tX], [tx1T])
                yield
                for ch in range(8):
                    pf, tf = nextF()
                    for kc in range(8):
                        mm(pf[:, 0:128], wq_[:, kc, ch * 128:(ch + 1) * 128], x1T[:, kc, :], kc == 0, kc == 7, [tWq, tx1T], [tf])
                    cp("act" if ch % 2 else "dve", qT[:, ch, :], pf[:, 0:128], [tf], [tqT])
                yield
                if ti < 16:
                    for h in range(4):
                        pf, tf = nextF()
                        pf3 = pf[:, 0:256].rearrange("p (a b) -> p a b", a=2)
                        for mt in range(2):
                            for cc in range(2):
                                mm(pf3[:, mt, :], mKT[:, h * 2 + cc, mt * 128:(mt + 1) * 128], qT[:, h * 2 + cc, :],
                                   cc == 0, cc == 1, [tmKT, tqT], [tf])
                        act(pTm[:], pf3, AF.Exp, [tf], [tpTm], scale=1.0 / 16)
                        for cc in range(2):
                            ch = h * 2 + cc
                            bank, tb = pO8[ch // 4], tO8[ch // 4]
                            for mt in range(2):
                                mm(bank[:, (ch % 4) * 128:(ch % 4 + 1) * 128], mV[:, mt, ch * 128:(ch + 1) * 128], pTm[:, mt, :],
                                   mt == 0, mt == 1, [tmV, tpTm], [tb])
                        pf2, tf2 = nextF()
                        for mt in range(2):
                            mm(pf2[:, 0:128], ones_b[:], pTm[:, mt, :], mt == 0, mt == 1, [tC, tpTm], [tf2])
                        recip(rdn[:], pf2[:, 0:128], [tf2], [trdn])
                        for cc in range(2):
                            ch = h * 2 + cc
                            bank, tb = pO8[ch // 4], tO8[ch // 4]
                            tt(omT[:, ch, :], bank[:, (ch % 4) * 128:(ch % 4 + 1) * 128], rdn[:], ALU.mult, [tb, trdn], [tomT])
                else:
                    pf_d, tf_d = pV, tV
                    pfd4 = pf_d[:, :].rearrange("p (b h t) -> p h b t", b=16, h=4)
                    for b in range(16):
                        dma("pool", cK[:], cmk[b].rearrange("(mt p) f -> p mt f", p=128), [], [tcK])
                        dma("pool", cV[:], cmv[b].rearrange("(mt p) f -> p mt f", p=128), [], [tcV])
                        for mt in range(2):
                            for kc in range(8):
                                tr(pX[:, kc * 128:(kc + 1) * 128], cK[:, mt, kc * 128:(kc + 1) * 128], ident_b[:], [tcK, tC], [tX])
                            cp("dve" if mt == 0 else "act", cKT[:, :, mt * 128:(mt + 1) * 128],
                               pX[:, :].rearrange("p (a b) -> p a b", a=8), [tX], [tcKT])
                        pf, tf = nextF()
                        pf4 = pf[:, 0:64].rearrange("p (m h t) -> p m h t", m=2, h=4)
                        for h in range(4):
                            for mt in range(2):
                                for cc in range(2):
                                    mm(pf4[:, mt, h, :], cKT[:, h * 2 + cc, mt * 128:(mt + 1) * 128], qT[:, h * 2 + cc, 8 * b:8 * b + 8],
                                       cc == 0, cc == 1, [tcKT, tqT], [tf])
                        act(pTs[:].rearrange("p m h t -> p (m h t)"), pf[:, 0:64], AF.Exp, [tf], [tpTs], scale=1.0 / 16)
                        for ch in range(8):
                            h = ch // 2
                            bank, tb = pO8[ch // 4], tO8[ch // 4]
                            for mt in range(2):
                                mm(bank[:, (ch % 4) * 128 + 8 * b:(ch % 4) * 128 + 8 * b + 8], cV[:, mt, ch * 128:(ch + 1) * 128],
                                   pTs[:, mt, h, :], mt == 0, mt == 1, [tcV, tpTs], [tb])
                        for mt in range(2):
                            mm(pf_d[:, b * 32:(b + 1) * 32], ones_b[:], pTs[:, mt].rearrange("p h t -> p (h t)"), mt == 0, mt == 1,
                               [tC, tpTs], [tf_d])
                    for h in range(4):
                        recip(rdn[:].rearrange("p (b t) -> p b t", b=16), pfd4[:, h], [tf_d], [trdn])
                        for cc in range(2):
                            ch = h * 2 + cc
                            bank, tb = pO8[ch // 4], tO8[ch // 4]
                            tt(omT[:, ch, :], bank[:, (ch % 4) * 128:(ch % 4 + 1) * 128], rdn[:], ALU.mult, [tb, trdn], [tomT])
                yield
                for half in range(2):
                    for kc in range(8):
                        mm(pM[half][:, :], omT[:, kc, :], wmo_[:, kc, half * 512:(half + 1) * 512], kc == 0, kc == 7,
                           [tomT, tWmo], [tMM[half]])
                    stt(u[:, half * 512:(half + 1) * 512], x1[:, half * 512:(half + 1) * 512], ALPHA, pM[half][:, :],
                        ALU.mult, ALU.add, [tx1, tMM[half]], [tu])
                yield
                layer_norm(u, tu, 2, x2, tx2, st, tst)
                cp("act", x2b[:], x2[:], [tx2], [tx2b])
                for kc in range(8):
                    tr(pX[:, kc * 128:(kc + 1) * 128], x2b[:, kc * 128:(kc + 1) * 128], ident_b[:], [tx2b, tC], [tX])
                cp("dve", x2T[:, :, ti * 128:(ti + 1) * 128], pX[:, :].rearrange("p (a b) -> p a b", a=8), [tX], [tx2t[ti]])
                yield
                for half in range(2):
                    for k4 in range(4):
                        kc = half * 4 + k4
                        tr(pM[half][:, k4 * 128:(k4 + 1) * 128], x2[:, kc * 128:(kc + 1) * 128], ident_f[:], [tx2, tC], [tMM[half]])
                    cp("act", x2Tf[:, half * 4:(half + 1) * 4, :].rearrange("p a b -> p (a b)"), pM[half][:, :], [tMM[half]], [tx2Tf])
                pf, tf = nextF()
                for kc in range(8):
                    mm(pf[:, 0:36], x2Tf[:, kc, :], wr_[:, kc, :], kc == 0, kc == 7, [tx2Tf, tln], [tf])
                tt(lg[:], pf[:, 0:36], br_[:], ALU.add, [tf, tln], [tlg])
                yield
                R = [tlg, trt]
                red(rt[:, 0, 0:1], lg[:, 0:4], ALU.max, R, [trt])
                ts(rt[:, 0, 1:2], rt[:, 0, 0:1], -1.0, None, ALU.mult, None, R, [trt])
                act(rt[:, 1, 0:4], lg[:, 0:4], AF.Exp, R, [trt], bias=rt[:, 0, 1:2])
                red(rt[:, 0, 2:3], rt[:, 1, 0:4], ALU.add, R, [trt])
                recip(rt[:, 0, 3:4], rt[:, 0, 2:3], R, [trt])
                ts(rt[:, 1, 4:8], lg[:, 0:4], rt[:, 0, 0:1], None, ALU.is_equal, None, R, [trt])
                ts(rt[:, 1, 8:12], rt[:, 1, 4:8], -1.0, 30000.0, ALU.add, ALU.mult, R, [trt])
                for g in range(4):
                    ts(rt[:, 2, g * 8:(g + 1) * 8], lg[:, 4 + g * 8:4 + (g + 1) * 8], rt[:, 1, 8 + g:9 + g], None,
                       ALU.add, None, R, [trt])
                red(rt[:, 0, 4:5], rt[:, 2, :], ALU.max, R, [trt])
                ts(rt[:, 3, :], rt[:, 2, :], rt[:, 0, 4:5], None, ALU.is_equal, None, R, [trt])
                stt(rt[:, 4, :], rt[:, 3, :], -60000.0, rt[:, 2, :], ALU.mult, ALU.add, R, [trt])
                red(rt[:, 0, 5:6], rt[:, 4, :], ALU.max, R, [trt])
                ts(rt[:, 5, :], rt[:, 4, :], rt[:, 0, 5:6], None, ALU.is_equal, None, R, [trt])
                tt(rt[:, 0, 6:7], rt[:, 0, 5:6], rt[:, 0, 4:5], ALU.subtract, R, [trt])
                act(rt[:, 0, 7:8], rt[:, 0, 6:7], AF.Exp, R, [trt])
                ts(rt[:, 0, 8:9], rt[:, 0, 7:8], 1.0, None, ALU.add, None, R, [trt])
                recip(rt[:, 0, 9:10], rt[:, 0, 8:9], R, [trt])
                tt(rt[:, 0, 10:11], rt[:, 0, 9:10], rt[:, 0, 7:8], ALU.mult, R, [trt])
                tt(rt[:, 0, 11:12], rt[:, 0, 9:10], rt[:, 0, 3:4], ALU.mult, R, [trt])
                tt(rt[:, 0, 12:13], rt[:, 0, 10:11], rt[:, 0, 3:4], ALU.mult, R, [trt])
                ts(rt[:, 6, :], rt[:, 3, :], rt[:, 0, 11:12], None, ALU.mult, None, R, [trt])
                stt(gate[:, ti, :], rt[:, 5, :], rt[:, 0, 12:13], rt[:, 6, :], ALU.mult, ALU.add, R, [tgate])
                yield
                ts(x2[:, :], x2[:, :], ALPHA, None, ALU.mult, None, [tx2], [tx2], eng="pool")
                dma("sp", x2scr[ti * 128:(ti + 1) * 128, :], x2[:, :], [tx2], [tgate])
                yield
            order = [(0, 1), (2, 3), (4, 5), (6, 7), (8, 9), (10, 11), (12, 13), (14, 15), (16,)]
            for grp_ in order:
                gens_ = [tile_body(t_) for t_ in grp_]
                while gens_:
                    for g_ in list(gens_):
                        try:
                            next(g_)
                        except StopIteration:
                            gens_.remove(g_)
        P.barrier()

        P.enabled = "D" in phases
        with contextlib.ExitStack() as esD:
            yacc = SB(esD, "yacc", [128, NT, 1024], F32)
            lnf = SB(esD, "lnf", [128, 2, 1024], F32)
            tyt = [Tok() for _ in range(NT)]
            tlnf = Tok()
            for ti in range(NT):
                dma("sp", yacc[:, ti, :], x2scr[ti * 128:(ti + 1) * 128, :], [tgate], [tyt[ti]])
            dma("sp", lnf[:, 0, :], lnp[4], [], [tlnf])
            dma("sp", lnf[:, 1, :], lnp[5], [], [tlnf])
            esE = contextlib.ExitStack()
            Wg = [SB(esE, "Wg%d" % i, [128, 8, 512], BF16) for i in range(2)]
            Wu = [SB(esE, "Wu%d" % i, [128, 8, 512], BF16) for i in range(2)]
            Wd = [SB(esE, "Wd%d" % i, [128, 4, 1024], BF16) for i in range(2)]
            tWg, tWu, tWd = [Tok(), Tok()], [Tok(), Tok()], [Tok(), Tok()]
            sg_ = [SB(esE, "sg%d" % i, [128, 512], F32) for i in range(2)]
            tsg = [Tok(), Tok()]
            hT = [SB(esE, "hT%d" % i, [128, 4, 512], BF16) for i in range(2)]
            thT = [Tok(), Tok()]
            ytmp = [SB(esE, "ytmp%d" % i, [128, 512], F32) for i in range(2)]
            tytmp = [Tok(), Tok()]
            pG = [(pF[0], tF0), (pF[1], tF1)]
            pU = [(pZ, tZa), (pB, tBa)]
            pY = [(pO, tO), (pA, tA), (pV, tV)]
            blocks = [(0, 512), (512, 512), (1024, 512), (1536, 512), (2048, 128)]
            cg, cy, ch_ = 0, 0, 0
            for e in range(32):
                wb = e % 2
                dma("pool", Wg[wb][:], w_eg[e].rearrange("(kc p) n -> p kc n", p=128), [], [tWg[wb]])
                dma("pool", Wu[wb][:], w_eu[e].rearrange("(kc p) n -> p kc n", p=128), [], [tWu[wb]])
                dma("pool", Wd[wb][:], w_ed[e].rearrange("(kc p) n -> p kc n", p=128), [], [tWd[wb]])
                for (t0, nt) in blocks:
                    hb_i = ch_ % 2
                    ch_ += 1
                    tiles = list(range(t0 // 128, (t0 + nt) // 128))
                    for hc in range(4):
                        (pg, tg), (pu, tu_) = pG[cg % 2], pU[cg % 2]
                        sgi = cg % 2
                        cg += 1
                        for kc in range(8):
                            mm(pg[:, 0:nt], Wg[wb][:, kc, hc * 128:(hc + 1) * 128], x2T[:, kc, t0:t0 + nt], kc == 0, kc == 7,
                               [tWg[wb]] + [tx2t[i] for i in tiles], [tg])
                        for kc in range(8):
                            mm(pu[:, 0:nt], Wu[wb][:, kc, hc * 128:(hc + 1) * 128], x2T[:, kc, t0:t0 + nt], kc == 0, kc == 7,
                               [tWu[wb]] + [tx2t[i] for i in tiles], [tu_])
                        act(sg_[sgi][:, 0:nt], pg[:, 0:nt], AF.Silu, [tg], [tsg[sgi]])
                        tt(hT[hb_i][:, hc, 0:nt], sg_[sgi][:, 0:nt], pu[:, 0:nt], ALU.mult, [tsg[sgi], tu_], [thT[hb_i]])
                    for ti in tiles:
                        o0 = ti * 128 - t0
                        for half in range(2):
                            py, ty = pY[cy % 3]
                            cy += 1
                            for hc in range(4):
                                mm(py[:, :], hT[hb_i][:, hc, o0:o0 + 128], Wd[wb][:, hc, half * 512:(half + 1) * 512], hc == 0, hc == 3,
                                   [thT[hb_i], tWd[wb]], [ty])
                            ya = yacc[:, ti, half * 512:(half + 1) * 512]
                            if half == 0:
                                stt(ya, py[:, :], gate[:, ti, e:e + 1], ya, ALU.mult, ALU.add, [ty, tgate, tyt[ti]], [tyt[ti]])
                            else:
                                iy = ti % 2
                                act(ytmp[iy][:, :], py[:, :], AF.Identity, [ty, tgate], [tytmp[iy]], scale=gate[:, ti, e:e + 1])
                                tt(ya, ya, ytmp[iy][:, :], ALU.add, [tytmp[iy], tyt[ti]], [tyt[ti]], eng="pool")
            P.barrier()
            esE.close()
            st2 = SB(esD, "st2", [128, 16], F32)
            junk2 = SB(esD, "junk2", [128, 1024], F32)
            yo = [SB(esD, "yo%d" % i, [128, 1024], F32) for i in range(2)]
            tst2, tj2 = Tok(), Tok()
            tyo = [Tok(), Tok()]
            for ti in range(NT):
                src = yacc[:, ti, :]
                dst, tdst = yo[ti % 2], tyo[ti % 2]
                red(st2[:, 0:1], src, ALU.add, [tyt[ti]], [tst2])
                act(junk2[:], src, AF.Square, [tyt[ti]], [tj2])
                red(st2[:, 1:2], junk2[:, :], ALU.add, [tj2], [tst2])
                ts(st2[:, 2:3], st2[:, 0:1], 1.0 / 1024, None, ALU.mult, None, [tst2], [tst2])
                tt(st2[:, 3:4], st2[:, 2:3], st2[:, 2:3], ALU.mult, [tst2], [tst2])
                stt(st2[:, 4:5], st2[:, 1:2], 1.0 / 1024, st2[:, 3:4], ALU.mult, ALU.subtract, [tst2], [tst2])
                ts(st2[:, 5:6], st2[:, 4:5], EPS, None, ALU.add, None, [tst2], [tst2])
                act(st2[:, 5:6], st2[:, 5:6], AF.Ln, [tst2], [tst2])
                act(st2[:, 5:6], st2[:, 5:6], AF.Exp, [tst2], [tst2], scale=-0.5)
                ts(dst[:, :], src, st2[:, 2:3], st2[:, 5:6], ALU.subtract, ALU.mult, [tyt[ti], tst2], [tdst])
                tt(dst[:, :], dst[:, :], lnf[:, 0, :], ALU.mult, [tdst, tlnf], [tdst], eng="pool")
                tt(dst[:, :], dst[:, :], lnf[:, 1, :], ALU.add, [tdst, tlnf], [tdst], eng="pool")
                dma("sp", y_out[ti * 128:(ti + 1) * 128, :], dst[:, :], [tdst], [])
        P.enabled = True
        P.emit()
        nc._kstats = P.stats
        nc._nops = len(P.ops)
        nc._marks = getattr(P, 'marks', [])
        nc._oplist = [(o.eng, o.is_dma, P.lines.get(o.idx)) for o in P.ops]
        nc._ext_in, nc._ext_out = ext_in, ext_out
    return nc


_NC_CACHE = {}


def _make_in_maps(x_prompt, x_sample, mem_prompt, cache_dil_k, cache_dil_v, state_gla, cache_mem_k, cache_mem_v,
           w_in, w_gate_lr, b_gate, g_gla_norm, w_out, ln_mix_g, ln_mix_b,
           w_mem_q, w_mem_k, w_mem_v, w_mem_o, ln_mem_g, ln_mem_b,
           w_route_group, b_route_group, w_route_expert, b_route_expert,
           w_exp_gate, w_exp_up, w_exp_down, ln_ffn_g, ln_ffn_b):
    f = lambda a: np.ascontiguousarray(np.asarray(a, dtype=np.float32))
    x_prompt, x_sample = f(x_prompt), f(x_sample)
    consts = _const_tables()
    rep = lambda v: np.ascontiguousarray(np.broadcast_to(np.asarray(v, np.float32).reshape(1, -1), (128, np.asarray(v).size)))
    shared = {
        "w_in": f(w_in[0]),
        "wlr": np.ascontiguousarray(np.concatenate([f(w_gate_lr[0]), f(b_gate[0]).reshape(1, 256)], axis=0)),
        "g4": np.ascontiguousarray(f(g_gla_norm[0]).reshape(4, 128).T),
        "w_out": f(w_out[0]), "w_q": f(w_mem_q[0]), "w_k": f(w_mem_k[0]), "w_v": f(w_mem_v[0]), "w_o": f(w_mem_o[0]),
        "lnp": np.ascontiguousarray(np.stack([rep(ln_mix_g[0]), rep(ln_mix_b[0]), rep(ln_mem_g[0]), rep(ln_mem_b[0]),
                                              rep(ln_ffn_g[0]), rep(ln_ffn_b[0])], axis=0)),
        "w_r": np.ascontiguousarray(np.concatenate([f(w_route_group[0]), f(w_route_expert[0])], axis=1)),
        "b_r": rep(np.concatenate([f(b_route_group[0]), f(b_route_expert[0])], axis=0)),
        "w_eg": f(w_exp_gate[0]), "w_eu": f(w_exp_up[0]), "w_ed": f(w_exp_down[0]),
    }
    for k, v in consts.items():
        shared["c_" + k] = v
    in_maps = []
    for c in range(8):
        b, s = c // 4, c % 4
        xp = np.zeros((8192, 1024), np.float32)
        lo = (s - 3) * 2048
        src_lo = max(lo, 0)
        xp[src_lo - lo:] = x_prompt[b, src_lo:(s + 1) * 2048]
        xs = x_sample[16 * c:16 * c + 16].reshape(128, 1024)
        m = dict(shared)
        m.update({
            "xp": xp,
            "halo_bias": np.full((128, 1), 0.0 if s > 0 else NEG, np.float32),
            "xs": np.ascontiguousarray(xs),
            "xres": np.ascontiguousarray(np.concatenate([x_prompt[b, s * 2048:(s + 1) * 2048], xs], axis=0)),
            "memp": f(mem_prompt[b]),
            "cdk": f(cache_dil_k[0, 16 * c:16 * c + 16]).reshape(16, 2048, 512),
            "cdv": f(cache_dil_v[0, 16 * c:16 * c + 16]).reshape(16, 2048, 512),
            "sgla": f(state_gla[0, 16 * c:16 * c + 16]),
            "cmk": f(cache_mem_k[0, 16 * c:16 * c + 16]).reshape(16, 256, 1024),
            "cmv": f(cache_mem_v[0, 16 * c:16 * c + 16]).reshape(16, 256, 1024),
        })
        in_maps.append(m)
    return in_maps


def kernel(**inputs):
    if "nc" not in _NC_CACHE:
        _NC_CACHE["nc"] = build_nc()
    nc = _NC_CACHE["nc"]
    in_maps = _make_in_maps(**inputs)
    res = run_bass_kernel_spmd(nc, in_maps, core_ids=list(range(8)))
    return _assemble(res.results)


def _assemble(R):
    y_prompt = np.stack([np.concatenate([R[4 * b + s]["y"][:2048] for s in range(4)], axis=0) for b in range(2)], axis=0)
    y_sample = np.concatenate([R[c]["y"][2048:].reshape(16, 8, 1024) for c in range(8)], axis=0)
    ndk_p = np.stack([R[4 * b + 3]["ndk_p"].reshape(2048, 4, 128) for b in range(2)], axis=0)[None]
    ndv_p = np.stack([R[4 * b + 3]["ndv_p"].reshape(2048, 4, 128) for b in range(2)], axis=0)[None]
    sg_p = np.stack([R[4 * b + 3]["sg_p"] for b in range(2)], axis=0)[None]
    mk_p = np.stack([R[4 * b]["mk_p"].reshape(256, 4, 256) for b in range(2)], axis=0)[None]
    mv_p = np.stack([R[4 * b]["mv_p"].reshape(256, 4, 256) for b in range(2)], axis=0)[None]
    ndk_s = np.concatenate([R[c]["ndk_s"].reshape(16, 2048, 4, 128) for c in range(8)], axis=0)[None]
    ndv_s = np.concatenate([R[c]["ndv_s"].reshape(16, 2048, 4, 128) for c in range(8)], axis=0)[None]
    sg_s = np.concatenate([R[c]["sg_s"] for c in range(8)], axis=0)[None]
    outs = (y_prompt, y_sample, ndk_p, ndv_p, sg_p, mk_p, mv_p, ndk_s, ndv_s, sg_s)
    return tuple(np.ascontiguousarray(o.astype(np.float32)) for o in outs)
```

```python
import contextlib
import numpy as np
import concourse.bass as bass
import concourse.mybir as mybir
from concourse.bass_utils import run_bass_kernel_spmd

F32 = mybir.dt.float32
BF16 = mybir.dt.bfloat16
AF = mybir.ActivationFunctionType
ALU = mybir.AluOpType
AX = mybir.AxisListType

NEG = -30000.0
ALPHA = 2.0 ** 0.25
EPS = 1e-5
NTOK = 2176
NT = 17


class Tok:
    __slots__ = ("writer", "readers", "psum")

    def __init__(self, psum=False):
        self.writer = None
        self.readers = []
        self.psum = psum


class Op:
    __slots__ = ("eng", "fn", "deps", "is_dma", "signal", "sigval", "slot", "idx", "is_barrier")

    def __init__(self, eng, fn, is_dma):
        self.eng = eng
        self.fn = fn
        self.deps = []
        self.is_dma = is_dma
        self.signal = False
        self.sigval = 0
        self.slot = None
        self.idx = -1
        self.is_barrier = False


class Prog:
    NSLOT = 8
    CE = ("pe", "act", "dve", "pool")

    def __init__(self, nc):
        self.nc = nc
        self.ops = []
        self.engs = {"pe": nc.tensor, "act": nc.scalar, "dve": nc.vector, "pool": nc.gpsimd, "sp": nc.sync}
        self.last = {}
        self.dmas = []
        self.enabled = True

    def _add(self, eng, fn, reads, writes, is_dma):
        if not self.enabled:
            return None
        op = Op(eng, fn, is_dma)
        op.idx = len(self.ops)
        import sys as _sys
        fr = _sys._getframe(2)
        lines = []
        while fr is not None and len(lines) < 4:
            lines.append(fr.f_lineno)
            fr = fr.f_back
        self.lines = getattr(self, "lines", {})
        self.lines[op.idx] = lines
        deps = {}
        for t in reads:
            if t.writer is not None:
                deps[t.writer.idx] = t.writer
            if t.psum:
                for r in t.readers:
                    if r.eng != eng:
                        deps[r.idx] = r
        for t in writes:
            if t.writer is not None:
                deps[t.writer.idx] = t.writer
            for r in t.readers:
                deps[r.idx] = r
        for t in reads:
            t.readers.append(op)
        for t in writes:
            t.writer = op
            t.readers = []
        for d in deps.values():
            if (not d.is_dma) and (not is_dma) and d.eng == "pe" and eng == "pe":
                continue
            op.deps.append(d)
            d.signal = True
        self.ops.append(op)
        if is_dma:
            self.dmas.append(op)
        else:
            self.last[eng] = op
        return op

    def op(self, eng, fn, reads=(), writes=()):
        return self._add(eng, fn, list(reads), list(writes), False)

    def dma(self, queue, fn, reads=(), writes=()):
        o = self._add(queue, fn, list(reads), list(writes), True)
        if o is not None:
            o.signal = True
        return o

    def barrier(self):
        if not self.enabled:
            return
        prev = list(self.last.values()) + list(self.dmas)
        self.dmas = []
        for e in ("pe", "act", "dve", "pool", "sp"):
            op = Op(e, lambda eng: eng.nop(), False)
            op.is_barrier = True
            op.idx = len(self.ops)
            for d in prev:
                op.deps.append(d)
                d.signal = True
            self.ops.append(op)
            if e != "sp":
                self.last[e] = op

    def emit(self):
        nc = self.nc
        with contextlib.ExitStack() as es:
            esem = {e: es.enter_context(nc.semaphore("s_" + e)) for e in self.CE}
            dsem = {q: [es.enter_context(nc.semaphore("d_%s%d" % (q, i))) for i in range(self.NSLOT)]
                    for q in ("sp", "pool", "act")}
            ecount = {e: 0 for e in esem}
            dcount = {q: [0] * self.NSLOT for q in dsem}
            dnext = {q: 0 for q in dsem}
            waited = {}

            def wait(engname, sem, val):
                key = (engname, id(sem))
                if waited.get(key, 0) >= val:
                    return
                waited[key] = val
                self.engs[engname].wait_ge(sem, val)

            import os as _os
            _kstop = int(_os.environ.get("KSTOP", "0")) or len(self.ops)
            for op in self.ops[:_kstop]:
                e = op.eng
                for d in op.deps:
                    if d.is_dma:
                        wait(e, dsem[d.eng][d.slot], d.sigval)
                    else:
                        wait(e, esem[d.eng], d.sigval)
                if op.is_dma:
                    s = dnext[e]
                    dnext[e] = (s + 1) % self.NSLOT
                    if dcount[e][s] > 0:
                        wait(e, dsem[e][s], dcount[e][s])
                    ins = op.fn(self.engs[e])
                    dcount[e][s] += 16
                    op.slot = s
                    op.sigval = dcount[e][s]
                    ins.then_inc(dsem[e][s], 16)
                else:
                    if getattr(op, "is_barrier", False) and e == "pe":
                        self.marks = getattr(self, "marks", []) + [ecount["pe"]]
                    ins = op.fn(self.engs[e])
                    if op.signal and e in esem:
                        ecount[e] += 1
                        op.sigval = ecount[e]
                        ins.then_inc(esem[e], 1)
            self.stats = (dict(ecount), {q: list(v) for q, v in dcount.items()})
            for q in dsem:
                for s in range(self.NSLOT):
                    if dcount[q][s] > 0:
                        wait("sp", dsem[q][s], dcount[q][s])
            for e in esem:
                if ecount[e] > 0:
                    wait("sp", esem[e], ecount[e])


def _const_tables():
    c = {}
    j = np.arange(128)[:, None]
    i = np.arange(128)[None, :]
    c["uneg"] = np.where(j <= i, -1.0 / 16, 0.0).astype(np.float32)
    c["mgtneg"] = np.where(j > i, -1.0 / 16, 0.0).astype(np.float32)
    am = (j <= i).astype(np.float32)
    c["amask"] = np.ascontiguousarray(np.broadcast_to(am[:, None, :], (128, 4, 128)))
    sameb = (j // 8) == (i // 8)
    c["uneg_s"] = np.where(sameb & (j <= i), -1.0 / 16, 0.0).astype(np.float32)
    c["mgtneg_s"] = np.where(sameb & (j > i), -1.0 / 16, 0.0).astype(np.float32)
    ams = (sameb & (j <= i)).astype(np.float32)
    c["amask_s"] = np.ascontiguousarray(np.broadcast_to(ams[:, None, :], (128, 4, 128)))
    c["rowmask"] = ((np.arange(128)[:, None] // 8) == np.arange(16)[None, :]).astype(np.float32)
    qrel = np.arange(256)[None, :]
    kk = np.arange(128)[:, None]
    c["dilmask"] = np.where((qrel - kk >= 0) & (qrel - kk <= 128), 0.0, NEG).astype(np.float32)
    part = np.arange(128)[:, None, None]
    jj = np.arange(16)[None, :, None]
    t = np.arange(8)[None, None, :]
    delta = 2048 + t - (16 * part + jj)

    def mult(d):
        return ((d >= 0) & (d <= 128)).astype(np.float32) + ((d >= 0) & (d % 4 == 0) & (d <= 512)) + \
               ((d >= 0) & (d % 16 == 0) & (d <= 2048))
    sm = mult(delta).astype(np.float32)
    c["smult"] = np.ascontiguousarray(np.broadcast_to(sm[:, None, :, :], (128, 4, 16, 8))).reshape(128, 512)
    dn = (i % 8) - (j % 8)
    c["snew"] = np.where(sameb, mult(dn), 0.0).astype(np.float32)
    c["ident"] = np.eye(128, dtype=np.float32)
    return c


def build_nc(phases="STUPBCD"):
    nc = bass.Bass("TRN2", target_bir_lowering=False)

    ext_in, ext_out = [], []

    def din(name, shape, ph="*", dt=F32):
        on = ph == "*" or any(c in phases for c in ph)
        if on:
            ext_in.append(name)
        return nc.dram_tensor(name, list(shape), dt, kind="ExternalInput" if on else "Internal").ap()

    def dout(name, shape, ph="*"):
        on = ph == "*" or any(c in phases for c in ph)
        if on:
            ext_out.append(name)
        return nc.dram_tensor(name, list(shape), F32, kind="ExternalOutput" if on else "Internal").ap()

    def dscr(name, shape, dt):
        return nc.dram_tensor(name, list(shape), dt, kind="Internal").ap()

    xp = din("xp", [8192, 1024], "P")
    halo_bias = din("halo_bias", [128, 1])
    xs_in = din("xs", [128, 1024], "S")
    xres = din("xres", [NTOK, 1024], "C")
    memp = din("memp", [256, 1024], "C")
    cdk = din("cdk", [16, 2048, 512], "UD")
    cdv = din("cdv", [16, 2048, 512], "UD")
    sgla = din("sgla", [16, 4, 64, 128], "ST")
    cmk = din("cmk", [16, 256, 1024], "C")
    cmv = din("cmv", [16, 256, 1024], "C")
    w_in = din("w_in", [1024, 3088])
    wlr_in = din("wlr", [17, 256])
    g4_in = din("g4", [128, 4])
    w_out = din("w_out", [1024, 1024], "C")
    w_q = din("w_q", [1024, 1024], "C")
    w_k = din("w_k", [1024, 1024], "C")
    w_v = din("w_v", [1024, 1024], "C")
    w_o = din("w_o", [1024, 1024], "C")
    lnp = din("lnp", [6, 128, 1024], "CD")
    w_r = din("w_r", [1024, 36], "C")
    b_r = din("b_r", [128, 36], "C")
    w_eg = din("w_eg", [32, 1024, 512], "D")
    w_eu = din("w_eu", [32, 1024, 512], "D")
    w_ed = din("w_ed", [32, 512, 1024], "D")
    ctab = {k: din("c_" + k, list(v.shape)) for k, v in _const_tables().items()}
    y_out = dout("y", [NTOK, 1024], "D")
    ndk_p = dout("ndk_p", [2048, 512], "P")
    ndv_p = dout("ndv_p", [2048, 512], "P")
    sg_p = dout("sg_p", [4, 64, 128], "P")
    mk_p = dout("mk_p", [256, 1024], "C")
    mv_p = dout("mv_p", [256, 1024], "C")
    ndk_s = dout("ndk_s", [16, 2048, 512], "SUD")
    ndv_s = dout("ndv_s", [16, 2048, 512], "SUD")
    sg_s = dout("sg_s", [16, 4, 64, 128], "T")
    dbg = dout("dbg", [128, 2048], "Z")
    vscr = dscr("vscr", [4096, 512], BF16)
    qscr = dscr("qscr", [128, 4, 2048], BF16)
    kscr = dscr("kscr", [128, 4, 4096], BF16)
    x2scr = dscr("x2scr", [NTOK, 1024], F32)

    P = Prog(nc)

    def mm(out, lhsT, rhs, start, stop, reads, writes, skip=False):
        if skip:
            P.op("pe", lambda e: e.matmul(out, lhsT, rhs, start=start, stop=stop, skip_group_check=True), reads, writes)
        else:
            P.op("pe", lambda e: e.matmul(out, lhsT, rhs, start=start, stop=stop), reads, writes)

    def tr(out, in_, ident, reads, writes):
        P.op("pe", lambda e: e.transpose(out, in_, ident), reads, writes)

    def act(out, in_, func, reads, writes, bias=None, scale=1.0):
        if bias is None:
            P.op("act", lambda e: e.activation(out=out, in_=in_, func=func, scale=scale), reads, writes)
        else:
            P.op("act", lambda e: e.activation(out=out, in_=in_, func=func, bias=bias, scale=scale), reads, writes)

    def cp(eng, out, in_, reads, writes):
        if eng == "act":
            P.op("act", lambda e: e.copy(out, in_), reads, writes)
        else:
            P.op(eng, lambda e: e.tensor_copy(out, in_), reads, writes)

    def tt(out, in0, in1, op, reads, writes, eng="dve"):
        P.op(eng, lambda e: e.tensor_tensor(out=out, in0=in0, in1=in1, op=op), reads, writes)

    def stt(out, in0, scalar, in1, op0, op1, reads, writes, eng="dve"):
        P.op(eng, lambda e: e.scalar_tensor_tensor(out=out, in0=in0, scalar=scalar, in1=in1, op0=op0, op1=op1), reads, writes)

    def ts(out, in0, s1, s2, op0, op1, reads, writes, eng="dve"):
        if s2 is None:
            P.op(eng, lambda e: e.tensor_scalar(out=out, in0=in0, scalar1=s1, scalar2=None, op0=op0), reads, writes)
        else:
            P.op(eng, lambda e: e.tensor_scalar(out=out, in0=in0, scalar1=s1, scalar2=s2, op0=op0, op1=op1), reads, writes)

    def recip(out, in_, reads, writes):
        P.op("dve", lambda e: e.reciprocal(out, in_), reads, writes)

    def red(out, in_, op, reads, writes):
        P.op("dve", lambda e: e.tensor_reduce(out=out, in_=in_, axis=AX.X, op=op), reads, writes)

    def dma(q, out, in_, reads, writes):
        P.dma(q, lambda e: e.dma_start(out=out, in_=in_), reads, writes)

    with contextlib.ExitStack() as es0:
        def SB(es, name, shape, dt):
            return es.enter_context(nc.sbuf_tensor("sb_" + name, list(shape), dt))

        def PS(name, shape, dt):
            return es0.enter_context(nc.psum_tensor("ps_" + name, list(shape), dt))

        pX = PS("pX", [128, 1024], BF16)
        pF = [PS("pF0", [128, 512], F32), PS("pF1", [128, 512], F32)]
        pZ = PS("pZ", [128, 512], F32)
        pB = PS("pB", [128, 512], F32)
        pV = PS("pV", [128, 512], F32)
        pA = PS("pA", [128, 512], F32)
        pO = PS("pO", [128, 512], F32)
        tX, tF0, tF1, tZa, tBa, tV, tA, tO = [Tok(psum=True) for _ in range(8)]
        tZb, tBb = tZa, tBa
        tF = [tF0, tF1]
        fcnt = [0]

        def nextF():
            i = fcnt[0] % 2
            fcnt[0] += 1
            return pF[i], tF[i]

        ident_f = SB(es0, "ident_f", [128, 128], F32)
        ident_b = SB(es0, "ident_b", [128, 128], BF16)
        ones_b = SB(es0, "ones_b", [128, 128], BF16)
        ones_f = SB(es0, "ones_f", [128, 128], F32)
        cst = SB(es0, "cst", [128, 4], F32)
        hb = SB(es0, "hb", [128, 1], F32)
        catT = SB(es0, "catT", [128, 8, NTOK], BF16)
        gate = SB(es0, "gate", [128, NT, 32], F32)
        tC, tcat, tx2T, tgate = Tok(), Tok(), Tok(), Tok()
        tcatt = [Tok() for _ in range(NT)]
        x2T, tx2t = catT, tcatt
        dma("sp", ident_f[:], ctab["ident"], [], [tC])
        cp("dve", ident_b[:], ident_f[:], [tC], [tC])
        P.op("pool", lambda e: e.memset(ones_b[:], 1.0), [], [tC])
        P.op("pool", lambda e: e.memset(ones_f[:], 1.0), [], [tC])
        P.op("pool", lambda e: e.memset(cst[:, 0:1], 1.0), [], [tC])
        P.op("pool", lambda e: e.memset(cst[:, 1:2], 0.0), [], [tC])
        P.op("pool", lambda e: e.memset(cst[:, 2:3], EPS), [], [tC])
        dma("sp", hb[:], halo_bias, [], [tC])
        ONE = cst[:, 0:1]

        with contextlib.ExitStack() as esA:
            wi = SB(esA, "wi", [128, 8, 3088], BF16)
            twi = Tok()
            dma("pool", wi[:], w_in.rearrange("(kc p) n -> p kc n", p=128), [], [twi])
            wlr = SB(esA, "wlr", [17, 256], F32)
            g4 = SB(esA, "g4", [128, 4], F32)
            cm = {}
            for k in ("uneg", "mgtneg", "amask", "uneg_s", "mgtneg_s", "amask_s", "rowmask", "snew"):
                shp = list(ctab[k].shape)
                cm[k] = SB(esA, "c_" + k, shp, F32)
            tM = Tok()
            dma("sp", wlr[:], wlr_in, [], [tM])
            dma("sp", g4[:], g4_in, [], [tM])
            for k in cm:
                dma("sp", cm[k][:], ctab[k], [], [tM])

            alrT = SB(esA, "alrT", [32, 512], F32)
            talr = Tok()
            P.op("pool", lambda e: e.memset(alrT[:], 1.0), [], [talr])
            qg_f = SB(esA, "qg_f", [128, 2, 512], F32)
            kg_f = SB(esA, "kg_f", [128, 2, 512], F32)
            sr_f = SB(esA, "sr_f", [128, 4, 512], F32)
            tqg, tkg, tsr = Tok(), Tok(), Tok()
            e1 = SB(esA, "e1", [128, 256], F32)
            sp = SB(esA, "sp", [128, 256], F32)
            eb = SB(esA, "eb", [128, 2, 128], F32)
            enb = SB(esA, "enb", [128, 2, 128], F32)
            qt = SB(esA, "qt", [128, 2, 128], BF16)
            kt = SB(esA, "kt", [128, 4, 128], BF16)
            A_bf = SB(esA, "A_bf", [128, 4, 128], BF16)
            v_bf = SB(esA, "v_bf", [128, 512], BF16)
            ek = SB(esA, "ek", [128, 256], F32)
            kend = SB(esA, "kend", [128, 256], BF16)
            S32 = SB(esA, "S32", [128, 2, 128], F32)
            S_bf = SB(esA, "S_bf", [128, 4, 128], BF16)
            sq = SB(esA, "sq", [128, 512], BF16)
            rstd = SB(esA, "rstd", [128, 4, 128], F32)
            t1 = SB(esA, "t1", [128, 4, 128], F32)
            te1, tsp, teb, tenb, tqt, tkt, tAbf, tvbf, tek, tkend, tS32, tSbf, tsq, trstd, tt1 = [Tok() for _ in range(15)]
            P.op("pool", lambda e: e.memset(S32[:], 0.0), [], [tS32])
            P.op("pool", lambda e: e.memset(S_bf[:], 0.0), [], [tSbf])
            P.op("pool", lambda e: e.memset(kt[:], 0.0), [], [tkt])

            pZ3 = pZ
            pB3 = pB[:, 0:256].rearrange("p (a b) -> p a b", a=2)
            pA3 = pA[:, :].rearrange("p (a b) -> p a b", a=4)
            pO3 = pO[:, :].rearrange("p (a b) -> p a b", a=4)
            pV3 = pV[:, :].rearrange("p (a b) -> p a b", a=2)

            def fm_proj(col0, ncol, xT, txT, t0, ntok, evac):
                pf, tf = nextF()
                for kc in range(8):
                    mm(pf[0:ncol, 0:ntok], wi[:, kc, col0:col0 + ncol], xT[:, kc, t0:t0 + ntok], kc == 0, kc == 7,
                       [twi, txT], [tf])
                evac(pf[0:ncol, 0:ntok], tf)

            def tm_proj(dst, tdst, col0, ncol, xT, txT, t0):
                for kc in range(8):
                    mm(dst, xT[:, kc, t0:t0 + 128], wi[:, kc, col0:col0 + ncol], kc == 0, kc == 7, [twi, txT], [tdst])

            def block_fm_gla(xT, txT, ntok, with_q):
                fm_proj(1536, 16, xT, txT, 0, ntok,
                        lambda p, tf: cp("dve", alrT[0:16, 0:ntok], p, [tf], [talr]))
                if with_q:
                    for pr in range(2):
                        fm_proj(pr * 128, 128, xT, txT, 0, ntok,
                                lambda p, tf, pr=pr: cp("act", qg_f[:, pr, 0:ntok], p, [tf], [tqg]))
                        fm_proj(256 + pr * 128, 128, xT, txT, 0, ntok,
                                lambda p, tf, pr=pr: cp("dve", kg_f[:, pr, 0:ntok], p, [tf], [tkg]))
                    for h in range(4):
                        fm_proj(1024 + h * 128, 128, xT, txT, 0, ntok,
                                lambda p, tf, h=h: act(sr_f[:, h, 0:ntok], p, AF.Silu, [tf], [tsr]))

            def gla_chunk(xT, txT, c0, mode, cat_t0=None, tcat_tok=None):
                full = mode != "pre"
                sfx = "_s" if mode == "sample" else ""
                uneg, mgtneg, amask = cm["uneg" + sfx], cm["mgtneg" + sfx], cm["amask" + sfx]
                mm(pZ[:, 0:256], alrT[0:17, c0:c0 + 128], wlr[0:17, :], True, True, [talr, tM], [tZa])
                act(e1[:], pZ[:, 0:256], AF.Exp, [tZa], [te1], scale=-1.0)
                act(sp[:], e1[:], AF.Ln, [te1], [tsp], bias=ONE)
                tm_proj(pB[:, 256:512], tBb, 256, 256, xT, txT, c0)
                tm_proj(pV[:, :], tV, 512, 512, xT, txT, c0)
                cp("act", v_bf[:], pV[:, :], [tV], [tvbf])
                mm(pZ[:, 256:512], mgtneg[:], sp[:], True, True, [tM, tsp], [tZb])
                act(ek[:], pZ[:, 256:512], AF.Exp, [tZb], [tek])
                tt(kend[:], pB[:, 256:512], ek[:], ALU.mult, [tBb, tek], [tkend])
                if full:
                    for pr in range(2):
                        mm(pB3[:, pr, :], sp[:, pr * 128:(pr + 1) * 128], uneg[:], True, True, [tsp, tM], [tBa])
                    act(eb[:], pB3, AF.Exp, [tBa], [teb])
                    act(enb[:], pB3, AF.Exp, [tBa], [tenb], scale=-1.0)
                    stt(qt[:], qg_f[:, :, c0:c0 + 128], 0.125, eb[:], ALU.mult, ALU.mult, [tqg, teb], [tqt])
                    for hp in range(2):
                        rs = slice(64 * hp, 64 * hp + 64)
                        tt(kt[rs, hp::2, :], kg_f[rs, :, c0:c0 + 128], enb[rs, :, :], ALU.mult, [tkg, tenb], [tkt])
                    for h in range(4):
                        mm(pA3[:, h, :], kt[:, h, :], qt[:, h // 2, :], True, True, [tkt, tqt], [tA])
                    tt(A_bf[:], pA3, amask[:], ALU.mult, [tA, tM], [tAbf])
                else:
                    for pr in range(2):
                        mm(pB3[:, pr, 127:128], sp[:, pr * 128:(pr + 1) * 128], uneg[:, 127:128], True, True,
                           [tsp, tM], [tBa])
                    act(eb[:, :, 127:128], pB3[:, :, 127:128], AF.Exp, [tBa], [teb])
                return full

            def gla_out(c0, cat_t0, tcat_tok):
                act(sq[:], pO[:, :], AF.Square, [tO], [tsq])
                mm(pA[:, :], ones_b[:], sq[:], True, True, [tC, tsq], [tA])
                ts(rstd[:].rearrange("p a b -> p (a b)"), pA[:, :], 1.0 / 128, EPS, ALU.mult, ALU.add, [tA], [trstd])
                act(rstd[:].rearrange("p a b -> p (a b)"), rstd[:].rearrange("p a b -> p (a b)"), AF.Ln, [trstd], [trstd])
                act(rstd[:].rearrange("p a b -> p (a b)"), rstd[:].rearrange("p a b -> p (a b)"), AF.Exp, [trstd], [trstd], scale=-0.5)
                for h in range(4):
                    stt(t1[:, h, :], pO3[:, h, :], g4[:, h:h + 1], rstd[:, h, :], ALU.mult, ALU.mult, [tO, tM, trstd], [tt1])
                tt(catT[:, 0:4, cat_t0:cat_t0 + 128], t1[:], sr_f[:, :, c0:c0 + 128], ALU.mult, [tt1, tsr], [tcat_tok])

            def state_update():
                for pr in range(2):
                    mm(pV3[:, pr, :], kend[:, pr * 128:(pr + 1) * 128], v_bf[:, pr * 256:(pr + 1) * 256], True, True,
                       [tkend, tvbf], [tV])
                for h in range(4):
                    pr, r0 = h // 2, 64 * (h % 2)
                    stt(S32[r0:r0 + 64, pr, :], S32[r0:r0 + 64, pr, :], eb[r0:r0 + 64, pr, 127:128],
                        pV3[r0:r0 + 64, pr, (h % 2) * 128:(h % 2) * 128 + 128], ALU.mult, ALU.add, [tS32, teb, tV], [tS32])
                for hp in range(2):
                    rs = slice(64 * hp, 64 * hp + 64)
                    cp("act" if hp else "dve", S_bf[rs, hp::2, :], S32[rs, :, :], [tS32], [tSbf])

            P.enabled = "S" in phases
            with contextlib.ExitStack() as esS:
                xs_bf = SB(esS, "xs_bf", [128, 1024], BF16)
                xsT = SB(esS, "xsT", [128, 8, 128], BF16)
                qdT_s = SB(esS, "qdT_s", [128, 4, 128], BF16)
                kdT_s = SB(esS, "kdT_s", [128, 4, 128], BF16)
                kd_new = SB(esS, "kd_new", [128, 512], F32)
                vd_new = SB(esS, "vd_new", [128, 512], F32)
                vd_new_b = SB(esS, "vd_new_b", [128, 512], BF16)
                esG = contextlib.ExitStack()
                S0b = SB(esG, "S0b", [128, 16, 4, 128], BF16)
                S0f = SB(esG, "S0f", [128, 16, 2, 128], F32)
                Snew = S0f
                Vblk = SB(esG, "Vblk", [128, 16, 128], BF16)
                txs, txsT, tqd, tkd, tkdn, tvdn, tvdnb, tS0, tSn, tVb = [Tok() for _ in range(10)]
                dma("pool", xs_bf[:], xs_in, [], [txs])
                sg_v = sgla.rearrange("b (pr h2) k v -> (h2 k) b pr v", h2=2)
                P.op("pool", lambda e: e.memset(S0b[:], 0.0), [], [tS0])
                sg_h = sgla.rearrange("b (pr h2) k v -> h2 k b pr v", h2=2)
                for b4 in range(4):
                    for hp in range(2):
                        dma("pool", S0b[64 * hp:64 * hp + 64, b4 * 4:(b4 + 1) * 4, hp::2, :], sg_h[hp, :, b4 * 4:(b4 + 1) * 4], [], [tS0])
                    dma("sp", S0f[:, b4 * 4:(b4 + 1) * 4], sg_v[:, b4 * 4:(b4 + 1) * 4], [], [tS0])
                for kc in range(8):
                    tr(pX[:, kc * 128:(kc + 1) * 128], xs_bf[:, kc * 128:(kc + 1) * 128], ident_b[:], [txs, tC], [tX])
                cp("dve", xsT[:].rearrange("p a b -> p (a b)"), pX[:, :], [tX], [txsT])
                block_fm_gla(xsT, txsT, 128, True)
                for h in range(4):
                    fm_proj(1552 + h * 128, 128, xsT, txsT, 0, 128,
                            lambda p, tf, h=h: cp("dve", qdT_s[:, h, :], p, [tf], [tqd]))
                    fm_proj(2064 + h * 128, 128, xsT, txsT, 0, 128,
                            lambda p, tf, h=h: cp("act", kdT_s[:, h, :], p, [tf], [tkd]))
                pf, tf = nextF()
                tm_proj(pf[:, :], tf, 2064, 512, xsT, txsT, 0)
                cp("dve", kd_new[:], pf[:, :], [tf], [tkdn])
                pf, tf = nextF()
                tm_proj(pf[:, :], tf, 2576, 512, xsT, txsT, 0)
                cp("dve", vd_new[:], pf[:, :], [tf], [tvdn])
                cp("act", vd_new_b[:], pf[:, :], [tf], [tvdnb])
                for b in range(16):
                    dma("sp", ndk_s[b, 2040:2048, :], kd_new[8 * b:8 * b + 8, :], [tkdn], [])
                    dma("sp", ndv_s[b, 2040:2048, :], vd_new[8 * b:8 * b + 8, :], [tvdn], [])
                gla_chunk(xsT, txsT, 0, "sample")
                for h in range(4):
                    pr, r0 = h // 2, 64 * (h % 2)
                    mm(pO3[:, h, :], v_bf[:, h * 128:(h + 1) * 128], A_bf[:, h, :], True, False, [tvbf, tAbf], [tO], skip=True)
                    for b in range(16):
                        mm(pO3[:, h, 8 * b:8 * b + 8], S0b[:, b, h, :], qt[:, pr, 8 * b:8 * b + 8],
                           False, b == 15, [tS0, tqt], [tO], skip=True)
                gla_out(0, 2048, tcatt[16])
                P.enabled = "T" in phases
                for h in range(4):
                    pr, r0 = h // 2, 64 * (h % 2)
                    for b in range(16):
                        ts(Vblk[:, b, :], v_bf[:, h * 128:(h + 1) * 128], cm["rowmask"][:, b:b + 1], None, ALU.mult, None,
                           [tvbf, tM], [tVb])
                    for q4 in range(4):
                        pf, tf = nextF()
                        mm(pf[:, :], kend[:, pr * 128:(pr + 1) * 128],
                           Vblk[:, q4 * 4:(q4 + 1) * 4, :].rearrange("p a b -> p (a b)"), True, True, [tkend, tVb], [tf])
                        for bb in range(4):
                            b = q4 * 4 + bb
                            stt(Snew[r0:r0 + 64, b, pr, :], S0f[r0:r0 + 64, b, pr, :], eb[r0:r0 + 64, pr, 8 * b + 7:8 * b + 8],
                                pf[r0:r0 + 64, bb * 128:(bb + 1) * 128], ALU.mult, ALU.add, [tS0, teb, tf], [tSn])
                sgs_v = sg_s.rearrange("b (pr h2) k v -> (h2 k) b pr v", h2=2)
                for b4 in range(4):
                    dma("sp", sgs_v[:, b4 * 4:(b4 + 1) * 4], Snew[:, b4 * 4:(b4 + 1) * 4], [tSn], [])

                P.enabled = ("S" in phases) or ("T" in phases)
                P.barrier()
                esG.close()
                P.enabled = "U" in phases
                Kc = SB(esS, "Kc", [128, 16, 512], BF16)
                Vc = SB(esS, "Vc", [128, 16, 512], BF16)
                KcTb = [SB(esS, "KcT%d" % i, [128, 16, 128], BF16) for i in range(2)]
                tKcTb = [Tok(), Tok()]
                Pe = SB(esS, "Pe", [128, 512], F32)
                Pm = SB(esS, "Pm", [128, 4, 16, 8], BF16)
                smult = SB(esS, "smult", [128, 512], F32)
                Pn = SB(esS, "Pn", [128, 128], F32)
                Pnb = SB(esS, "Pnb", [128, 4, 128], BF16)
                rd = SB(esS, "rd", [128, 4, 128], F32)
                tKc, tVc, tPe, tPm, tsm, tPn, tPnb, trd = [Tok() for _ in range(8)]
                dma("sp", smult[:], ctab["smult"], [], [tsm])
                for h in range(4):
                    pf, tf = nextF()
                    mm(pf[:, 0:128], kdT_s[:, h, :], qdT_s[:, h, :], True, True, [tkd, tqd], [tf])
                    act(Pn[:], pf[:, 0:128], AF.Exp, [tf], [tPn], scale=128.0 ** -0.5)
                    tt(Pnb[:, h, :], Pn[:], cm["snew"][:], ALU.mult, [tPn, tM], [tPnb])
                Pms = SB(esS, "Pms", [128, 4, 8], F32)
                tPms = Tok()
                pVn = pV[:, :].rearrange("p (a b) -> p a b", a=4)
                for h in range(4):
                    mm(pO3[:, h, :], vd_new_b[:, h * 128:(h + 1) * 128], Pnb[:, h, :], True, False, [tvdnb, tPnb], [tO], skip=True)
                    mm(pVn[:, h, :], ones_b[:], Pnb[:, h, :], True, True, [tC, tPnb], [tV])
                for b in range(16):
                    dma("pool", Kc[:], cdk[b].rearrange("(p j) f -> p j f", j=16), [], [tKc])
                    dma("pool", Vc[:], cdv[b].rearrange("(p j) f -> p j f", j=16), [], [tVc])
                    pf, tf = nextF()
                    pf4 = pf[:, :].rearrange("p (h j t) -> p h j t", h=4, j=16)
                    for h in range(4):
                        KcT, tKcT = KcTb[h % 2], tKcTb[h % 2]
                        for j2 in range(2):
                            for jj in range(8):
                                j = j2 * 8 + jj
                                tr(pX[:, jj * 128:(jj + 1) * 128], Kc[:, j, h * 128:(h + 1) * 128], ident_b[:], [tKc, tC], [tX])
                            cp("dve" if j2 == 0 else "act", KcT[:, j2 * 8:(j2 + 1) * 8, :].rearrange("p a b -> p (a b)"),
                               pX[:, :], [tX], [tKcT])
                        for j in range(16):
                            mm(pf4[:, h, j, :], KcT[:, j, :], qdT_s[:, h, 8 * b:8 * b + 8], True, True, [tKcT, tqd], [tf])
                    act(Pe[:], pf[:, :], AF.Exp, [tf], [tPe], scale=128.0 ** -0.5)
                    tt(Pm[:].rearrange("p h j t -> p (h j t)"), Pe[:], smult[:], ALU.mult, [tPe, tsm], [tPm])
                    for h in range(4):
                        for j in range(16):
                            last = (b == 15 and j == 15)
                            mm(pO3[:, h, 8 * b:8 * b + 8], Vc[:, j, h * 128:(h + 1) * 128], Pm[:, h, j, :], False, last,
                               [tVc, tPm], [tO], skip=True)
                    P.op("dve", lambda e: e.tensor_reduce(out=Pms[:], in_=Pm[:].rearrange("p h j t -> p h t j"), axis=AX.X, op=ALU.add),
                         [tPm], [tPms])
                    mm(pA[:, b * 32:(b + 1) * 32], ones_f[:], Pms[:].rearrange("p h t -> p (h t)"), True, True, [tC, tPms], [tA])
                rd4 = rd[:].rearrange("p h (b t) -> p h b t", b=16)
                cp("act", rd[:], pVn, [tV], [trd])
                tt(rd4, pA[:, :].rearrange("p (b h t) -> p h b t", b=16, h=4), rd4, ALU.add, [tA, trd], [trd])
                recip(rd[:], rd[:], [trd], [trd])
                tt(catT[:, 4:8, 2048:2176], pO3, rd[:], ALU.mult, [tO, trd], [tcatt[16]])
            P.barrier()

            P.enabled = "P" in phases
            with contextlib.ExitStack() as esP:
                xld = [SB(esP, "xld%d" % i, [128, 4, 1024], BF16) for i in range(2)]
                xTb = [SB(esP, "xTb%d" % i, [128, 8, 512], BF16) for i in range(2)]
                txld = [Tok(), Tok()]
                txTb = [Tok(), Tok()]
                stg = [SB(esP, "stg%d" % i, [128, 512], BF16) for i in range(2)]
                tstg = [Tok(), Tok()]
                stf = [SB(esP, "stf%d" % i, [128, 512], F32) for i in range(2)]
                tstf = [Tok(), Tok()]
                scnt = [0, 0]
                tscrQ, tscrK, tscrV = Tok(), Tok(), Tok()
                for blk in range(16):
                    mode = "pre" if blk < 8 else ("halo" if blk < 12 else "main")
                    bi = blk % 2
                    xl, xT, txl, txT = xld[bi], xTb[bi], txld[bi], txTb[bi]
                    dma("pool", xl[:], xp[blk * 512:(blk + 1) * 512, :].rearrange("(j p) f -> p j f", p=128), [], [txl])
                    for j in range(4):
                        for kc in range(8):
                            tr(pX[:, kc * 128:(kc + 1) * 128], xl[:, j, kc * 128:(kc + 1) * 128], ident_b[:], [txl, tC], [tX])
                        cp("dve" if j % 2 == 0 else "act", xT[:, :, j * 128:(j + 1) * 128],
                           pX[:, :].rearrange("p (a b) -> p a b", a=8), [tX], [txT])
                    block_fm_gla(xT, txT, 512, mode == "main")
                    if mode != "pre":
                        hoff = (blk - 8) * 512
                        for h in range(4):
                            def ev_k(p, tf, h=h):
                                i = scnt[0] % 2
                                scnt[0] += 1
                                cp("dve", stg[i][:, :], p, [tf], [tstg[i]])
                                dma("sp", kscr[:, h, hoff:hoff + 512], stg[i][:, :], [tstg[i]], [tscrK])
                            fm_proj(2064 + h * 128, 128, xT, txT, 0, 512, ev_k)
                        if mode == "main":
                            moff = (blk - 12) * 512
                            for h in range(4):
                                def ev_q(p, tf, h=h):
                                    i = scnt[0] % 2
                                    scnt[0] += 1
                                    cp("act", stg[i][:, :], p, [tf], [tstg[i]])
                                    dma("sp", qscr[:, h, moff:moff + 512], stg[i][:, :], [tstg[i]], [tscrQ])
                                fm_proj(1552 + h * 128, 128, xT, txT, 0, 512, ev_q)
                    for j in range(4):
                        c0 = j * 128
                        full = gla_chunk(xT, txT, c0, "main" if mode == "main" else "pre")
                        if mode != "pre":
                            pf, tf = nextF()
                            tm_proj(pf[:, :], tf, 2576, 512, xT, txT, c0)
                            i = scnt[0] % 2
                            scnt[0] += 1
                            cp("act", stg[i][:, :], pf[:, :], [tf], [tstg[i]])
                            row0 = (blk - 8) * 512 + c0
                            dma("sp", vscr[row0:row0 + 128, :], stg[i][:, :], [tstg[i]], [tscrV])
                            if mode == "main":
                                orow = (blk - 12) * 512 + c0
                                i2 = scnt[1] % 2
                                scnt[1] += 1
                                cp("dve", stf[i2][:, :], pf[:, :], [tf], [tstf[i2]])
                                dma("sp", ndv_p[orow:orow + 128, :], stf[i2][:, :], [tstf[i2]], [])
                                pf, tf = nextF()
                                tm_proj(pf[:, :], tf, 2064, 512, xT, txT, c0)
                                i2 = scnt[1] % 2
                                scnt[1] += 1
                                cp("dve", stf[i2][:, :], pf[:, :], [tf], [tstf[i2]])
                                dma("sp", ndk_p[orow:orow + 128, :], stf[i2][:, :], [tstf[i2]], [])
                        if full:
                            tok0 = (blk - 12) * 512 + c0
                            for h in range(4):
                                pr, r0 = h // 2, 64 * (h % 2)
                                mm(pO3[:, h, :], v_bf[:, h * 128:(h + 1) * 128], A_bf[:, h, :], True, False, [tvbf, tAbf], [tO])
                                mm(pO3[:, h, :], S_bf[:, h, :], qt[:, pr, :], False, True, [tSbf, tqt], [tO])
                            gla_out(c0, tok0, tcatt[tok0 // 128])
                        state_update()
                dma("sp", sg_p.rearrange("(pr h2) k v -> (h2 k) pr v", h2=2), S32[:], [tS32], [])
            P.barrier()
        P.barrier()

        P.enabled = "B" in phases
        with contextlib.ExitStack() as esB:
            QT = SB(esB, "QT", [128, 4, 2048], BF16)
            KT = SB(esB, "KT", [128, 4, 4096], BF16)
            dmask = SB(esB, "dmask", [128, 256], F32)
            dmask_b = SB(esB, "dmask_b", [128, 256], BF16)
            nacc = SB(esB, "nacc", [128, 2048], F32)
            dacc = SB(esB, "dacc", [128, 2048], F32)
            tQ, tK, tdm, tnacc, tdacc = [Tok() for _ in range(5)]
            NV = 8
            Vt = [SB(esB, "Vt%d" % i, [128, 128], BF16) for i in range(NV)]
            tVt = [Tok() for _ in range(NV)]
            NP_ = 8
            PT = [SB(esB, "PT%d" % i, [128, 256], BF16) for i in range(NP_)]
            tPT = [Tok() for _ in range(NP_)]
            for h in range(4):
                dma("sp", QT[:, h, :], qscr[:, h, :], [tscrQ], [tQ])
                dma("sp", KT[:, h, :], kscr[:, h, :], [tscrK], [tK])
            dma("sp", dmask[:], ctab["dilmask"], [], [tdm])
            cp("dve", dmask_b[:], dmask[:], [tdm], [tdm])
            scale = 128.0 ** -0.5
            numb = [(pO, tO), (pA, tA)]
            denb = [(pZ, tZa), (pB, tBa)]
            cnt = [0, 0, 0]
            for h in range(4):
                first_branch = True
                for (d, nres, nb) in ((1, 1, 16), (4, 4, 4), (16, 16, 1)):
                    qlist = [(r, qb) for r in range(nres) for qb in range(nb)]
                    for g0 in range(0, len(qlist), 4):
                        grp = qlist[g0:g0 + 4]
                        pn, tn = numb[cnt[2] % 2]
                        pd, td = denb[cnt[2] % 2]
                        cnt[2] += 1
                        ktiles = []
                        for (r, qb) in grp:
                            for kt_ in (qb - 1, qb):
                                if (r, kt_) not in ktiles:
                                    ktiles.append((r, kt_))
                        slot_of = {rq: i for i, rq in enumerate(grp)}
                        opened = set()
                        pend = []
                        for (r, kt_) in ktiles:
                            qbs = [qb for qb in (kt_, kt_ + 1) if (r, qb) in slot_of]
                            q_lo = min(qbs)
                            ncol = 128 * len(qbs)
                            mcol0 = 128 * (q_lo - kt_)
                            kidx0 = 2048 + r + d * 128 * kt_
                            qidx0 = r + d * 128 * q_lo
                            pf, tf = nextF()
                            mm(pf[:, 0:ncol], KT[:, h, kidx0:kidx0 + d * 127 + 1:d], QT[:, h, qidx0:qidx0 + d * (ncol - 1) + 1:d],
                               True, False, [tK, tQ], [tf])
                            mm(pf[:, 0:ncol], ident_b[:], dmask_b[:, mcol0:mcol0 + ncol], False, True, [tC, tdm], [tf])
                            ip = cnt[0] % NP_
                            cnt[0] += 1
                            act(PT[ip][:, 0:ncol], pf[:, 0:ncol], AF.Exp, [tf, tC], [tPT[ip]],
                                bias=(hb[:, 0:1] if kt_ < 0 else cst[:, 1:2]), scale=scale)
                            iv = cnt[1] % NV
                            cnt[1] += 1
                            row0 = 2048 + r + d * 128 * kt_
                            dma("sp", Vt[iv][:, :], vscr[row0:row0 + d * 127 + 1:d, h * 128:(h + 1) * 128], [tscrV], [tVt[iv]])
                            pend.append((ip, iv, qbs, q_lo, r, kt_))
                        contrib = {}
                        for (ip, iv, qbs, q_lo, r, kt_) in pend:
                            for qb in qbs:
                                contrib.setdefault((r, qb), []).append((ip, iv, 128 * (qb - q_lo)))
                        for (r, qb), lst in contrib.items():
                            sl = slot_of[(r, qb)]
                            for n_, (ip, iv, c0) in enumerate(lst):
                                st, sp_ = n_ == 0, n_ == len(lst) - 1
                                mm(pn[:, sl * 128:(sl + 1) * 128], Vt[iv][:, :], PT[ip][:, c0:c0 + 128], st, sp_,
                                   [tVt[iv], tPT[ip]], [tn])
                                mm(pd[:, sl * 128:(sl + 1) * 128], ones_b[:], PT[ip][:, c0:c0 + 128], st, sp_,
                                   [tC, tPT[ip]], [td])
                        for sl, (r, qb) in enumerate(grp):
                            a0 = r + d * 128 * qb
                            dst_n = nacc[:, a0:a0 + d * 127 + 1:d]
                            dst_d = dacc[:, a0:a0 + d * 127 + 1:d]
                            if first_branch:
                                cp("dve", dst_n, pn[:, sl * 128:(sl + 1) * 128], [tn], [tnacc])
                                cp("act", dst_d, pd[:, sl * 128:(sl + 1) * 128], [td], [tdacc])
                            else:
                                tt(dst_n, dst_n, pn[:, sl * 128:(sl + 1) * 128], ALU.add, [tn, tnacc], [tnacc])
                                tt(dst_d, dst_d, pd[:, sl * 128:(sl + 1) * 128], ALU.add, [td, tdacc], [tdacc])
                    first_branch = False
                recip(dacc[:], dacc[:], [tdacc], [tdacc])
                for q4 in range(4):
                    tt(catT[:, 4 + h, q4 * 512:(q4 + 1) * 512], nacc[:, q4 * 512:(q4 + 1) * 512], dacc[:, q4 * 512:(q4 + 1) * 512],
                       ALU.mult, [tnacc, tdacc], [tcatt[q4 * 4 + i] for i in range(4)])
        P.barrier()

        P.enabled = "C" in phases
        with contextlib.ExitStack() as esC:
            wo_ = SB(esC, "wout", [128, 8, 1024], BF16)
            wq_ = SB(esC, "wq", [128, 8, 1024], BF16)
            wmo_ = SB(esC, "wmo", [128, 8, 1024], BF16)
            mKT = SB(esC, "mKT", [128, 8, 256], BF16)
            mV = SB(esC, "mV", [128, 2, 1024], BF16)
            lnt = SB(esC, "lnt", [128, 4, 1024], F32)
            wr_ = SB(esC, "wr", [128, 8, 36], F32)
            br_ = SB(esC, "br", [128, 36], F32)
            esM = contextlib.ExitStack()
            wtmp = SB(esM, "wtmp", [128, 8, 1024], BF16)
            tW, tWt, tln = Tok(), Tok(), Tok()
            tWo, tWq, tWmo = Tok(), Tok(), Tok()
            for i in range(4):
                dma("sp", lnt[:, i, :], lnp[i], [], [tln])
            dma("sp", wr_[:], w_r.rearrange("(kc p) n -> p kc n", p=128), [], [tln])
            dma("sp", br_[:], b_r, [], [tln])
            mem_bf = SB(esM, "mem_bf", [128, 2, 1024], BF16)
            memT = SB(esM, "memT", [128, 8, 256], BF16)
            mo32 = [SB(esM, "mo32_%d" % i, [128, 512], F32) for i in range(2)]
            tmem, tmemT, tmKT, tmV = Tok(), Tok(), Tok(), Tok()
            tmo32 = [Tok(), Tok()]
            dma("pool", mem_bf[:], memp.rearrange("(mt p) f -> p mt f", p=128), [], [tmem])
            for mt in range(2):
                for kc in range(8):
                    tr(pX[:, kc * 128:(kc + 1) * 128], mem_bf[:, mt, kc * 128:(kc + 1) * 128], ident_b[:], [tmem, tC], [tX])
                cp("dve", memT[:, :, mt * 128:(mt + 1) * 128], pX[:, :].rearrange("p (a b) -> p a b", a=8), [tX], [tmemT])
            mcnt = [0]
            for which, wsrc, dst in ((0, w_k, mk_p), (1, w_v, mv_p)):
                dma("pool", wtmp[:], wsrc.rearrange("(kc p) n -> p kc n", p=128), [], [tWt])
                if which == 0:
                    dma("pool", wo_[:], w_out.rearrange("(kc p) n -> p kc n", p=128), [], [tWo])
                    dma("pool", wq_[:], w_q.rearrange("(kc p) n -> p kc n", p=128), [], [tWq])
                    dma("pool", wmo_[:], w_o.rearrange("(kc p) n -> p kc n", p=128), [], [tWmo])
                for mt in range(2):
                    for half in range(2):
                        pf, tf = nextF()
                        for kc in range(8):
                            mm(pf[:, :], memT[:, kc, mt * 128:(mt + 1) * 128], wtmp[:, kc, half * 512:(half + 1) * 512],
                               kc == 0, kc == 7, [tmemT, tWt], [tf])
                        i = mcnt[0] % 2
                        mcnt[0] += 1
                        cp("dve", mo32[i][:, :], pf[:, :], [tf], [tmo32[i]])
                        dma("sp", dst[mt * 128:(mt + 1) * 128, half * 512:(half + 1) * 512], mo32[i][:, :], [tmo32[i]], [])
                        if which == 1:
                            cp("act", mV[:, mt, half * 512:(half + 1) * 512], pf[:, :], [tf], [tmV])
                if which == 0:
                    for ch in range(8):
                        pf, tf = nextF()
                        for kc in range(8):
                            mm(pf[:, 0:256], wtmp[:, kc, ch * 128:(ch + 1) * 128], memT[:, kc, :], kc == 0, kc == 7,
                               [tWt, tmemT], [tf])
                        cp("act", mKT[:, ch, :], pf[:, 0:256], [tf], [tmKT])

            P.barrier()
            esM.close()
            xin = [SB(esC, "xin%d" % i, [128, 1024], F32) for i in range(2)]
            txin = [Tok(), Tok()]
            SETS = []
            for si in range(2):
                d_ = {}
                for nm, shp, dt in (("u", [128, 1024], F32), ("x1", [128, 1024], F32), ("x1b", [128, 1024], BF16),
                                    ("x1T", [128, 8, 128], BF16), ("qT", [128, 8, 128], BF16), ("pTm", [128, 2, 128], BF16),
                                    ("omT", [128, 8, 128], BF16), ("rdn", [128, 128], F32), ("x2", [128, 1024], F32),
                                    ("x2b", [128, 1024], BF16), ("x2Tf", [128, 8, 128], F32), ("st", [128, 16], F32),
                                    ("lg", [128, 36], F32), ("rt", [128, 8, 32], F32)):
                    d_[nm] = SB(esC, "%s_%d" % (nm, si), shp, dt)
                    d_["t" + nm] = Tok()
                SETS.append(d_)
            cK = SB(esC, "cK", [128, 2, 1024], BF16)
            cV = SB(esC, "cV", [128, 2, 1024], BF16)
            cKT = SB(esC, "cKT", [128, 8, 256], BF16)
            pTs = SB(esC, "pTs", [128, 2, 4, 8], BF16)
            tcK, tcV, tcKT, tpTs = Tok(), Tok(), Tok(), Tok()

            def layer_norm(src, tsrc, gi, dst, tdst, st, tst):
                red(st[:, 0:1], src[:, :], ALU.add, [tsrc], [tst])
                act(dst[:, :], src[:, :], AF.Square, [tsrc], [tdst])
                red(st[:, 1:2], dst[:, :], ALU.add, [tdst], [tst])
                ts(st[:, 2:3], st[:, 0:1], 1.0 / 1024, None, ALU.mult, None, [tst], [tst])
                tt(st[:, 3:4], st[:, 2:3], st[:, 2:3], ALU.mult, [tst], [tst])
                stt(st[:, 4:5], st[:, 1:2], 1.0 / 1024, st[:, 3:4], ALU.mult, ALU.subtract, [tst], [tst])
                ts(st[:, 5:6], st[:, 4:5], EPS, None, ALU.add, None, [tst], [tst])
                act(st[:, 5:6], st[:, 5:6], AF.Ln, [tst], [tst])
                act(st[:, 5:6], st[:, 5:6], AF.Exp, [tst], [tst], scale=-0.5)
                ts(dst[:, :], src[:, :], st[:, 2:3], st[:, 5:6], ALU.subtract, ALU.mult, [tsrc, tst], [tdst])
                tt(dst[:, :], dst[:, :], lnt[:, gi, :], ALU.mult, [tdst, tln], [tdst], eng="pool")
                tt(dst[:, :], dst[:, :], lnt[:, gi + 1, :], ALU.add, [tdst, tln], [tdst], eng="pool")

            pM = [pO, pA]
            tMM = [tO, tA]
            pO8 = [pZ, pB]
            tO8 = [tZa, tBa]
            def tile_body(ti):
                d_ = SETS[ti % 2]
                u, x1, x1b, x1T, qT, pTm, omT, rdn, x2, x2b, x2Tf, st, lg, rt = [d_[k] for k in (
                    "u", "x1", "x1b", "x1T", "qT", "pTm", "omT", "rdn", "x2", "x2b", "x2Tf", "st", "lg", "rt")]
                tu, tx1, tx1b, tx1T, tqT, tpTm, tomT, trdn, tx2, tx2b, tx2Tf, tst, tlg, trt = [d_["t" + k] for k in (
                    "u", "x1", "x1b", "x1T", "qT", "pTm", "omT", "rdn", "x2", "x2b", "x2Tf", "st", "lg", "rt")]
                xi, txi = xin[ti % 2], txin[ti % 2]
                dma("sp", xi[:], xres[ti * 128:(ti + 1) * 128, :], [], [txi])
                for half in range(2):
                    for kc in range(8):
                        mm(pM[half][:, :], catT[:, kc, ti * 128:(ti + 1) * 128], wo_[:, kc, half * 512:(half + 1) * 512],
                           kc == 0, kc == 7, [tcatt[ti], tWo], [tMM[half]])
                    stt(u[:, half * 512:(half + 1) * 512], xi[:, half * 512:(half + 1) * 512], ALPHA, pM[half][:, :],
                        ALU.mult, ALU.add, [txi, tMM[half]], [tu])
                yield
                layer_norm(u, tu, 0, x1, tx1, st, tst)
                cp("act", x1b[:], x1[:], [tx1], [tx1b])
                for kc in range(8):
                    tr(pX[:, kc * 128:(kc + 1) * 128], x1b[:, kc * 128:(kc + 1) * 128], ident_b[:], [tx1b, tC], [tX])
                cp("dve", x1T[:].rearrange("p a b -> p (a b)"), pX[:, :], [tX], [tx1T])
                yield
                for ch in range(8):
                    pf, tf = nextF()
                    for kc in range(8):
                        mm(pf[:, 0:128], wq_[:, kc, ch * 128:(ch + 1) * 128], x1T[:, kc, :], kc == 0, kc == 7, [tWq, tx1T], [tf])
                    cp("act" if ch % 2 else "dve", qT[:, ch, :], pf[:, 0:128], [tf], [tqT])
                yield
                if ti < 16:
                    for h in range(4):
                        pf, tf = nextF()
                        pf3 = pf[:, 0:256].rearrange("p (a b) -> p a b", a=2)
                        for mt in range(2):
                            for cc in range(2):
                                mm(pf3[:, mt, :], mKT[:, h * 2 + cc, mt * 128:(mt + 1) * 128], qT[:, h * 2 + cc, :],
                                   cc == 0, cc == 1, [tmKT, tqT], [tf])
                        act(pTm[:], pf3, AF.Exp, [tf], [tpTm], scale=1.0 / 16)
                        for cc in range(2):
                            ch = h * 2 + cc
                            bank, tb = pO8[ch // 4], tO8[ch // 4]
                            for mt in range(2):
                                mm(bank[:, (ch % 4) * 128:(ch % 4 + 1) * 128], mV[:, mt, ch * 128:(ch + 1) * 128], pTm[:, mt, :],
                                   mt == 0, mt == 1, [tmV, tpTm], [tb])
                        pf2, tf2 = nextF()
                        for mt in range(2):
                            mm(pf2[:, 0:128], ones_b[:], pTm[:, mt, :], mt == 0, mt == 1, [tC, tpTm], [tf2])
                        recip(rdn[:], pf2[:, 0:128], [tf2], [trdn])
                        for cc in range(2):
                            ch = h * 2 + cc
                            bank, tb = pO8[ch // 4], tO8[ch // 4]
                            tt(omT[:, ch, :], bank[:, (ch % 4) * 128:(ch % 4 + 1) * 128], rdn[:], ALU.mult, [tb, trdn], [tomT])
                else:
                    pf_d, tf_d = pV, tV
                    pfd4 = pf_d[:, :].rearrange("p (b h t) -> p h b t", b=16, h=4)
                    for b in range(16):
                        dma("pool", cK[:], cmk[b].rearrange("(mt p) f -> p mt f", p=128), [], [tcK])
                        dma("pool", cV[:], cmv[b].rearrange("(mt p) f -> p mt f", p=128), [], [tcV])
                        for mt in range(2):
                            for kc in range(8):
                                tr(pX[:, kc * 128:(kc + 1) * 128], cK[:, mt, kc * 128:(kc + 1) * 128], ident_b[:], [tcK, tC], [tX])
                            cp("dve" if mt == 0 else "act", cKT[:, :, mt * 128:(mt + 1) * 128],
                               pX[:, :].rearrange("p (a b) -> p a b", a=8), [tX], [tcKT])
                        pf, tf = nextF()
                        pf4 = pf[:, 0:64].rearrange("p (m h t) -> p m h t", m=2, h=4)
                        for h in range(4):
                            for mt in range(2):
                                for cc in range(2):
                                    mm(pf4[:, mt, h, :], cKT[:, h * 2 + cc, mt * 128:(mt + 1) * 128], qT[:, h * 2 + cc, 8 * b:8 * b + 8],
                                       cc == 0, cc == 1, [tcKT, tqT], [tf])
                        act(pTs[:].rearrange("p m h t -> p (m h t)"), pf[:, 0:64], AF.Exp, [tf], [tpTs], scale=1.0 / 16)
                        for ch in range(8):
                            h = ch // 2
                            bank, tb = pO8[ch // 4], tO8[ch // 4]
                            for mt in range(2):
                                mm(bank[:, (ch % 4) * 128 + 8 * b:(ch % 4) * 128 + 8 * b + 8], cV[:, mt, ch * 128:(ch + 1) * 128],
                                   pTs[:, mt, h, :], mt == 0, mt == 1, [tcV, tpTs], [tb])
                        for mt in range(2):
                            mm(pf_d[:, b * 32:(b + 1) * 32], ones_b[:], pTs[:, mt].rearrange("p h t -> p (h t)"), mt == 0, mt == 1,
                               [tC, tpTs], [tf_d])
                    for h in range(4):
                        recip(rdn[:].rearrange("p (b t) -> p b t", b=16), pfd4[:, h], [tf_d], [trdn])
                        for cc in range(2):
                            ch = h * 2 + cc
                            bank, tb = pO8[ch // 4], tO8[ch // 4]
                            tt(omT[:, ch, :], bank[:, (ch % 4) * 128:(ch % 4 + 1) * 128], rdn[:], ALU.mult, [tb, trdn], [tomT])
                yield
                for half in range(2):
                    for kc in range(8):
                        mm(pM[half][:, :], omT[:, kc, :], wmo_[:, kc, half * 512:(half + 1) * 512], kc == 0, kc == 7,
                           [tomT, tWmo], [tMM[half]])
                    stt(u[:, half * 512:(half + 1) * 512], x1[:, half * 512:(half + 1) * 512], ALPHA, pM[half][:, :],
                        ALU.mult, ALU.add, [tx1, tMM[half]], [tu])
                yield
                layer_norm(u, tu, 2, x2, tx2, st, tst)
                cp("act", x2b[:], x2[:], [tx2], [tx2b])
                for kc in range(8):
                    tr(pX[:, kc * 128:(kc + 1) * 128], x2b[:, kc * 128:(kc + 1) * 128], ident_b[:], [tx2b, tC], [tX])
                cp("dve", x2T[:, :, ti * 128:(ti + 1) * 128], pX[:, :].rearrange("p (a b) -> p a b", a=8), [tX], [tx2t[ti]])
                yield
                for half in range(2):
                    for k4 in range(4):
                        kc = half * 4 + k4
                        tr(pM[half][:, k4 * 128:(k4 + 1) * 128], x2[:, kc * 128:(kc + 1) * 128], ident_f[:], [tx2, tC], [tMM[half]])
                    cp("act", x2Tf[:, half * 4:(half + 1) * 4, :].rearrange("p a b -> p (a b)"), pM[half][:, :], [tMM[half]], [tx2Tf])
                pf, tf = nextF()
                for kc in range(8):
                    mm(pf[:, 0:36], x2Tf[:, kc, :], wr_[:, kc, :], kc == 0, kc == 7, [tx2Tf, tln], [tf])
                tt(lg[:], pf[:, 0:36], br_[:], ALU.add, [tf, tln], [tlg])
                yield
                R = [tlg, trt]
                red(rt[:, 0, 0:1], lg[:, 0:4], ALU.max, R, [trt])
                ts(rt[:, 0, 1:2], rt[:, 0, 0:1], -1.0, None, ALU.mult, None, R, [trt])
                act(rt[:, 1, 0:4], lg[:, 0:4], AF.Exp, R, [trt], bias=rt[:, 0, 1:2])
                red(rt[:, 0, 2:3], rt[:, 1, 0:4], ALU.add, R, [trt])
                recip(rt[:, 0, 3:4], rt[:, 0, 2:3], R, [trt])
                ts(rt[:, 1, 4:8], lg[:, 0:4], rt[:, 0, 0:1], None, ALU.is_equal, None, R, [trt])
                ts(rt[:, 1, 8:12], rt[:, 1, 4:8], -1.0, 30000.0, ALU.add, ALU.mult, R, [trt])
                for g in range(4):
                    ts(rt[:, 2, g * 8:(g + 1) * 8], lg[:, 4 + g * 8:4 + (g + 1) * 8], rt[:, 1, 8 + g:9 + g], None,
                       ALU.add, None, R, [trt])
                red(rt[:, 0, 4:5], rt[:, 2, :], ALU.max, R, [trt])
                ts(rt[:, 3, :], rt[:, 2, :], rt[:, 0, 4:5], None, ALU.is_equal, None, R, [trt])
                stt(rt[:, 4, :], rt[:, 3, :], -60000.0, rt[:, 2, :], ALU.mult, ALU.add, R, [trt])
                red(rt[:, 0, 5:6], rt[:, 4, :], ALU.max, R, [trt])
                ts(rt[:, 5, :], rt[:, 4, :], rt[:, 0, 5:6], None, ALU.is_equal, None, R, [trt])
                tt(rt[:, 0, 6:7], rt[:, 0, 5:6], rt[:, 0, 4:5], ALU.subtract, R, [trt])
                act(rt[:, 0, 7:8], rt[:, 0, 6:7], AF.Exp, R, [trt])
                ts(rt[:, 0, 8:9], rt[:, 0, 7:8], 1.0, None, ALU.add, None, R, [trt])
                recip(rt[:, 0, 9:10], rt[:, 0, 8:9], R, [trt])
                tt(rt[:, 0, 10:11], rt[:, 0, 9:10], rt[:, 0, 7:8], ALU.mult, R, [trt])
                tt(rt[:, 0, 11:12], rt[:, 0, 9:10], rt[:, 0, 3:4], ALU.mult, R, [trt])
                tt(rt[:, 0, 12:13], rt[:, 0, 10:11], rt[:, 0, 3:4], ALU.mult, R, [trt])
                ts(rt[:, 6, :], rt[:, 3, :], rt[:, 0, 11:12], None, ALU.mult, None, R, [trt])
                stt(gate[:, ti, :], rt[:, 5, :], rt[:, 0, 12:13], rt[:, 6, :], ALU.mult, ALU.add, R, [tgate])
                yield
                ts(x2[:, :], x2[:, :], ALPHA, None, ALU.mult, None, [tx2], [tx2], eng="pool")
                dma("sp", x2scr[ti * 128:(ti + 1) * 128, :], x2[:, :], [tx2], [tgate])
                yield
            order = [(0, 1), (2, 3), (4, 5), (6, 7), (8, 9), (10, 11), (12, 13), (14, 15), (16,)]
            for grp_ in order:
                gens_ = [tile_body(t_) for t_ in grp_]
                while gens_:
                    for g_ in list(gens_):
                        try:
                            next(g_)
                        except StopIteration:
                            gens_.remove(g_)
        P.barrier()

        P.enabled = "D" in phases
        with contextlib.ExitStack() as esD:
            yacc = SB(esD, "yacc", [128, NT, 1024], F32)
            lnf = SB(esD, "lnf", [128, 2, 1024], F32)
            tyt = [Tok() for _ in range(NT)]
            tlnf = Tok()
            for ti in range(NT):
                dma("sp", yacc[:, ti, :], x2scr[ti * 128:(ti + 1) * 128, :], [tgate], [tyt[ti]])
            dma("sp", lnf[:, 0, :], lnp[4], [], [tlnf])
            dma("sp", lnf[:, 1, :], lnp[5], [], [tlnf])
            for b in range(16):
                dma("sp", ndk_s[b, 0:2040, :].rearrange("(a r) f -> a (r f)", a=120),
                    cdk[b, 8:2048, :].rearrange("(a r) f -> a (r f)", a=120), [], [])
                dma("sp", ndv_s[b, 0:2040, :].rearrange("(a r) f -> a (r f)", a=120),
                    cdv[b, 8:2048, :].rearrange("(a r) f -> a (r f)", a=120), [], [])
            esE = contextlib.ExitStack()
            Wg = [SB(esE, "Wg%d" % i, [128, 8, 512], BF16) for i in range(2)]
            Wu = [SB(esE, "Wu%d" % i, [128, 8, 512], BF16) for i in range(2)]
            Wd = [SB(esE, "Wd%d" % i, [128, 4, 1024], BF16) for i in range(2)]
            tWg, tWu, tWd = [Tok(), Tok()], [Tok(), Tok()], [Tok(), Tok()]
            sg_ = [SB(esE, "sg%d" % i, [128, 512], F32) for i in range(2)]
            tsg = [Tok(), Tok()]
            hT = [SB(esE, "hT%d" % i, [128, 4, 512], BF16) for i in range(2)]
            thT = [Tok(), Tok()]
            ytmp = [SB(esE, "ytmp%d" % i, [128, 512], F32) for i in range(2)]
            tytmp = [Tok(), Tok()]
            pG = [(pF[0], tF0), (pF[1], tF1)]
            pU = [(pZ, tZa), (pB, tBa)]
            pY = [(pO, tO), (pA, tA), (pV, tV)]
            blocks = [(0, 512), (512, 512), (1024, 512), (1536, 512), (2048, 128)]
            cg, cy, ch_ = 0, 0, 0
            for e in range(32):
                wb = e % 2
                dma("pool", Wg[wb][:], w_eg[e].rearrange("(kc p) n -> p kc n", p=128), [], [tWg[wb]])
                dma("pool", Wu[wb][:], w_eu[e].rearrange("(kc p) n -> p kc n", p=128), [], [tWu[wb]])
                dma("pool", Wd[wb][:], w_ed[e].rearrange("(kc p) n -> p kc n", p=128), [], [tWd[wb]])
                for (t0, nt) in blocks:
                    hb_i = ch_ % 2
                    ch_ += 1
                    tiles = list(range(t0 // 128, (t0 + nt) // 128))
                    for hc in range(4):
                        (pg, tg), (pu, tu_) = pG[cg % 2], pU[cg % 2]
                        sgi = cg % 2
                        cg += 1
                        for kc in range(8):
                            mm(pg[:, 0:nt], Wg[wb][:, kc, hc * 128:(hc + 1) * 128], x2T[:, kc, t0:t0 + nt], kc == 0, kc == 7,
                               [tWg[wb]] + [tx2t[i] for i in tiles], [tg])
                        for kc in range(8):
                            mm(pu[:, 0:nt], Wu[wb][:, kc, hc * 128:(hc + 1) * 128], x2T[:, kc, t0:t0 + nt], kc == 0, kc == 7,
                               [tWu[wb]] + [tx2t[i] for i in tiles], [tu_])
                        act(sg_[sgi][:, 0:nt], pg[:, 0:nt], AF.Silu, [tg], [tsg[sgi]])
                        tt(hT[hb_i][:, hc, 0:nt], sg_[sgi][:, 0:nt], pu[:, 0:nt], ALU.mult, [tsg[sgi], tu_], [thT[hb_i]])
                    for ti in tiles:
                        o0 = ti * 128 - t0
                        for half in range(2):
                            py, ty = pY[cy % 3]
                            cy += 1
                            for hc in range(4):
                                mm(py[:, :], hT[hb_i][:, hc, o0:o0 + 128], Wd[wb][:, hc, half * 512:(half + 1) * 512], hc == 0, hc == 3,
                                   [thT[hb_i], tWd[wb]], [ty])
                            ya = yacc[:, ti, half * 512:(half + 1) * 512]
                            if half == 0:
                                stt(ya, py[:, :], gate[:, ti, e:e + 1], ya, ALU.mult, ALU.add, [ty, tgate, tyt[ti]], [tyt[ti]])
                            else:
                                iy = ti % 2
                                act(ytmp[iy][:, :], py[:, :], AF.Identity, [ty, tgate], [tytmp[iy]], scale=gate[:, ti, e:e + 1])
                                tt(ya, ya, ytmp[iy][:, :], ALU.add, [tytmp[iy], tyt[ti]], [tyt[ti]], eng="pool")
            P.barrier()
            esE.close()
            st2 = SB(esD, "st2", [128, 16], F32)
            junk2 = SB(esD, "junk2", [128, 1024], F32)
            yo = [SB(esD, "yo%d" % i, [128, 1024], F32) for i in range(2)]
            tst2, tj2 = Tok(), Tok()
            tyo = [Tok(), Tok()]
            for ti in range(NT):
                src = yacc[:, ti, :]
                dst, tdst = yo[ti % 2], tyo[ti % 2]
                red(st2[:, 0:1], src, ALU.add, [tyt[ti]], [tst2])
                act(junk2[:], src, AF.Square, [tyt[ti]], [tj2])
                red(st2[:, 1:2], junk2[:, :], ALU.add, [tj2], [tst2])
                ts(st2[:, 2:3], st2[:, 0:1], 1.0 / 1024, None, ALU.mult, None, [tst2], [tst2])
                tt(st2[:, 3:4], st2[:, 2:3], st2[:, 2:3], ALU.mult, [tst2], [tst2])
                stt(st2[:, 4:5], st2[:, 1:2], 1.0 / 1024, st2[:, 3:4], ALU.mult, ALU.subtract, [tst2], [tst2])
                ts(st2[:, 5:6], st2[:, 4:5], EPS, None, ALU.add, None, [tst2], [tst2])
                act(st2[:, 5:6], st2[:, 5:6], AF.Ln, [tst2], [tst2])
                act(st2[:, 5:6], st2[:, 5:6], AF.Exp, [tst2], [tst2], scale=-0.5)
                ts(dst[:, :], src, st2[:, 2:3], st2[:, 5:6], ALU.subtract, ALU.mult, [tyt[ti], tst2], [tdst])
                tt(dst[:, :], dst[:, :], lnf[:, 0, :], ALU.mult, [tdst, tlnf], [tdst], eng="pool")
                tt(dst[:, :], dst[:, :], lnf[:, 1, :], ALU.add, [tdst, tlnf], [tdst], eng="pool")
                dma("sp", y_out[ti * 128:(ti + 1) * 128, :], dst[:, :], [tdst], [])
        P.enabled = True
        P.emit()
        nc._kstats = P.stats
        nc._nops = len(P.ops)
        nc._marks = getattr(P, 'marks', [])
        nc._oplist = [(o.eng, o.is_dma, P.lines.get(o.idx)) for o in P.ops]
        nc._ext_in, nc._ext_out = ext_in, ext_out
    return nc


_NC_CACHE = {}


def _make_in_maps(x_prompt, x_sample, mem_prompt, cache_dil_k, cache_dil_v, state_gla, cache_mem_k, cache_mem_v,
           w_in, w_gate_lr, b_gate, g_gla_norm, w_out, ln_mix_g, ln_mix_b,
           w_mem_q, w_mem_k, w_mem_v, w_mem_o, ln_mem_g, ln_mem_b,
           w_route_group, b_route_group, w_route_expert, b_route_expert,
           w_exp_gate, w_exp_up, w_exp_down, ln_ffn_g, ln_ffn_b):
    f = lambda a: np.ascontiguousarray(np.asarray(a, dtype=np.float32))
    x_prompt, x_sample = f(x_prompt), f(x_sample)
    consts = _const_tables()
    rep = lambda v: np.ascontiguousarray(np.broadcast_to(np.asarray(v, np.float32).reshape(1, -1), (128, np.asarray(v).size)))
    shared = {
        "w_in": f(w_in[0]),
        "wlr": np.ascontiguousarray(np.concatenate([f(w_gate_lr[0]), f(b_gate[0]).reshape(1, 256)], axis=0)),
        "g4": np.ascontiguousarray(f(g_gla_norm[0]).reshape(4, 128).T),
        "w_out": f(w_out[0]), "w_q": f(w_mem_q[0]), "w_k": f(w_mem_k[0]), "w_v": f(w_mem_v[0]), "w_o": f(w_mem_o[0]),
        "lnp": np.ascontiguousarray(np.stack([rep(ln_mix_g[0]), rep(ln_mix_b[0]), rep(ln_mem_g[0]), rep(ln_mem_b[0]),
                                              rep(ln_ffn_g[0]), rep(ln_ffn_b[0])], axis=0)),
        "w_r": np.ascontiguousarray(np.concatenate([f(w_route_group[0]), f(w_route_expert[0])], axis=1)),
        "b_r": rep(np.concatenate([f(b_route_group[0]), f(b_route_expert[0])], axis=0)),
        "w_eg": f(w_exp_gate[0]), "w_eu": f(w_exp_up[0]), "w_ed": f(w_exp_down[0]),
    }
    for k, v in consts.items():
        shared["c_" + k] = v
    in_maps = []
    for c in range(8):
        b, s = c // 4, c % 4
        xp = np.zeros((8192, 1024), np.float32)
        lo = (s - 3) * 2048
        src_lo = max(lo, 0)
        xp[src_lo - lo:] = x_prompt[b, src_lo:(s + 1) * 2048]
        xs = x_sample[16 * c:16 * c + 16].reshape(128, 1024)
        m = dict(shared)
        m.update({
            "xp": xp,
            "halo_bias": np.full((128, 1), 0.0 if s > 0 else NEG, np.float32),
            "xs": np.ascontiguousarray(xs),
            "xres": np.ascontiguousarray(np.concatenate([x_prompt[b, s * 2048:(s + 1) * 2048], xs], axis=0)),
            "memp": f(mem_prompt[b]),
            "cdk": f(cache_dil_k[0, 16 * c:16 * c + 16]).reshape(16, 2048, 512),
            "cdv": f(cache_dil_v[0, 16 * c:16 * c + 16]).reshape(16, 2048, 512),
            "sgla": f(state_gla[0, 16 * c:16 * c + 16]),
            "cmk": f(cache_mem_k[0, 16 * c:16 * c + 16]).reshape(16, 256, 1024),
            "cmv": f(cache_mem_v[0, 16 * c:16 * c + 16]).reshape(16, 256, 1024),
        })
        in_maps.append(m)
    return in_maps


def kernel(**inputs):
    if "nc" not in _NC_CACHE:
        _NC_CACHE["nc"] = build_nc()
    nc = _NC_CACHE["nc"]
    in_maps = _make_in_maps(**inputs)
    res = run_bass_kernel_spmd(nc, in_maps, core_ids=list(range(8)))
    return _assemble(res.results)


def _assemble(R):
    y_prompt = np.stack([np.concatenate([R[4 * b + s]["y"][:2048] for s in range(4)], axis=0) for b in range(2)], axis=0)
    y_sample = np.concatenate([R[c]["y"][2048:].reshape(16, 8, 1024) for c in range(8)], axis=0)
    ndk_p = np.stack([R[4 * b + 3]["ndk_p"].reshape(2048, 4, 128) for b in range(2)], axis=0)[None]
    ndv_p = np.stack([R[4 * b + 3]["ndv_p"].reshape(2048, 4, 128) for b in range(2)], axis=0)[None]
    sg_p = np.stack([R[4 * b + 3]["sg_p"] for b in range(2)], axis=0)[None]
    mk_p = np.stack([R[4 * b]["mk_p"].reshape(256, 4, 256) for b in range(2)], axis=0)[None]
    mv_p = np.stack([R[4 * b]["mv_p"].reshape(256, 4, 256) for b in range(2)], axis=0)[None]
    ndk_s = np.concatenate([R[c]["ndk_s"].reshape(16, 2048, 4, 128) for c in range(8)], axis=0)[None]
    ndv_s = np.concatenate([R[c]["ndv_s"].reshape(16, 2048, 4, 128) for c in range(8)], axis=0)[None]
    sg_s = np.concatenate([R[c]["sg_s"] for c in range(8)], axis=0)[None]
    outs = (y_prompt, y_sample, ndk_p, ndv_p, sg_p, mk_p, mv_p, ndk_s, ndv_s, sg_s)
    return tuple(np.ascontiguousarray(o.astype(np.float32)) for o in outs)
```
